# Optimizing a Trainium2 kernel written in Bass

```python
import math
import jax
import jax.numpy as jnp
from jax import lax
import numpy as np

D_MODEL = 1024
BATCH = 16
SEQ = 4096
DEPTH = 2

MEM_TOKENS = 256
X_HEADS = 4
X_HEAD_DIM = 64
MAX_POS_OFFSET = 1024

A_HEADS = 8
NOPE_DIM = 64
ROPE_DIM = 32
QK_DIM = NOPE_DIM + ROPE_DIM
V_DIM = 64
Q_LORA = 256
KV_LORA = 128
ROPE_THETA = 10000.0
Q_BLOCK = 128

B_HEADS = 8
SSD_HEAD_DIM = 64
D_INNER = B_HEADS * SSD_HEAD_DIM
SSD_GROUPS = 2
SSD_STATE = 128
CONV_K = 4
CONV_CH = D_INNER + 2 * SSD_GROUPS * SSD_STATE
CHUNK = 128

IN_SPLITS = (Q_LORA,
             Q_LORA + KV_LORA,
             Q_LORA + KV_LORA + ROPE_DIM,
             Q_LORA + KV_LORA + ROPE_DIM + D_INNER,
             Q_LORA + KV_LORA + ROPE_DIM + D_INNER + CONV_CH)
IN_DIM = Q_LORA + KV_LORA + ROPE_DIM + D_INNER + CONV_CH + B_HEADS
MIX_WIDTH = A_HEADS * V_DIM + D_INNER

POOL_WINDOWS = (2, 4, 8, 16)
POOL_GROUP = D_MODEL // 4

MOE_GROUPS = 4
EXPERTS_PER_GROUP = 8
N_EXPERTS = MOE_GROUPS * EXPERTS_PER_GROUP
TOP_K = 2
EXPERT_FF = 256
ROW_BLOCK = 128

RMS_EPS = 1e-6

kernel_name = "hybrid_mla_ssd_pool_hmoe_trunk"


def rms_norm(u, g):
    uf = u.astype(jnp.float32)
    y = uf * lax.rsqrt(jnp.mean(uf * uf, axis=-1, keepdims=True) + RMS_EPS)
    return (y * g.astype(jnp.float32)).astype(u.dtype)


def rope_angles(positions):
    inv = ROPE_THETA ** (-jnp.arange(0, ROPE_DIM // 2, dtype=jnp.float32) * 2.0 / ROPE_DIM)
    ang = positions.astype(jnp.float32)[..., None] * inv
    return jnp.cos(ang)[:, :, None, :], jnp.sin(ang)[:, :, None, :]


def apply_rope(u, cos, sin):
    u1, u2 = jnp.split(u.astype(jnp.float32), 2, axis=-1)
    return jnp.concatenate([u1 * cos - u2 * sin, u2 * cos + u1 * sin], axis=-1).astype(u.dtype)


def causal_block_attention(q, k, v):
    _, s_len, _, dq = q.shape
    scale = dq ** -0.5
    outs = []
    for i in range(s_len // Q_BLOCK):
        lo, hi = i * Q_BLOCK, (i + 1) * Q_BLOCK
        s = jnp.einsum('bqhd,bkhd->bhqk', q[:, lo:hi], k[:, :hi]).astype(jnp.float32) * scale
        mask = (lo + jnp.arange(Q_BLOCK))[:, None] >= jnp.arange(hi)[None, :]
        p = jax.nn.softmax(jnp.where(mask, s, -jnp.inf), axis=-1).astype(v.dtype)
        outs.append(jnp.einsum('bhqk,bkhd->bqhd', p, v[:, :hi]))
    return jnp.concatenate(outs, axis=1)


def causal_depthwise_conv(u, w, b):
    c = u.shape[-1]
    y = lax.conv_general_dilated(u, w[:, None, :].astype(u.dtype), window_strides=(1,),
                                 padding=[(w.shape[0] - 1, 0)],
                                 dimension_numbers=('NWC', 'WIO', 'NWC'),
                                 feature_group_count=c)
    return y + b.astype(u.dtype)


def ssd_chunked_scan(x, dt, a, bm, cm):
    b, L, H, P = x.shape
    G, N = bm.shape[2], bm.shape[3]
    R = H // G
    nc = L // CHUNK
    xdt = (x * dt[..., None]).reshape(b, nc, CHUNK, G, R, P)
    adt = (dt * a).reshape(b, nc, CHUNK, G, R).transpose(0, 3, 4, 1, 2)
    bc = bm.reshape(b, nc, CHUNK, G, N)
    cc = cm.reshape(b, nc, CHUNK, G, N)
    a_cs = jnp.cumsum(adt, axis=-1)
    tri = jnp.tril(jnp.ones((CHUNK, CHUNK), dtype=bool))
    decay = jnp.exp(jnp.where(tri, a_cs[..., :, None] - a_cs[..., None, :], -jnp.inf))
    cb = jnp.einsum('bctgn,bcsgn->bgcts', cc, bc)
    y_diag = jnp.einsum('bgcts,bgrcts,bcsgrp->bctgrp', cb, decay, xdt)
    decay_to_end = jnp.exp(a_cs[..., -1:] - a_cs)
    chunk_states = jnp.einsum('bcsgn,bgrcs,bcsgrp->cbgrpn', bc, decay_to_end, xdt)
    chunk_decay = jnp.exp(a_cs[..., -1]).transpose(3, 0, 1, 2)

    def step(state, inp):
        s_c, d_c = inp
        return state * d_c[..., None, None] + s_c, state

    _, prev = lax.scan(step, jnp.zeros(chunk_states.shape[1:], x.dtype), (chunk_states, chunk_decay))
    y_off = jnp.einsum('bctgn,cbgrpn,bgrct->bctgrp', cc, prev, jnp.exp(a_cs))
    return (y_diag + y_off).reshape(b, L, H, P)


def latent_attention_ssd_mixer(h, cos, sin, w_in, q_lat_norm, w_uq, kv_lat_norm, w_ukv, q_norm, k_norm,
                               conv_w, conv_b, dt_bias, a_log, d_skip, ssd_norm, w_out):
    bsz, s_len, _ = h.shape
    proj = h @ w_in
    q_lat, kv_lat, k_rope, z, xbc, dt_raw = jnp.split(proj, IN_SPLITS, axis=-1)

    q = (rms_norm(q_lat, q_lat_norm) @ w_uq).reshape(bsz, s_len, A_HEADS, QK_DIM)
    kv = (rms_norm(kv_lat, kv_lat_norm) @ w_ukv).reshape(bsz, s_len, A_HEADS, NOPE_DIM + V_DIM)
    k_nope, v = kv[..., :NOPE_DIM], kv[..., NOPE_DIM:]
    k = jnp.concatenate([k_nope, jnp.broadcast_to(k_rope[:, :, None, :],
                                                  (bsz, s_len, A_HEADS, ROPE_DIM))], axis=-1)
    q = rms_norm(q, q_norm)
    k = rms_norm(k, k_norm)
    q = jnp.concatenate([q[..., :NOPE_DIM], apply_rope(q[..., NOPE_DIM:], cos, sin)], axis=-1)
    k = jnp.concatenate([k[..., :NOPE_DIM], apply_rope(k[..., NOPE_DIM:], cos, sin)], axis=-1)
    attn = causal_block_attention(q, k, v).reshape(bsz, s_len, A_HEADS * V_DIM)

    xbc = jax.nn.silu(causal_depthwise_conv(xbc, conv_w, conv_b))
    xs, bm, cm = jnp.split(xbc, [D_INNER, D_INNER + SSD_GROUPS * SSD_STATE], axis=-1)
    xs_h = xs.reshape(bsz, s_len, B_HEADS, SSD_HEAD_DIM).astype(jnp.float32)
    dt = jax.nn.softplus(dt_raw.astype(jnp.float32) + dt_bias.astype(jnp.float32))
    a = -jnp.exp(a_log.astype(jnp.float32))
    y = ssd_chunked_scan(xs_h, dt, a,
                         bm.reshape(bsz, s_len, SSD_GROUPS, SSD_STATE).astype(jnp.float32),
                         cm.reshape(bsz, s_len, SSD_GROUPS, SSD_STATE).astype(jnp.float32))
    y = y + xs_h * d_skip.astype(jnp.float32)[:, None]
    y = y.reshape(bsz, s_len, D_INNER).astype(h.dtype)
    y = rms_norm(y * jax.nn.silu(z), ssd_norm)

    return jnp.concatenate([attn, y], axis=-1) @ w_out


def multiscale_pool_mixer(h, pool_w, pool_b, pool_scale):
    bsz, s_len, d = h.shape
    hf = h.astype(jnp.float32)
    cs = jnp.pad(jnp.cumsum(hf, axis=1), ((0, 0), (1, 0), (0, 0)))
    pos_count = jnp.arange(1, s_len + 1, dtype=jnp.float32)[None, :, None]
    diffs = []
    for g, w in enumerate(POOL_WINDOWS):
        sl = slice(g * POOL_GROUP, (g + 1) * POOL_GROUP)
        csg = cs[:, :, sl]
        win_sum = csg[:, 1:] - jnp.pad(csg[:, :s_len + 1 - w], ((0, 0), (w - 1, 0), (0, 0)))
        diffs.append(win_sum / jnp.minimum(pos_count, float(w)) - hf[:, :, sl])
    dlt = jnp.stack(diffs, axis=2).astype(h.dtype)
    y = jnp.einsum('bsgc,gcd->bsgd', dlt, pool_w).reshape(bsz, s_len, d) + pool_b
    return y * pool_scale


def memory_cross_attention(hq, mem_n, wq, wkv, q_norm, k_norm, wo):
    bsz, s_len, _ = hq.shape
    m_len = mem_n.shape[1]
    q = rms_norm((hq @ wq).reshape(bsz, s_len, X_HEADS, X_HEAD_DIM), q_norm)
    kv = (mem_n @ wkv).reshape(bsz, m_len, 2, X_HEADS, X_HEAD_DIM)
    k = rms_norm(kv[:, :, 0], k_norm)
    v = kv[:, :, 1]
    s = jnp.einsum('bshd,bmhd->bhsm', q, k).astype(jnp.float32) * (X_HEAD_DIM ** -0.5)
    p = jax.nn.softmax(s, axis=-1).astype(v.dtype)
    o = jnp.einsum('bhsm,bmhd->bshd', p, v).reshape(bsz, s_len, X_HEADS * X_HEAD_DIM)
    return o @ wo


def routed_expert_ffn(hf, expert_idx, gates, w_gate, w_up, w_down):
    n_tok, d = hf.shape
    n_exp = w_gate.shape[0]
    m = n_tok * TOP_K
    flat_e = expert_idx.reshape(m)
    order = jnp.argsort(flat_e)
    sorted_e = flat_e[order]
    tok = (order // TOP_K).astype(jnp.int32)
    counts = jnp.bincount(flat_e, length=n_exp)
    padded = (counts + ROW_BLOCK - 1) // ROW_BLOCK * ROW_BLOCK
    pad_end = jnp.cumsum(padded)
    pad_start = pad_end - padded
    start = jnp.cumsum(counts) - counts
    dest = pad_start[sorted_e] + jnp.arange(m) - start[sorted_e]
    n_blocks = -(-m // ROW_BLOCK) + n_exp
    row_tok = jnp.zeros((n_blocks * ROW_BLOCK,), jnp.int32).at[dest].set(tok)
    block_e = jnp.minimum(jnp.searchsorted(pad_end, jnp.arange(n_blocks) * ROW_BLOCK, side='right'),
                          n_exp - 1)
    xb = hf[row_tok].reshape(n_blocks, ROW_BLOCK, d)

    def expert_block(args):
        xblk, e = args
        return (jax.nn.silu(xblk @ w_gate[e]) * (xblk @ w_up[e])) @ w_down[e]

    yb = lax.map(expert_block, (xb, block_e)).reshape(n_blocks * ROW_BLOCK, d)
    y_assign = yb[dest] * gates.reshape(m)[order][:, None]
    return jax.ops.segment_sum(y_assign, tok, num_segments=n_tok)


def hierarchical_moe(h, rg_w, rg_b, re_w, re_b, w_gate, w_up, w_down):
    bsz, s_len, d = h.shape
    n_tok = bsz * s_len
    hf = h.reshape(n_tok, d)
    g_prob = jax.nn.softmax((hf @ rg_w).astype(jnp.float32) + rg_b.astype(jnp.float32), axis=-1)
    g_p, g_idx = lax.top_k(g_prob, 1)
    e_logits = ((hf @ re_w).astype(jnp.float32) + re_b.astype(jnp.float32)).reshape(
        n_tok, MOE_GROUPS, EXPERTS_PER_GROUP)
    e_logits = jnp.take_along_axis(e_logits, g_idx[:, :, None], axis=1)[:, 0]
    e_p, e_idx = lax.top_k(jax.nn.softmax(e_logits, axis=-1), TOP_K)
    gates = g_p * e_p / jnp.sum(e_p, axis=-1, keepdims=True)
    expert_idx = (g_idx * EXPERTS_PER_GROUP + e_idx).astype(jnp.int32)
    y = routed_expert_ffn(hf, expert_idx, gates.astype(h.dtype), w_gate, w_up, w_down)
    return y.reshape(bsz, s_len, d)


def setup_inputs(seed: int = 0) -> dict:
    key = jax.random.key(seed)
    keys = iter(jax.random.split(key, 48))

    def nrm(shape, scale):
        return scale * jax.random.normal(next(keys), shape, jnp.float32)

    def gain(shape):
        return 1.0 + 0.05 * jax.random.normal(next(keys), shape, jnp.float32)

    ne, no = (DEPTH + 1) // 2, DEPTH // 2
    x = nrm((BATCH, SEQ, D_MODEL), 1.0)
    mem = nrm((BATCH, MEM_TOKENS, D_MODEL), 1.0)
    positions = (jax.random.randint(next(keys), (BATCH, 1), 0, MAX_POS_OFFSET, jnp.int32)
                 + jnp.arange(SEQ, dtype=jnp.int32)[None, :])
    ln_mix = gain((DEPTH, D_MODEL))
    w_in = nrm((ne, D_MODEL, IN_DIM), D_MODEL ** -0.5)
    q_lat_norm = gain((ne, Q_LORA))
    w_uq = nrm((ne, Q_LORA, A_HEADS * QK_DIM), Q_LORA ** -0.5)
    kv_lat_norm = gain((ne, KV_LORA))
    w_ukv = nrm((ne, KV_LORA, A_HEADS * (NOPE_DIM + V_DIM)), KV_LORA ** -0.5)
    q_norm = gain((ne, QK_DIM))
    k_norm = gain((ne, QK_DIM))
    conv_w = nrm((ne, CONV_K, CONV_CH), CONV_K ** -0.5)
    conv_b = nrm((ne, CONV_CH), 0.02)
    dt0 = jnp.exp(jax.random.uniform(next(keys), (ne, B_HEADS), jnp.float32,
                                     math.log(1e-3), math.log(1e-1)))
    dt_bias = dt0 + jnp.log(-jnp.expm1(-dt0))
    a_log = jnp.log(jax.random.uniform(next(keys), (ne, B_HEADS), jnp.float32, 1.0, 16.0))
    d_skip = gain((ne, B_HEADS))
    ssd_norm = gain((ne, D_INNER))
    w_out = nrm((ne, MIX_WIDTH, D_MODEL), MIX_WIDTH ** -0.5)
    pool_w = nrm((no, len(POOL_WINDOWS), POOL_GROUP, POOL_GROUP), POOL_GROUP ** -0.5)
    pool_b = nrm((no, D_MODEL), 0.02)
    pool_scale = gain((no, D_MODEL))
    ln_xq = gain((DEPTH, D_MODEL))
    ln_mem = gain((DEPTH, D_MODEL))
    xq_w = nrm((DEPTH, D_MODEL, X_HEADS * X_HEAD_DIM), D_MODEL ** -0.5)
    xkv_w = nrm((DEPTH, D_MODEL, 2 * X_HEADS * X_HEAD_DIM), D_MODEL ** -0.5)
    xq_norm = gain((DEPTH, X_HEAD_DIM))
    xk_norm = gain((DEPTH, X_HEAD_DIM))
    xo_w = nrm((DEPTH, X_HEADS * X_HEAD_DIM, D_MODEL), (X_HEADS * X_HEAD_DIM) ** -0.5)
    ln_ffn = gain((DEPTH, D_MODEL))
    rg_w = nrm((DEPTH, D_MODEL, MOE_GROUPS), D_MODEL ** -0.5)
    rg_b = nrm((DEPTH, MOE_GROUPS), 0.01)
    re_w = nrm((DEPTH, D_MODEL, N_EXPERTS), D_MODEL ** -0.5)
    re_b = nrm((DEPTH, N_EXPERTS), 0.01)
    exp_w_gate = nrm((DEPTH, N_EXPERTS, D_MODEL, EXPERT_FF), D_MODEL ** -0.5)
    exp_w_up = nrm((DEPTH, N_EXPERTS, D_MODEL, EXPERT_FF), D_MODEL ** -0.5)
    exp_w_down = nrm((DEPTH, N_EXPERTS, EXPERT_FF, D_MODEL), EXPERT_FF ** -0.5)
    return {"x": x, "mem": mem, "positions": positions, "ln_mix": ln_mix,
            "w_in": w_in, "q_lat_norm": q_lat_norm, "w_uq": w_uq, "kv_lat_norm": kv_lat_norm,
            "w_ukv": w_ukv, "q_norm": q_norm, "k_norm": k_norm, "conv_w": conv_w, "conv_b": conv_b,
            "dt_bias": dt_bias, "a_log": a_log, "d_skip": d_skip, "ssd_norm": ssd_norm, "w_out": w_out,
            "pool_w": pool_w, "pool_b": pool_b, "pool_scale": pool_scale,
            "ln_xq": ln_xq, "ln_mem": ln_mem, "xq_w": xq_w, "xkv_w": xkv_w, "xq_norm": xq_norm,
            "xk_norm": xk_norm, "xo_w": xo_w, "ln_ffn": ln_ffn, "rg_w": rg_w, "rg_b": rg_b,
            "re_w": re_w, "re_b": re_b, "exp_w_gate": exp_w_gate, "exp_w_up": exp_w_up,
            "exp_w_down": exp_w_down}


def reference(x, mem, positions, ln_mix, w_in, q_lat_norm, w_uq, kv_lat_norm, w_ukv, q_norm, k_norm,
              conv_w, conv_b, dt_bias, a_log, d_skip, ssd_norm, w_out, pool_w, pool_b, pool_scale,
              ln_xq, ln_mem, xq_w, xkv_w, xq_norm, xk_norm, xo_w, ln_ffn, rg_w, rg_b, re_w, re_b,
              exp_w_gate, exp_w_up, exp_w_down):
    cos, sin = rope_angles(positions)
    for layer in range(DEPTH):
        j = layer // 2
        h = rms_norm(x, ln_mix[layer])
        if layer % 2 == 0:
            mixed = latent_attention_ssd_mixer(h, cos, sin, w_in[j], q_lat_norm[j], w_uq[j],
                                               kv_lat_norm[j], w_ukv[j], q_norm[j], k_norm[j],
                                               conv_w[j], conv_b[j], dt_bias[j], a_log[j], d_skip[j],
                                               ssd_norm[j], w_out[j])
        else:
            mixed = multiscale_pool_mixer(h, pool_w[j], pool_b[j], pool_scale[j])
        x = x + mixed
        x = x + memory_cross_attention(rms_norm(x, ln_xq[layer]), rms_norm(mem, ln_mem[layer]),
                                       xq_w[layer], xkv_w[layer], xq_norm[layer], xk_norm[layer],
                                       xo_w[layer])
        x = x + hierarchical_moe(rms_norm(x, ln_ffn[layer]), rg_w[layer], rg_b[layer], re_w[layer],
                                 re_b[layer], exp_w_gate[layer], exp_w_up[layer], exp_w_down[layer])
    return x
```

```python
import contextlib
import os
import numpy as np
import ml_dtypes
import concourse.bass as bass
import concourse.mybir as mybir
from concourse.bass_utils import run_bass_kernel_spmd

F32 = mybir.dt.float32
BF16 = mybir.dt.bfloat16
I32 = mybir.dt.int32
AF = mybir.ActivationFunctionType
ALU = mybir.AluOpType
AX = mybir.AxisListType

RSTD_LNEXP = bool(int(os.environ.get('MK_LNEXP', '1')))
SILU_EXP = bool(int(os.environ.get('MK_SILUEXP', '0')))
NO_SAME_ENGINE_SYNC = bool(int(os.environ.get('MK_NOSAME', '0')))
COMPUTE = ("pe", "act", "dve", "pool")
ALLQ = ("pe", "act", "dve", "pool", "sp")

D = 1024
MEM = 256
XH, XD = 4, 64
AH, NOPE, ROPE, QK, VD = 8, 64, 32, 96, 64
QL, KVL = 256, 128
BH, HP, DI, SG, SN, CK = 8, 64, 512, 2, 128, 4
IN_DIM = 1960
NEXP, EFF = 32, 256
EPS = 1e-6
THETA = 10000.0


class Prog:
    def __init__(self, nc, n_dma_sems=16):
        self.nc = nc
        self.stack = None
        self.q = {e: [] for e in ALLQ}
        self.dsem = [nc.alloc_semaphore("dsem%d" % i) for i in range(n_dma_sems)]
        self.dtot = [0] * n_dma_sems
        self.dq = {"sp": list(range(0, n_dma_sems // 2)), "pool": list(range(n_dma_sems // 2, n_dma_sems)),
                   "act": list(range(0, n_dma_sems // 2))}
        self.drr = {"sp": 0, "pool": 0, "act": 3}
        self.semobj = {}
        for i, s in enumerate(self.dsem):
            self.semobj["dsem%d" % i] = s
        self.known = {e: {} for e in ALLQ}
        self.last_w = {}
        self.readers = {}
        self.epoch = -1
        self.n_inst = 0
        self.n_wait = 0
        self.esem = {}
        self.ecnt = {}
        self.stream = 0
        self.hook = None
        self._new_sems()

    def _new_sems(self):
        self.epoch += 1
        for e in COMPUTE:
            nm = "sem_%s_%d" % (e, self.epoch)
            s = self.nc.alloc_semaphore(nm)
            self.esem[e] = (nm, s)
            self.semobj[nm] = s
            self.ecnt[e] = 0

    def sb(self, name, shape, dtype=F32):
        self._uid = getattr(self, "_uid", 0) + 1
        return self.stack.enter_context(self.nc.sbuf_tensor("%s_u%d" % (name, self._uid), list(shape), dtype))

    @staticmethod
    def key(x):
        if isinstance(x, (str, tuple)):
            return x
        if hasattr(x, "tensor"):
            return x.tensor.name
        return x.name

    def _wait(self, eng, tok, force=False):
        semname, val = tok
        if val <= 0:
            return
        kn = self.known[eng]
        if kn.get(semname, 0) >= val:
            return
        if eng in COMPUTE and semname == self.esem[eng][0] and not force and (eng == "pe" or NO_SAME_ENGINE_SYNC):
            return
        kn[semname] = val
        sem = self.semobj[semname]
        self.q[eng].append(lambda e, sem=sem, val=val: e.wait_ge(sem, val))
        self.n_wait += 1

    def _deps(self, eng, reads, writes):
        deps = {}
        for k in reads:
            t = self.last_w.get(self.key(k))
            if t:
                deps[t[0]] = max(deps.get(t[0], 0), t[1])
        for k in writes:
            k = self.key(k)
            t = self.last_w.get(k)
            if t:
                deps[t[0]] = max(deps.get(t[0], 0), t[1])
            for t in self.readers.get(k, ()):
                deps[t[0]] = max(deps.get(t[0], 0), t[1])
        for s, v in deps.items():
            self._wait(eng, (s, v))

    def _record(self, tok, reads, writes):
        for k in reads:
            lst = self.readers.setdefault(self.key(k), [])
            for i, t in enumerate(lst):
                if t[0] == tok[0]:
                    lst[i] = tok
                    break
            else:
                lst.append(tok)
        for k in writes:
            k = self.key(k)
            self.last_w[k] = tok
            self.readers[k] = []

    def op(self, eng, fn, reads=(), writes=()):
        self._deps(eng, reads, writes)
        self.ecnt[eng] += 1
        nm, sem = self.esem[eng]
        self.q[eng].append(lambda e, fn=fn, sem=sem: fn(e).then_inc(sem, 1))
        tok = (nm, self.ecnt[eng])
        self._record(tok, reads, writes)
        self.n_inst += 1
        if self.hook:
            self.hook()
        return tok

    def dma(self, out, in_, queue="sp", reads=None, writes=None, **kw):
        reads = [in_] if reads is None else reads
        writes = [out] if writes is None else writes
        lst = self.dq[queue]
        i = lst[self.drr[queue] % len(lst)]
        self.drr[queue] += 1
        semname = "dsem%d" % i
        self._wait(queue, (semname, self.dtot[i]))
        self._deps(queue, reads, writes)
        self.dtot[i] += 16
        sem = self.dsem[i]
        self.q[queue].append(lambda e, sem=sem: e.dma_start(out=out, in_=in_, **kw).then_inc(sem, 16))
        tok = (semname, self.dtot[i])
        self._record(tok, reads, writes)
        self.n_inst += 1
        if self.hook:
            self.hook()
        return tok

    def dma_fn(self, fn, queue, reads, writes):
        lst = self.dq[queue]
        i = lst[self.drr[queue] % len(lst)]
        self.drr[queue] += 1
        semname = "dsem%d" % i
        self._wait(queue, (semname, self.dtot[i]))
        self._deps(queue, reads, writes)
        self.dtot[i] += 16
        sem = self.dsem[i]
        self.q[queue].append(lambda e, sem=sem: fn(e).then_inc(sem, 16))
        tok = (semname, self.dtot[i])
        self._record(tok, reads, writes)
        self.n_inst += 1
        return tok

    def barrier(self, new_sems=True):
        for e in ALLQ:
            for c in COMPUTE:
                self._wait(e, (self.esem[c][0], self.ecnt[c]), force=True)
            for i in range(len(self.dsem)):
                self._wait(e, ("dsem%d" % i, self.dtot[i]))
        self.last_w = {}
        self.readers = {}
        if new_sems:
            self._new_sems()

    def mm(self, out, lhsT, rhs, start=True, stop=True, reads=None, writes=None):
        reads = [lhsT, rhs] if reads is None else reads
        writes = [out] if writes is None else writes
        return self.op("pe", lambda e: e.matmul(out, lhsT, rhs, start=start, stop=stop), reads, writes)

    def tr(self, out, in_, ident):
        return self.op("pe", lambda e: e.transpose(out, in_, ident), [in_, ident], [out])

    def actf(self, out, in_, func, bias=None, scale=None, accum_out=None):
        r = [in_]
        kw = {}
        if bias is not None:
            kw["bias"] = bias
            if not isinstance(bias, (int, float)):
                r.append(bias)
        if scale is not None:
            kw["scale"] = scale
            if not isinstance(scale, (int, float)):
                r.append(scale)
        w = [out]
        if accum_out is not None:
            kw["accum_out"] = accum_out
            w.append(accum_out)
        return self.op("act", lambda e: e.activation(out, in_, func, **kw), r, w)

    def tt(self, out, in0, in1, op, eng="dve"):
        return self.op(eng, lambda e: e.tensor_tensor(out, in0, in1, op), [in0, in1], [out])

    def ts(self, out, in0, s1, s2, op0, op1=None, eng="dve"):
        r = [in0]
        if not isinstance(s1, (int, float)):
            r.append(s1)
        if s2 is not None and not isinstance(s2, (int, float)):
            r.append(s2)
        if op1 is None:
            return self.op(eng, lambda e: e.tensor_scalar(out, in0, s1, None, op0), r, [out])
        return self.op(eng, lambda e: e.tensor_scalar(out, in0, s1, s2, op0, op1), r, [out])

    def stt(self, out, in0, scalar, in1, op0, op1, eng="dve"):
        r = [in0, in1]
        if not isinstance(scalar, (int, float)):
            r.append(scalar)
        return self.op(eng, lambda e: e.scalar_tensor_tensor(out, in0, scalar, in1, op0, op1), r, [out])

    def copy(self, out, in_, eng="dve"):
        if eng == "act":
            return self.op("act", lambda e: e.copy(out, in_), [in_], [out])
        return self.op(eng, lambda e: e.tensor_copy(out, in_), [in_], [out])

    def red(self, out, in_, op, axis=None, eng="dve"):
        axis = AX.X if axis is None else axis
        return self.op(eng, lambda e: e.tensor_reduce(out, in_, axis, op), [in_], [out])

    def recip(self, out, in_):
        return self.op("dve", lambda e: e.reciprocal(out, in_), [in_], [out])

    def memset(self, ap, val, eng="pool"):
        return self.op(eng, lambda e: e.memset(ap, val), [], [ap])

    def emit(self):
        nc = self.nc
        q = self.q
        self.q = {e: [] for e in ALLQ}
        with nc.Block() as block:
            @block.sync
            def _(e):
                for f in q["sp"]:
                    f(e)

            @block.tensor
            def _(e):
                for f in q["pe"]:
                    f(e)

            @block.scalar
            def _(e):
                for f in q["act"]:
                    f(e)

            @block.vector
            def _(e):
                for f in q["dve"]:
                    f(e)

            @block.gpsimd
            def _(e):
                for f in q["pool"]:
                    f(e)


def run_streams(P, bodies):
    import threading
    n = len(bodies)
    if n == 1:
        P.stream = 0
        bodies[0]()
        return
    sems = [threading.Semaphore(0) for _ in range(n)]
    main = threading.Semaphore(0)
    done = [False] * n
    cur = [0]
    errs = []

    def nxt(i):
        for d in range(1, n + 1):
            j = (i + d) % n
            if not done[j]:
                return j
        return None

    def hook():
        i = cur[0]
        j = nxt(i)
        if j is None or j == i:
            return
        cur[0] = j
        P.stream = j
        sems[j].release()
        sems[i].acquire()
        P.stream = i

    def runner(i):
        sems[i].acquire()
        P.stream = i
        try:
            bodies[i]()
        except BaseException as ex:
            errs.append(ex)
        done[i] = True
        j = nxt(i)
        if j is None:
            main.release()
        else:
            cur[0] = j
            P.stream = j
            sems[j].release()

    ths = [threading.Thread(target=runner, args=(i,)) for i in range(n)]
    for t in ths:
        t.start()
    P.hook = hook
    cur[0] = 0
    sems[0].release()
    main.acquire()
    P.hook = None
    P.stream = 0
    for t in ths:
        t.join()
    if errs:
        raise errs[0]


class SRot:
    def __init__(self, P, name, shape, dtype):
        self.P, self.name, self.shape, self.dtype = P, name, shape, dtype
        self.pools = []

    def setup(self, nstreams, n):
        self.pools = [Rot(self.P, "%s_s%d" % (self.name, i), self.shape, self.dtype, n) for i in range(nstreams)]

    def get(self):
        return self.pools[self.P.stream].get()


class SplitRot:
    def __init__(self, pools, P):
        self.pools, self.P = pools, P

    def get(self):
        return self.pools[min(self.P.stream, len(self.pools) - 1)].get()


class Rot:
    def __init__(self, P, name, shape, dtype, n):
        self.t = [P.sb("%s_%d" % (name, i), shape, dtype) for i in range(n)]
        self.i = 0

    def get(self):
        t = self.t[self.i % len(self.t)]
        self.i += 1
        return t


def bc_mid(ap2d, n):
    p, f = ap2d.shape
    return ap2d.unsqueeze(1).to_broadcast([p, n, f])


def bc_last(ap2d, n):
    p, h = ap2d.shape
    return ap2d.unsqueeze(2).to_broadcast([p, h, n])


def build_program(NS, S, dbg=False, stages=None):
    NT = S // 128
    NG = S // 512
    NTT = NS * NT
    nc = bass.Bass("TRN2", target_bir_lowering=False)

    def din(name, shape, dt=F32):
        return nc.dram_tensor(name, list(shape), dt, kind="ExternalInput").ap()

    def dscr(name, shape, dt=F32):
        kind = "ExternalOutput" if dbg else "Internal"
        return nc.dram_tensor(name, list(shape), dt, kind=kind).ap()

    x_d = din("x", [NS, S, D])
    mem_d = din("mem", [NS, MEM, D])
    posT_d = din("posT", [128, NTT], I32)
    ln_mix = din("ln_mix", [2, D]); w_in = din("w_in", [1, D, IN_DIM])
    q_lat_norm = din("q_lat_norm", [1, QL]); w_uq = din("w_uq", [1, QL, AH * QK])
    kv_lat_norm = din("kv_lat_norm", [1, KVL]); w_ukv = din("w_ukv", [1, KVL, AH * (NOPE + VD)])
    q_norm = din("q_norm", [1, QK]); k_norm = din("k_norm", [1, QK])
    conv_wl = din("conv_wl", [128, 8, CK]); conv_bl = din("conv_bl", [128, 8])
    dt_bias = din("dt_bias", [1, BH]); a_log = din("a_log", [1, BH]); d_skip = din("d_skip", [1, BH])
    ssd_norm = din("ssd_norm", [1, DI]); w_out = din("w_out", [1, D, D])
    pool_w = din("pool_w", [1, 4, 256, 256]); pool_b = din("pool_b", [1, D]); pool_scale = din("pool_scale", [1, D])
    ln_xq = din("ln_xq", [2, D]); ln_mem = din("ln_mem", [2, D])
    xq_w = din("xq_w", [2, D, XH * XD]); xkv_w = din("xkv_w", [2, D, 2 * XH * XD])
    xq_norm = din("xq_norm", [2, XD]); xk_norm = din("xk_norm", [2, XD]); xo_w = din("xo_w", [2, XH * XD, D])
    ln_ffn = din("ln_ffn", [2, D]); rg_w = din("rg_w", [2, D, 4]); rg_b = din("rg_b", [2, 4])
    re_w = din("re_w", [2, D, NEXP]); re_b = din("re_b", [2, NEXP])
    c_ident = din("c_ident", [128, 128]); c_ut = din("c_ut", [128, 128]); c_ls = din("c_ls", [128, 128])
    c_invf = din("c_invf", [ROPE // 2]); c_icnt = din("c_icnt", [4, 512])
    NTOK = NS * S
    BLK = 512
    NBLK = (2 * NTOK) // BLK + NEXP
    NROWS = NBLK * BLK
    NTI = NTOK // 128
    c_uts = din("c_uts", [128, 128]); c_thr = din("c_thr", [16]); c_jidx = din("c_jidx", [NBLK]); c_iota = din("c_iota", [128, 1])
    ewg_l = [din("ewg_l%d" % i, [NEXP * 128, 2048]) for i in range(2)]
    ewu_l = [din("ewu_l%d" % i, [NEXP * 128, 2048]) for i in range(2)]
    ewd_l = [din("ewd_l%d" % i, [NEXP * 128, 2048]) for i in range(2)]
    Hn_d = nc.dram_tensor("Hn_s", [NTOK, D], BF16, kind="Internal").ap()
    Xs_d = nc.dram_tensor("Xs_s", [NROWS, D], BF16, kind="Internal").ap()
    Ys_d = nc.dram_tensor("Ys_s", [NROWS, D], BF16, kind="Internal").ap()

    out_d = nc.dram_tensor("out", [NS, S, D], F32, kind="ExternalOutput").ap()
    qT_d = dscr("qT_s", [NS, AH, QK, S], BF16)
    kT_d = dscr("kT_s", [NS, AH, QK, S], BF16)
    v_d = dscr("v_s", [NS, S, AH * VD], BF16)
    yT_d = dscr("yT_s", [NS, 4, 128, S], BF16)
    aT_d = dscr("aT_s", [NS, AH * VD, S], BF16)
    dbg_out = {}
    if dbg:
        for nm in ("mix0", "xa0", "moe0", "mix1", "xa1"):
            dbg_out[nm] = nc.dram_tensor("dbg_" + nm, [NS, S, D], F32, kind="ExternalOutput").ap()

    def okey(b, tile):
        return ("out", b, tile)

    with contextlib.ExitStack() as gst:
        P = Prog(nc)
        P.stack = gst
        banks = [gst.enter_context(nc.psum_tensor("pb%d" % i, [128, 512], F32)) for i in range(8)]
        bank_cfg = {"sets": [list(range(8))], "acc": [[6, 7]]}
        bank_i = {}
        acc_i = {}

        def set_banks(sets, acc):
            bank_cfg["sets"] = sets
            bank_cfg["acc"] = acc

        def bank():
            sidx = P.stream if P.stream < len(bank_cfg["sets"]) else 0
            ids = bank_cfg["sets"][sidx]
            k = bank_i.get(sidx, 0)
            bank_i[sidx] = k + 1
            return banks[ids[k % len(ids)]]

        def accbank():
            sidx = P.stream if P.stream < len(bank_cfg["acc"]) else 0
            ids = bank_cfg["acc"][sidx]
            k = acc_i.get(sidx, 0)
            acc_i[sidx] = k + 1
            return banks[ids[k % len(ids)]]

        identf = P.sb("identf", [128, 128]); identb = P.sb("identb", [128, 128], BF16)
        utf = P.sb("utf", [128, 128]); utb = P.sb("utb", [128, 128], BF16)
        lsf = P.sb("lsf", [128, 128]); onesf = P.sb("onesf", [128, 128])
        epsb = P.sb("epsb", [128, 1])
        cosT = P.sb("cosT", [128, NTT, 16]); sinT = P.sb("sinT", [128, NTT, 16])
        P.dma(identf[:], c_ident); P.dma(utf[:], c_ut); P.dma(lsf[:], c_ls)
        P.copy(identb[:], identf[:]); P.copy(utb[:], utf[:])
        P.memset(onesf[:], 1.0); P.memset(epsb[:], EPS)

        small = SRot(P, "small", [128, 128], F32)
        junk = SRot(P, "junk", [128, 1024], F32)
        junkb = SRot(P, "junkb", [128, 1024], BF16)

        def stage_pools(nstreams=1, nsmall=28, need_junk=False):
            small.setup(nstreams, nsmall)
            junkb.setup(nstreams, 1)
            if need_junk:
                junk.setup(nstreams, 1)

        def rstd_from_ss(ss, n, inv_d):
            r = small.get()
            r2 = small.get()
            if RSTD_LNEXP:
                P.actf(r[:, 0:n], ss, AF.Ln, bias=epsb[:, 0:1], scale=inv_d)
                P.actf(r2[:, 0:n], r[:, 0:n], AF.Exp, scale=-0.5)
            else:
                P.actf(r[:, 0:n], ss, AF.Sqrt, bias=epsb[:, 0:1], scale=inv_d)
                P.recip(r2[:, 0:n], r[:, 0:n])
            return r2[:, 0:n]

        def silu_exp(out_ap, x_ap, tmp_pool, n):
            e = tmp_pool.get()
            P.actf(e[:, 0:n], x_ap, AF.Exp, scale=-1.0)
            P.ts(e[:, 0:n], e[:, 0:n], 1.0, None, ALU.add)
            P.recip(e[:, 0:n], e[:, 0:n])
            P.tt(out_ap, x_ap, e[:, 0:n], ALU.mult)

        def rmsnorm_full(x_ap, dd, gain_bc, out_ap):
            ss = small.get()
            j = junkb.get()
            P.actf(j[:, 0:dd], x_ap, AF.Square, accum_out=ss[:, 0:1])
            r = rstd_from_ss(ss[:, 0:1], 1, 1.0 / dd)
            P.stt(out_ap, x_ap, r[:, 0:1], gain_bc, ALU.mult, ALU.mult)

        def transpose_to(in_ap, nchunk, csz, out_ap, dt=BF16, evac="dve"):
            done = 0
            per = (1024 if dt == BF16 else 512) // 128
            while done < nchunk:
                n = min(per, nchunk - done)
                bk = bank()
                bv = bk[:].bitcast(BF16) if dt == BF16 else bk[:]
                idn = identb if dt == BF16 else identf
                for c in range(n):
                    P.tr(bv[0:csz, c * 128:(c + 1) * 128], in_ap[:, (done + c) * csz:(done + c + 1) * csz], idn[:])
                src = bv[0:csz, 0:n * 128].rearrange("p (a c) -> p a c", a=n)
                P.copy(out_ap[:, done:done + n, :], src, eng=evac)
                done += n

        def load_bc(name, vec_ap, n, eng_q="sp"):
            t = P.sb(name, [128, n])
            P.dma(t[:], vec_ap.partition_broadcast(128), queue=eng_q)
            return t

        tmpst = contextlib.ExitStack()
        P.stack = tmpst
        posi = P.sb("posi", [128, NTT], I32); posf = P.sb("posf", [128, NTT])
        invf = load_bc("invf", c_invf, 16)
        P.dma(posi[:], posT_d)
        P.copy(posf[:], posi[:])
        ang = P.sb("ang", [128, NTT, 16]); a1 = P.sb("ang1", [128, NTT, 16]); ki = P.sb("angk", [128, NTT, 16], I32)
        kf = P.sb("angkf", [128, NTT, 16])
        P.tt(ang[:], bc_last(posf[:], 16), bc_mid(invf[:], NTT), ALU.mult)
        TWO_PI = float(2 * np.pi)

        def sin_of(dst, shift):
            P.ts(a1[:], ang[:], shift, 1.0 / TWO_PI, ALU.add, ALU.mult)
            P.copy(ki[:], a1[:])
            P.copy(kf[:], ki[:])
            P.ts(a1[:], ang[:], shift, None, ALU.add)
            P.stt(a1[:], kf[:], -TWO_PI, a1[:], ALU.mult, ALU.add)
            P.ts(kf[:], a1[:], float(np.pi), -TWO_PI, ALU.is_gt, ALU.mult)
            P.tt(a1[:], a1[:], kf[:], ALU.add)
            P.ts(kf[:], a1[:], float(-np.pi), TWO_PI, ALU.is_lt, ALU.mult)
            P.tt(a1[:], a1[:], kf[:], ALU.add)
            P.actf(dst, a1[:], AF.Sin)

        sin_of(sinT[:], 0.0)
        sin_of(cosT[:], float(np.pi / 2))
        P.barrier(new_sems=False)
        P.emit()
        tmpst.close()
        P.stack = gst

        def rope(u_ap, tile_idx, o1, o2, nh):
            u1 = u_ap[:, :, 0:16]; u2 = u_ap[:, :, 16:32]
            cb = bc_mid(cosT[:, tile_idx, :], nh); sb_ = bc_mid(sinT[:, tile_idx, :], nh)
            ta = small.get(); tb = small.get()
            tav = ta[:, 0:nh * 16].rearrange("p (h f) -> p h f", h=nh)
            tbv = tb[:, 0:nh * 16].rearrange("p (h f) -> p h f", h=nh)
            P.tt(tav, u1, cb, ALU.mult); P.tt(tbv, u2, sb_, ALU.mult)
            P.tt(o1, tav, tbv, ALU.subtract)
            tc_ = small.get(); td = small.get()
            tcv = tc_[:, 0:nh * 16].rearrange("p (h f) -> p h f", h=nh)
            tdv = td[:, 0:nh * 16].rearrange("p (h f) -> p h f", h=nh)
            P.tt(tcv, u2, cb, ALU.mult); P.tt(tdv, u1, sb_, ALU.mult)
            P.tt(o2, tcv, tdv, ALU.add)

        def softmax_finalize(ps_o, ncols, out_bf, P_osb, P_rl):
            osb = P_osb.get()
            P.copy(osb[0:65, 0:ncols], ps_o[0:65, 0:ncols], eng="act")
            rl = P_rl.get()
            P.recip(rl[64:65, 0:ncols], osb[64:65, 0:ncols])
            pb = bank()
            P.mm(pb[0:64, 0:ncols], onesf[64:65, 0:64], rl[64:65, 0:ncols])
            P.tt(out_bf, osb[0:64, 0:ncols], pb[0:64, 0:ncols], ALU.mult)


        def dbg_dump(name):
            if not dbg:
                return
            P.barrier(new_sems=False)
            for b in range(NS):
                P.dma(dbg_out[name][b], out_d[b], reads=["out_all"], writes=["dbg_" + name])
            P.barrier(new_sems=False)

        dumped = set()

        def dump(name, ap, dt=F32):
            if not dbg or name in dumped:
                return
            dumped.add(name)
            d = nc.dram_tensor("dmp_" + name, list(ap.shape), dt, kind="ExternalOutput").ap()
            P.dma(d, ap, queue="sp")

        def stage_end():
            P.barrier()
            P.emit()

        run = (lambda s: True) if stages is None else (lambda s: s in stages)

        if run("A"):
          with contextlib.ExitStack() as st:
            P.stack = st
            stage_pools(3, 9, need_junk=True)
            set_banks([[0, 1, 2], [3, 4], [5, 6, 7]], [[6, 7]])
            w_in_sb = P.sb("w_in_sb", [128, 8, IN_DIM], BF16)
            w_in_v = w_in[0].rearrange("(kc p) f -> p kc f", p=128)
            P.dma(w_in_sb[:, :, 0:416], w_in_v[:, :, 0:416], queue="pool")
            P.dma(w_in_sb[:, :, 416:424], w_in_v[:, :, 1952:1960], queue="pool")
            P.dma(w_in_sb[:, :, 424:1960], w_in_v[:, :, 416:1952], queue="pool")
            w_uq_sb = P.sb("w_uq_sb", [128, 2, AH * QK], BF16)
            P.dma(w_uq_sb[:], w_uq[0].rearrange("(kc p) f -> p kc f", p=128), queue="pool")
            w_ukv_sb = P.sb("w_ukv_sb", [128, 1024], BF16)
            P.dma(w_ukv_sb[:], w_ukv[0], queue="pool")
            g_mix = load_bc("g_mix", ln_mix[0], D)
            g_ql = load_bc("g_ql", q_lat_norm[0], QL); g_kvl = load_bc("g_kvl", kv_lat_norm[0], KVL)
            g_q = load_bc("g_q", q_norm[0], QK); g_k = load_bc("g_k", k_norm[0], QK)
            dtb = load_bc("dtb", dt_bias[0], BH); alog = load_bc("alog", a_log[0], BH)
            dsk8 = load_bc("dsk8", d_skip[0], BH); g_ssd = load_bc("g_ssd", ssd_norm[0], DI)
            abc = P.sb("abc", [128, BH])
            P.actf(abc[:], alog[:], AF.Exp)
            P.ts(abc[:], abc[:], -1.0, None, ALU.mult)
            cw = P.sb("cw", [128, 8, CK]); cb_ = P.sb("cb", [128, 8])
            P.dma(cw[:], conv_wl); P.dma(cb_[:], conv_bl)

            xt_r = Rot(P, "xt", [128, D], F32, 2)
            hb_r = Rot(P, "hb", [128, D], BF16, 1)
            hT_r = Rot(P, "hT", [128, 8, 512], BF16, 1)
            U = [P.sb("U%d" % c, [128, 515], F32) for c in range(8)]
            cacc_r = Rot(P, "cacc", [128, 512], F32, 1)
            xbcT_r = [Rot(P, "xbcT%d" % c, [128, 512], BF16, 1) for c in range(8)]
            qT_g_r = Rot(P, "qT_g", [96, AH, 512], BF16, 1)
            kT_g_r = Rot(P, "kT_g", [96, AH, 512], BF16, 1)
            v_g_r = Rot(P, "v_g", [128, 4, AH * VD], BF16, 1)
            yT_g_r = Rot(P, "yT_g", [128, 4, 512], BF16, 1)
            Sf = P.sb("Sf", [128, 512], F32); Sb = P.sb("Sb", [128, 512], BF16)
            f512 = SplitRot([Rot(P, "f512q", [128, 512], F32, 1), Rot(P, "f512kv", [128, 512], F32, 2), Rot(P, "f512s", [128, 512], F32, 3)], P)
            pa_r = Rot(P, "pa", [128, 424], F32, 2)
            se_r = Rot(P, "se", [128, 512], F32, 2) if SILU_EXP else None
            zs_r = Rot(P, "zs", [128, 512], F32, 2)
            b512 = SplitRot([Rot(P, "b512q", [128, 512], BF16, 2), Rot(P, "b512kv", [128, 512], BF16, 2), Rot(P, "b512s", [128, 512], BF16, 5)], P)
            f768 = Rot(P, "f768", [128, 768], F32, 2)
            f1k = Rot(P, "f1k", [128, 1024], F32, 1)
            b768 = SplitRot([Rot(P, "b768q", [128, 768], BF16, 1), Rot(P, "b768kv", [128, 768], BF16, 1)], P)
            lhs_r = Rot(P, "lhsh", [128, 128], F32, 4)
            dec_r = Rot(P, "dec", [128, 4, 128], F32, 1)
            MT_r = Rot(P, "MT", [128, 8, 128], BF16, 2)
            ps_keep = {}

            ASTOP = int(os.environ.get("A_STOP", "99"))

            class _Stop(Exception):
                pass

            def stop_if(k):
                if ASTOP == k:
                    raise _Stop()

            try:
              for b in range(NS):
                  for c in range(8):
                      P.memset(U[c][:, 0:3], 0.0)
                  P.memset(Sf[:], 0.0); P.memset(Sb[:], 0.0)
                  for g in range(NG):
                      hT = hT_r.get()
                      qT_g = qT_g_r.get(); kT_g = kT_g_r.get(); v_g = v_g_r.get(); yT_g = yT_g_r.get()
                      for t in range(4):
                          tile = g * 4 + t
                          xt = xt_r.get()
                          P.dma(xt[:], x_d[b, tile * 128:(tile + 1) * 128, :])
                          hb = hb_r.get()
                          rmsnorm_full(xt[:], D, g_mix[:], hb[:])
                          transpose_to(hb[:], 8, 128, hT[:, :, t * 128:(t + 1) * 128])
                      xbcT = []
                      for fc in range(8):
                          bk = bank()
                          for kc in range(8):
                              P.mm(bk[:, :], w_in_sb[:, kc, 936 + fc * 128:936 + (fc + 1) * 128], hT[:, kc, :],
                                   start=(kc == 0), stop=(kc == 7))
                          P.copy(U[fc][:, 3:515], bk[:, :], eng="act")
                          ca = cacc_r.get()
                          P.ts(ca[:], U[fc][:, 3:515], cw[:, fc, 3:4], None, ALU.mult)
                          for k in (2, 1, 0):
                              P.stt(ca[:], U[fc][:, k:k + 512], cw[:, fc, k:k + 1], ca[:], ALU.mult, ALU.add)
                          P.copy(U[fc][:, 0:3], U[fc][:, 512:515], eng="pool")
                          xo = xbcT_r[fc].get()
                          if SILU_EXP:
                              P.ts(ca[:], ca[:], cb_[:, fc:fc + 1], None, ALU.add)
                              silu_exp(xo[:], ca[:], se_r, 512)
                          else:
                              P.actf(xo[:], ca[:], AF.Silu, bias=cb_[:, fc:fc + 1])
                          xbcT.append(xo)
                          dump("xbcT%d" % fc, xo[:], BF16)
                      for t in range(4):
                          tile = g * 4 + t
                          gt = b * NT + tile
                          tsl = slice(t * 128, (t + 1) * 128)
                          ps_a = bank()
                          for kc in range(8):
                              P.mm(ps_a[:, 0:424], hT[:, kc, tsl], w_in_sb[:, kc, 0:424], start=(kc == 0), stop=(kc == 7))
                          ps_z = bank()
                          for kc in range(8):
                              P.mm(ps_z[:, :], hT[:, kc, tsl], w_in_sb[:, kc, 424:936], start=(kc == 0), stop=(kc == 7))
                          pa = pa_r.get()
                          P.copy(pa[:, 0:424], ps_a[:, 0:424], eng="act")
                          zs = zs_r.get()
                          if SILU_EXP:
                              silu_exp(zs[:], ps_z[:, :], se_r, 512)
                          else:
                              P.actf(zs[:], ps_z[:, :], AF.Silu)
                          def q_path():
                              qlb = b512.get()
                              rmsnorm_full(pa[:, 0:256], QL, g_ql[:], qlb[:, 0:256])
                              qlT = b512.get()
                              transpose_to(qlb[:, 0:256], 2, 128, qlT[:, 0:256].rearrange("p (a c) -> p a c", a=2))
                              q_sb = f768.get()
                              bq0 = bank(); bq1 = bank()
                              for kc in range(2):
                                  P.mm(bq0[:, :], qlT[:, kc * 128:(kc + 1) * 128], w_uq_sb[:, kc, 0:512], start=(kc == 0), stop=(kc == 1))
                              for kc in range(2):
                                  P.mm(bq1[:, 0:256], qlT[:, kc * 128:(kc + 1) * 128], w_uq_sb[:, kc, 512:768], start=(kc == 0), stop=(kc == 1))
                              P.copy(q_sb[:, 0:512], bq0[:, :], eng="act")
                              P.copy(q_sb[:, 512:768], bq1[:, 0:256], eng="act")
                              sq = junk.get()
                              P.actf(sq[:, 0:768], q_sb[:], AF.Square)
                              ssq = small.get()
                              P.red(ssq[:, 0:8], sq[:, 0:768].rearrange("p (h f) -> p h f", h=8), ALU.add)
                              rq = rstd_from_ss(ssq[:, 0:8], 8, 1.0 / QK)
                              qn = f768.get()
                              q3 = q_sb[:].rearrange("p (h f) -> p h f", h=8)
                              qn3 = qn[:].rearrange("p (h f) -> p h f", h=8)
                              P.tt(qn3, q3, bc_last(rq, QK), ALU.mult)
                              P.tt(qn3, qn3, bc_mid(g_q[:], 8), ALU.mult)
                              qf = b768.get()
                              qf3 = qf[:].rearrange("p (h f) -> p h f", h=8)
                              P.copy(qf3[:, :, 0:64], qn3[:, :, 0:64], eng=os.environ.get("MK_CAST", "act"))
                              rope(qn3[:, :, 64:96], gt, qf3[:, :, 64:80], qf3[:, :, 80:96], 8)
                              transpose_to(qf[:], 8, 96, qT_g[:, :, tsl])

                          def kv_path():
                              kvb = b512.get()
                              rmsnorm_full(pa[:, 256:384], KVL, g_kvl[:], kvb[:, 0:128])
                              kvT = b512.get()
                              transpose_to(kvb[:, 0:128], 1, 128, kvT[:, 0:128].rearrange("p (a c) -> p a c", a=1))
                              kv_sb = f1k.get()
                              for hf in range(2):
                                  bkv = bank()
                                  P.mm(bkv[:, :], kvT[:, 0:128], w_ukv_sb[:, hf * 512:(hf + 1) * 512])
                                  P.copy(kv_sb[:, hf * 512:(hf + 1) * 512], bkv[:, :], eng="act")
                              kv3 = kv_sb[:].rearrange("p (h f) -> p h f", h=8)
                              sqk = junk.get()
                              P.actf(sqk[:], kv_sb[:], AF.Square)
                              ssk = small.get()
                              P.red(ssk[:, 0:8], sqk[:].rearrange("p (h f) -> p h f", h=8)[:, :, 0:64], ALU.add)
                              ssr = small.get()
                              jr = small.get()
                              P.actf(jr[:, 0:32], pa[:, 384:416], AF.Square, accum_out=ssr[:, 0:1])
                              P.ts(ssk[:, 0:8], ssk[:, 0:8], ssr[:, 0:1], None, ALU.add)
                              rk = rstd_from_ss(ssk[:, 0:8], 8, 1.0 / QK)
                              kf_ = b768.get()
                              kf3 = kf_[:].rearrange("p (h f) -> p h f", h=8)
                              kn = f512.get()
                              kn3 = kn[:].rearrange("p (h f) -> p h f", h=8)
                              P.tt(kn3, kv3[:, :, 0:64], bc_last(rk, 64), ALU.mult)
                              P.tt(kf3[:, :, 0:64], kn3, bc_mid(g_k[:, 0:64], 8), ALU.mult)
                              krg = small.get()
                              P.tt(krg[:, 0:32], pa[:, 384:416], g_k[:, 64:96], ALU.mult)
                              kr = f512.get()
                              kr3 = kr[:, 0:256].rearrange("p (h f) -> p h f", h=8)
                              P.tt(kr3, bc_mid(krg[:, 0:32], 8), bc_last(rk, 32), ALU.mult)
                              rope(kr3, gt, kf3[:, :, 64:80], kf3[:, :, 80:96], 8)
                              transpose_to(kf_[:], 8, 96, kT_g[:, :, tsl])
                              P.copy(v_g[:, t, :].rearrange("p (h f) -> p h f", h=8), kv3[:, :, 64:128], eng=os.environ.get("MK_CAST", "act"))

                          def ssd_path():
                              xs_tm = b512.get(); B_tm = b512.get()
                              bk = bank(); bv = bk[:].bitcast(BF16)
                              for c in range(4):
                                  P.tr(bv[:, c * 128:(c + 1) * 128], xbcT[c][:, tsl], identb[:])
                              for c in range(2):
                                  P.tr(bv[:, 512 + c * 128:512 + (c + 1) * 128], xbcT[4 + c][:, tsl], identb[:])
                              P.copy(xs_tm[:], bv[:, 0:512])
                              P.copy(B_tm[:, 0:256], bv[:, 512:768])
                              dtr = small.get(); dte_ = small.get(); dtv = small.get(); adt = small.get()
                              P.tt(dtr[:, 0:8], pa[:, 416:424], dtb[:], ALU.add)
                              P.actf(dte_[:, 0:8], dtr[:, 0:8], AF.Exp)
                              P.actf(dtv[:, 0:8], dte_[:, 0:8], AF.Ln, bias=1.0)
                              P.tt(adt[:, 0:8], dtv[:, 0:8], abc[:], ALU.mult)
                              dump("dtv_%d" % t, dtv[:, 0:8]); dump("xs_tm_%d" % t, xs_tm[:], BF16)
                              ps_c = bank()
                              P.mm(ps_c[:, 0:8], utf[:], adt[:, 0:8])
                              P.mm(ps_c[:, 8:16], onesf[:], adt[:, 0:8])
                              ct = small.get()
                              P.copy(ct[:, 0:16], ps_c[:, 0:16])
                              acs = ct[:, 0:8]; tot = ct[:, 8:16]
                              MT = MT_r.get()
                              ps_cb = bank()
                              for gr in range(2):
                                  P.mm(ps_cb[:, gr * 128:(gr + 1) * 128], xbcT[4 + gr][:, tsl], xbcT[6 + gr][:, tsl])
                              cbm = f512.get()
                              cbm3 = cbm[:, 0:256].rearrange("p (g t) -> p g t", g=2)
                              P.tt(cbm3, ps_cb[:, 0:256].rearrange("p (g t) -> p g t", g=2), bc_mid(utf[:], 2), ALU.mult)
                              for gr in range(2):
                                  ps_d = bank()
                                  for hh in range(4):
                                      h = gr * 4 + hh
                                      lh = lhs_r.get()
                                      P.ts(lh[:], lsf[:], adt[:, h:h + 1], None, ALU.mult, eng=os.environ.get("MK_LH", "dve"))
                                      P.mm(ps_d[:, hh * 128:(hh + 1) * 128], lh[:], utf[:])
                                  dec = dec_r.get()
                                  P.actf(dec[:].rearrange("p a b -> p (a b)"), ps_d[:, :], AF.Exp)
                                  P.tt(MT[:, gr * 4:(gr + 1) * 4, :], dec[:], bc_mid(cbm3[:, gr, :], 4), ALU.mult)
                              xdt = b512.get()
                              P.tt(xdt[:].rearrange("p (h f) -> p h f", h=8), xs_tm[:].rearrange("p (h f) -> p h f", h=8),
                                   bc_last(dtv[:, 0:8], 64), ALU.mult)
                              e3 = small.get()
                              P.tt(e3[:, 0:8], tot, acs, ALU.subtract)
                              P.actf(e3[:, 0:8], e3[:, 0:8], AF.Exp)
                              P.actf(e3[:, 8:16], acs, AF.Exp)
                              P.actf(e3[:, 16:24], tot, AF.Exp)
                              xdte = b512.get()
                              P.tt(xdte[:].rearrange("p (h f) -> p h f", h=8), xdt[:].rearrange("p (h f) -> p h f", h=8),
                                   bc_last(e3[:, 0:8], 64), ALU.mult, eng="pool")
                              ps_yo = bank()
                              for gr in range(2):
                                  P.mm(ps_yo[:, gr * 256:(gr + 1) * 256], xbcT[6 + gr][:, tsl], Sb[:, gr * 256:(gr + 1) * 256])
                              ps_yd = bank()
                              for h in range(8):
                                  P.mm(ps_yd[:, h * 64:(h + 1) * 64], MT[:, h, :], xdt[:, h * 64:(h + 1) * 64])
                              y1 = f512.get()
                              P.tt(y1[:].rearrange("p (h f) -> p h f", h=8), ps_yo[:, :].rearrange("p (h f) -> p h f", h=8),
                                   bc_last(e3[:, 8:16], 64), ALU.mult)
                              P.tt(y1[:], y1[:], ps_yd[:, :], ALU.add)
                              y2 = f512.get()
                              P.tt(y2[:].rearrange("p (h f) -> p h f", h=8), xs_tm[:].rearrange("p (h f) -> p h f", h=8),
                                   bc_last(dsk8[:], 64), ALU.mult, eng="pool")
                              P.tt(y1[:], y1[:], y2[:], ALU.add)
                              dump("ydiag_%d" % t, ps_yd[:, :]) if False else None
                              dump("y1_%d" % t, y1[:]); dump("acs_%d" % t, ct[:, 0:16]); dump("MT_%d" % t, MT[:], BF16)
                              ps_s = bank()
                              for gr in range(2):
                                  P.mm(ps_s[:, gr * 256:(gr + 1) * 256], B_tm[:, gr * 128:(gr + 1) * 128], xdte[:, gr * 256:(gr + 1) * 256])
                              P.tt(Sf[:].rearrange("p (h f) -> p h f", h=8), Sf[:].rearrange("p (h f) -> p h f", h=8),
                                   bc_last(e3[:, 16:24], 64), ALU.mult)
                              P.tt(Sf[:], Sf[:], ps_s[:, :], ALU.add)
                              P.copy(Sb[:], Sf[:], eng="act")
                              P.tt(y1[:], y1[:], zs[:], ALU.mult)
                              ynb = b512.get()
                              rmsnorm_full(y1[:], DI, g_ssd[:], ynb[:])
                              transpose_to(ynb[:], 4, 128, yT_g[:, :, tsl])

                          run_streams(P, [q_path, kv_path, ssd_path])
                      gs = slice(g * 512, (g + 1) * 512)
                      P.dma(qT_d[b, :, :, gs].rearrange("h d s -> d h s"), qT_g[:], queue="pool", writes=[("qT", b)])
                      P.dma(kT_d[b, :, :, gs].rearrange("h d s -> d h s"), kT_g[:], queue="pool", writes=[("kT", b)])
                      P.dma(v_d[b, gs, :].rearrange("(t p) f -> p t f", p=128), v_g[:], queue="pool", writes=[("v", b)])
                      P.dma(yT_d[b, :, :, gs].rearrange("c p s -> p c s"), yT_g[:], queue="pool", writes=[("yT", b)])
            except _Stop:
                pass
            set_banks([list(range(8))], [[6, 7]])
            stage_end()

        if run("B"):
          with contextlib.ExitStack() as st:
            P.stack = st
            def body(b):
                kT_r = Rot(P, "kTh", [96, S], BF16, 2); qT_r = Rot(P, "qTh", [96, S], BF16, 2)
                Vx_r = Rot(P, "Vx", [128, NT, 65], BF16, 2)
                for t_ in Vx_r.t:
                    P.memset(t_[:, :, 64:65], 1.0)
                pt_r = Rot(P, "pt", [128, 512], BF16, 4)
                at_r = Rot(P, "at", [64, 512], BF16, 2)
                osb_r = Rot(P, "osb", [128, 512], F32, 2); rl_r = Rot(P, "rl", [128, 512], F32, 2)
                scale = float(QK ** -0.5)
                if True:
                    for h in range(AH):
                        kT = kT_r.get(); qT = qT_r.get(); Vx = Vx_r.get()
                        P.dma(kT[:], kT_d[b, h], reads=[("kT", b)])
                        P.dma(qT[:], qT_d[b, h], reads=[("qT", b)])
                        P.dma(Vx[:, :, 0:64], v_d[b, :, h * 64:(h + 1) * 64].rearrange("(n p) d -> p n d", p=128),
                              reads=[("v", b)])
                        for qg in range(NG):
                            ps_o = accbank()
                            nkb = 4 * qg + 4
                            SKEW = 2
                            pend = {}
                            for kk in range(nkb + SKEW):
                                if kk < nkb:
                                    kb = kk
                                    i = kb - 4 * qg
                                    q0 = 128 * i if i > 0 else 0
                                    ps_s = bank()
                                    P.mm(ps_s[:, q0:512], kT[:, kb * 128:(kb + 1) * 128], qT[:, qg * 512 + q0:(qg + 1) * 512])
                                    pend[kb] = (ps_s, q0, i)
                                kb = kk - SKEW
                                if kb >= 0:
                                    ps_s, q0, i = pend.pop(kb)
                                    pt = pt_r.get()
                                    P.actf(pt[:, q0:512], ps_s[:, q0:512], AF.Exp, scale=scale)
                                    if q0 > 0:
                                        P.memset(pt[:, 0:q0], 0.0)
                                    if i >= 0:
                                        P.tt(pt[:, q0:q0 + 128], pt[:, q0:q0 + 128], utb[:], ALU.mult, eng="pool")
                                    P.mm(ps_o[0:65, :], Vx[:, kb, :], pt[:, :], start=(kb == 0), stop=(kb == nkb - 1))
                            at = at_r.get()
                            softmax_finalize(ps_o, 512, at[:], osb_r, rl_r)
                            P.dma(aT_d[b, h * 64:(h + 1) * 64, qg * 512:(qg + 1) * 512], at[:], queue="pool",
                                  writes=[("aT", b)])

            stage_pools(NS)
            set_banks([[0, 1, 2], [4, 5, 6]] if NS > 1 else [list(range(6))], [[3], [7]] if NS > 1 else [[6, 7]])
            run_streams(P, [(lambda b=b: body(b)) for b in range(NS)])
            set_banks([list(range(8))], [[6, 7]])
            stage_end()

        if run("C"):
          with contextlib.ExitStack() as st:
            P.stack = st
            w_out_sb = P.sb("w_out_sb", [128, 8, D], BF16)
            P.dma(w_out_sb[:], w_out[0].rearrange("(kc p) f -> p kc f", p=128), queue="pool")
            def body(b):
                mixT_r = Rot(P, "mixT", [128, 8, 512], BF16, 2)
                xt_r = Rot(P, "xtc", [128, D], F32, 3)
                if True:
                    for g in range(NG):
                        mixT = mixT_r.get()
                        gs = slice(g * 512, (g + 1) * 512)
                        P.dma(mixT[:, 0:4, :], aT_d[b, :, gs].rearrange("(c p) s -> p c s", p=128), reads=[("aT", b)])
                        P.dma(mixT[:, 4:8, :], yT_d[b, :, :, gs].rearrange("c p s -> p c s"), reads=[("yT", b)])
                        for t in range(4):
                            tile = g * 4 + t
                            xt = xt_r.get()
                            P.dma(xt[:], x_d[b, tile * 128:(tile + 1) * 128, :])
                            for hf in range(2):
                                ps = bank()
                                for c in range(8):
                                    P.mm(ps[:, :], mixT[:, c, t * 128:(t + 1) * 128], w_out_sb[:, c, hf * 512:(hf + 1) * 512],
                                         start=(c == 0), stop=(c == 7))
                                P.tt(xt[:, hf * 512:(hf + 1) * 512], xt[:, hf * 512:(hf + 1) * 512], ps[:, :], ALU.add)
                            P.dma(out_d[b, tile * 128:(tile + 1) * 128, :], xt[:], queue="pool", writes=[okey(b, tile)])
            stage_pools(NS)
            set_banks([[0, 1, 2, 3], [4, 5, 6, 7]] if NS > 1 else [list(range(8))], [[6, 7]])
            run_streams(P, [(lambda b=b: body(b)) for b in range(NS)])
            set_banks([list(range(8))], [[6, 7]])
            dbg_dump("mix0")
            stage_end()

        def stage_xa(l):
          with contextlib.ExitStack() as st:
            P.stack = st
            ztile = P.sb("ztile", [128, D], BF16)
            P.memset(ztile[:], 0.0)
            zch = 2048 if NROWS % 2048 == 0 else 512
            for c in range(NROWS // zch):
                P.dma(Xs_d[c * zch:(c + 1) * zch, :].rearrange("(n p) d -> p n d", p=128),
                      ztile[:].unsqueeze(1).to_broadcast([128, zch // 128, D]), queue="act", writes=[("Xsz", c)])
            xq_sb = P.sb("xq_sb", [128, 8, XH * XD], BF16)
            P.dma(xq_sb[:], xq_w[l].rearrange("(kc p) f -> p kc f", p=128), queue="pool")
            xkv_sb = P.sb("xkv_sb", [128, 8, 2 * XH * XD], BF16)
            P.dma(xkv_sb[:], xkv_w[l].rearrange("(kc p) f -> p kc f", p=128), queue="pool")
            xo_sb = P.sb("xo_sb", [64, XH, D], BF16)
            P.dma(xo_sb[:], xo_w[l].rearrange("(h p) f -> p h f", p=64), queue="pool")
            g_xq = load_bc("g_xq", ln_xq[l], D); g_mem = load_bc("g_mem", ln_mem[l], D)
            g_q = load_bc("g_xqn", xq_norm[l], XD); g_k = load_bc("g_xkn", xk_norm[l], XD)
            memk = [P.sb("memk%d" % b, [64, XH, MEM], BF16) for b in range(NS)]
            memV = [P.sb("memV%d" % b, [128, 2, XH, 65], BF16) for b in range(NS)]
            def body(b):
                xg_r = Rot(P, "xg", [128, D], F32, 5)
                hb_r = Rot(P, "hbx", [128, D], BF16, 2)
                hT_r = Rot(P, "hTx", [128, 8, 512], BF16, 1)
                mT = P.sb("mT", [128, 8, MEM], BF16)
                f512 = Rot(P, "f512x", [128, 512], F32, 3)
                b256 = Rot(P, "b256x", [128, 256], BF16, 3)
                qT_r = Rot(P, "qTx", [64, XH, 512], BF16, 1)
                oT_r = Rot(P, "oTx", [64, XH, 512], BF16, 1)
                pt_r = Rot(P, "ptx", [128, 512], BF16, 3)
                osb_r = Rot(P, "osbx", [128, 512], F32, 1); rl_r = Rot(P, "rlx", [128, 512], F32, 1)
                scale = float(XD ** -0.5)

                def head_norm(src_ap, g_bc, out_bf):
                    sq = f512.get()
                    P.actf(sq[:, 0:256], src_ap, AF.Square)
                    ss = small.get()
                    P.red(ss[:, 0:4], sq[:, 0:256].rearrange("p (h f) -> p h f", h=4), ALU.add)
                    r = rstd_from_ss(ss[:, 0:4], 4, 1.0 / XD)
                    qn = f512.get()
                    qn3 = qn[:, 0:256].rearrange("p (h f) -> p h f", h=4)
                    P.tt(qn3, src_ap.rearrange("p (h f) -> p h f", h=4), bc_last(r, XD), ALU.mult)
                    P.tt(out_bf.rearrange("p (h f) -> p h f", h=4), qn3, bc_mid(g_bc[:], 4), ALU.mult)

                if True:
                    P.memset(memV[b][:, :, :, 64:65], 1.0)
                    for mt in range(2):
                        xm = xg_r.get()
                        P.dma(xm[:], mem_d[b, mt * 128:(mt + 1) * 128, :])
                        mb = hb_r.get()
                        rmsnorm_full(xm[:], D, g_mem[:], mb[:])
                        transpose_to(mb[:], 8, 128, mT[:, :, mt * 128:(mt + 1) * 128])
                    for mt in range(2):
                        ps = bank()
                        for kc in range(8):
                            P.mm(ps[:, :], mT[:, kc, mt * 128:(mt + 1) * 128], xkv_sb[:, kc, :], start=(kc == 0), stop=(kc == 7))
                        kv = f512.get()
                        P.copy(kv[:], ps[:, :], eng="act")
                        knb = b256.get()
                        head_norm(kv[:, 0:256], g_k, knb[:])
                        transpose_to(knb[:], 4, 64, memk[b][:, :, mt * 128:(mt + 1) * 128])
                        P.copy(memV[b][:, mt, :, 0:64], kv[:, 256:512].rearrange("p (h f) -> p h f", h=4), eng="pool")

                if True:
                    for g in range(NG):
                        hT = hT_r.get()
                        xg = []
                        for t in range(4):
                            tile = g * 4 + t
                            x1 = xg_r.get()
                            P.dma(x1[:], out_d[b, tile * 128:(tile + 1) * 128, :], reads=[okey(b, tile)])
                            hb = hb_r.get()
                            rmsnorm_full(x1[:], D, g_xq[:], hb[:])
                            transpose_to(hb[:], 8, 128, hT[:, :, t * 128:(t + 1) * 128])
                            xg.append(x1)
                        qT = qT_r.get()
                        for t in range(4):
                            tsl = slice(t * 128, (t + 1) * 128)
                            ps_q = bank()
                            for kc in range(8):
                                P.mm(ps_q[:, 0:256], hT[:, kc, tsl], xq_sb[:, kc, :], start=(kc == 0), stop=(kc == 7))
                            q_sb = f512.get()
                            P.copy(q_sb[:, 0:256], ps_q[:, 0:256], eng="act")
                            qb = b256.get()
                            head_norm(q_sb[:, 0:256], g_q, qb[:])
                            transpose_to(qb[:], 4, 64, qT[:, :, tsl])
                        oT = oT_r.get()
                        for hh in range(XH):
                            ps_o = accbank()
                            for mt in range(2):
                                ps_s = bank()
                                P.mm(ps_s[:, :], memk[b][:, hh, mt * 128:(mt + 1) * 128], qT[:, hh, :])
                                pt = pt_r.get()
                                P.actf(pt[:], ps_s[:, :], AF.Exp, scale=scale)
                                P.mm(ps_o[0:65, :], memV[b][:, mt, hh, :], pt[:], start=(mt == 0), stop=(mt == 1))
                            softmax_finalize(ps_o, 512, oT[:, hh, :], osb_r, rl_r)
                        for t in range(4):
                            tile = g * 4 + t
                            tsl = slice(t * 128, (t + 1) * 128)
                            for hf in range(2):
                                ps = bank()
                                for hh in range(XH):
                                    P.mm(ps[:, :], oT[:, hh, tsl], xo_sb[:, hh, hf * 512:(hf + 1) * 512],
                                         start=(hh == 0), stop=(hh == XH - 1))
                                P.tt(xg[t][:, hf * 512:(hf + 1) * 512], xg[t][:, hf * 512:(hf + 1) * 512], ps[:, :], ALU.add)
                            P.dma(out_d[b, tile * 128:(tile + 1) * 128, :], xg[t][:], queue="pool", writes=[okey(b, tile)])
            stage_pools(NS, 20)
            set_banks([[0, 1, 2], [4, 5, 6]] if NS > 1 else [list(range(6))], [[3], [7]] if NS > 1 else [[6, 7]])
            run_streams(P, [(lambda b=b: body(b)) for b in range(NS)])
            set_banks([list(range(8))], [[6, 7]])
            dbg_dump("xa%d" % l)
            stage_end()

        def stage_moe(l):
          NTOK = NS * S
          SBT = min(2048, NTOK)
          nsb = NTOK // SBT
          tsb = SBT // 128
          gsb = SBT // 512
          BIG = 30000.0
          for sbi in range(nsb):
           with contextlib.ExitStack() as st:
            P.stack = st
            stage_pools()
            wr = P.sb("wr", [128, 8, 36], F32)
            P.dma(wr[:, :, 0:4], rg_w[l].rearrange("(kc p) f -> p kc f", p=128))
            P.dma(wr[:, :, 4:36], re_w[l].rearrange("(kc p) f -> p kc f", p=128))
            rb = P.sb("rb", [128, 36], F32)
            P.dma(rb[:, 0:4], rg_b[l].partition_broadcast(128))
            P.dma(rb[:, 4:36], re_b[l].partition_broadcast(128))
            g_ffn = load_bc("g_ffn", ln_ffn[l], D)
            acc = [P.sb("acc%d" % i, [128, D], F32) for i in range(tsb)]
            h2T = [P.sb("h2T%d" % i, [128, 8, 512], BF16) for i in range(gsb)]
            G = [P.sb("G%d" % i, [128, 32], F32) for i in range(tsb)]
            h32_r = Rot(P, "h32", [128, D], F32, 2)
            h32T_r = Rot(P, "h32T", [128, 8, 128], F32, 2)
            Wg_r = Rot(P, "Wg", [128, 8, EFF], BF16, 2); Wu_r = Rot(P, "Wu", [128, 8, EFF], BF16, 2)
            Wd_r = Rot(P, "Wd", [128, 2, D], BF16, 2)
            aT_r = Rot(P, "aTm", [128, 2, 512], BF16, 2)
            sg_r = Rot(P, "sgm", [128, 512], F32, 3)

            def tile_of(i):
                gtile = sbi * tsb + i
                return gtile // NT, gtile % NT

            for i in range(tsb):
                b, tile = tile_of(i)
                P.dma(acc[i][:], out_d[b, tile * 128:(tile + 1) * 128, :], reads=[okey(b, tile)])
                h32 = h32_r.get()
                rmsnorm_full(acc[i][:], D, g_ffn[:], h32[:])
                h32T = h32T_r.get()
                transpose_to(h32[:], 8, 128, h32T[:], dt=F32)
                P.copy(h2T[i // 4][:, :, (i % 4) * 128:(i % 4 + 1) * 128], h32T[:], eng="pool")
                ps_r = bank()
                for kc in range(8):
                    P.mm(ps_r[:, 0:36], h32T[:, kc, :], wr[:, kc, :], start=(kc == 0), stop=(kc == 7))
                lg = small.get()
                P.tt(lg[:, 0:36], ps_r[:, 0:36], rb[:], ALU.add)
                gmax = small.get(); P.red(gmax[:, 0:1], lg[:, 0:4], ALU.max)
                goh = small.get(); P.ts(goh[:, 0:4], lg[:, 0:4], gmax[:, 0:1], None, ALU.is_ge)
                ngmax = small.get(); P.ts(ngmax[:, 0:1], gmax[:, 0:1], -1.0, None, ALU.mult)
                gex = small.get(); gsum = small.get()
                P.actf(gex[:, 0:4], lg[:, 0:4], AF.Exp, bias=ngmax[:, 0:1], accum_out=gsum[:, 0:1])
                gp = small.get(); P.recip(gp[:, 0:1], gsum[:, 0:1])
                pen = small.get(); P.ts(pen[:, 0:4], goh[:, 0:4], -1.0, BIG, ALU.add, ALU.mult)
                elm = small.get()
                P.tt(elm[:, 0:32].rearrange("p (g e) -> p g e", g=4), lg[:, 4:36].rearrange("p (g e) -> p g e", g=4),
                     bc_last(pen[:, 0:4], 8), ALU.add)
                m1 = small.get(); P.red(m1[:, 0:1], elm[:, 0:32], ALU.max)
                oh1 = small.get(); P.ts(oh1[:, 0:32], elm[:, 0:32], m1[:, 0:1], None, ALU.is_ge)
                elm2 = small.get(); P.stt(elm2[:, 0:32], oh1[:, 0:32], -BIG, elm[:, 0:32], ALU.mult, ALU.add)
                m2 = small.get(); P.red(m2[:, 0:1], elm2[:, 0:32], ALU.max)
                sel = small.get(); P.ts(sel[:, 0:32], elm2[:, 0:32], m2[:, 0:1], None, ALU.is_ge)
                P.tt(sel[:, 0:32], sel[:, 0:32], oh1[:, 0:32], ALU.add)
                nm1 = small.get(); P.ts(nm1[:, 0:1], m1[:, 0:1], -1.0, None, ALU.mult)
                ex = small.get(); P.actf(ex[:, 0:32], elm[:, 0:32], AF.Exp, bias=nm1[:, 0:1])
                wv = small.get(); P.tt(wv[:, 0:32], ex[:, 0:32], sel[:, 0:32], ALU.mult)
                ws = small.get(); P.red(ws[:, 0:1], wv[:, 0:32], ALU.add)
                rws = small.get(); P.recip(rws[:, 0:1], ws[:, 0:1])
                coef = small.get(); P.tt(coef[:, 0:1], rws[:, 0:1], gp[:, 0:1], ALU.mult)
                P.ts(G[i][:], wv[:, 0:32], coef[:, 0:1], None, ALU.mult)
                dump("G_%d_%d" % (l, i), G[i][:])
            for e in range(NEXP):
                Wg = Wg_r.get(); Wu = Wu_r.get(); Wd = Wd_r.get()
                P.dma(Wg[:], ewg_l[l][e * 128:(e + 1) * 128, :].rearrange("p (kc f) -> p kc f", kc=8), queue="pool")
                P.dma(Wu[:], ewu_l[l][e * 128:(e + 1) * 128, :].rearrange("p (kc f) -> p kc f", kc=8), queue="pool")
                P.dma(Wd[:], ewd_l[l][e * 128:(e + 1) * 128, :].rearrange("p (c f) -> p c f", c=2), queue="pool")
                for gi in range(gsb):
                    pg = [bank(), bank()]
                    pu = [bank(), bank()]
                    for fc in range(2):
                        for kc in range(8):
                            P.mm(pg[fc][:, :], Wg[:, kc, fc * 128:(fc + 1) * 128], h2T[gi][:, kc, :], start=(kc == 0), stop=(kc == 7))
                        for kc in range(8):
                            P.mm(pu[fc][:, :], Wu[:, kc, fc * 128:(fc + 1) * 128], h2T[gi][:, kc, :], start=(kc == 0), stop=(kc == 7))
                    aT = aT_r.get()
                    for fc in range(2):
                        sg = sg_r.get()
                        P.actf(sg[:], pg[fc][:, :], AF.Silu)
                        P.tt(aT[:, fc, :], sg[:], pu[fc][:, :], ALU.mult)
                    for t in range(4):
                        i = gi * 4 + t
                        for hf in range(2):
                            pd = bank()
                            for fc in range(2):
                                P.mm(pd[:, :], aT[:, fc, t * 128:(t + 1) * 128], Wd[:, fc, hf * 512:(hf + 1) * 512],
                                     start=(fc == 0), stop=(fc == 1))
                            P.stt(acc[i][:, hf * 512:(hf + 1) * 512], pd[:, :], G[i][:, e:e + 1],
                                  acc[i][:, hf * 512:(hf + 1) * 512], ALU.mult, ALU.add)
            for i in range(tsb):
                b, tile = tile_of(i)
                P.dma(out_d[b, tile * 128:(tile + 1) * 128, :], acc[i][:], queue="pool", writes=[okey(b, tile)])
            if sbi == nsb - 1:
                dbg_dump("moe%d" % l) if l == 0 else None
            stage_end()

        def stage_pool():
          with contextlib.ExitStack() as st:
            P.stack = st
            pw_sb = P.sb("pw_sb", [128, 4, 2, 256], BF16)
            for cg in range(4):
                P.dma(pw_sb[:, cg, :, :], pool_w[0, cg].rearrange("(cc p) d -> p cc d", p=128), queue="pool")
            pb_bc = load_bc("pb_bc", pool_b[0], D); psc_bc = load_bc("psc_bc", pool_scale[0], D)
            g_m1 = load_bc("g_m1", ln_mix[1], D)
            icnt = load_bc("icnt", c_icnt.rearrange("a b -> (a b)"), 4 * 512)
            def body(b):
                H = P.sb("H", [128, 8, 527], F32)
                xg_r = Rot(P, "xgp", [128, D], F32, 5)
                hb_r = Rot(P, "hbp", [128, D], BF16, 2)
                lv_r = Rot(P, "lv", [128, 2, 527], F32, 3)
                dl_r = [Rot(P, "dl%d" % c, [128, 2, 512], BF16, 1) for c in range(4)]
                f512 = Rot(P, "f512p", [128, 512], F32, 3)
                if True:
                    P.memset(H[:, :, 0:15], 0.0)
                    for g in range(NG):
                        xg = []
                        for t in range(4):
                            tile = g * 4 + t
                            x1 = xg_r.get()
                            P.dma(x1[:], out_d[b, tile * 128:(tile + 1) * 128, :], reads=[okey(b, tile)])
                            hb = hb_r.get()
                            rmsnorm_full(x1[:], D, g_m1[:], hb[:])
                            transpose_to(hb[:], 8, 128, H[:, :, 15 + t * 128:15 + (t + 1) * 128])
                            xg.append(x1)
                        dl = []
                        for cg in range(4):
                            w = 2 ** (cg + 1)
                            cur = H[:, 2 * cg:2 * cg + 2, :]
                            for k in range(cg + 1):
                                sh = 2 ** k
                                lo = 2 * sh - 1
                                nx = lv_r.get()
                                P.tt(nx[:, :, lo:527], cur[:, :, lo:527], cur[:, :, lo - sh:527 - sh], ALU.add,
                                     eng=("pool" if k % 2 == 0 else "dve"))
                                cur = nx[:]
                            d_ = dl_r[cg].get()
                            if g == 0:
                                tmp = lv_r.get()
                                P.tt(tmp[:, :, 0:512], cur[:, :, 15:527], bc_mid(icnt[:, cg * 512:(cg + 1) * 512], 2), ALU.mult)
                                P.tt(d_[:], tmp[:, :, 0:512], H[:, 2 * cg:2 * cg + 2, 15:527], ALU.subtract)
                            else:
                                P.stt(d_[:], cur[:, :, 15:527], 1.0 / w, H[:, 2 * cg:2 * cg + 2, 15:527], ALU.mult, ALU.subtract)
                            dl.append(d_)
                        P.copy(H[:, :, 0:15], H[:, :, 512:527], eng="pool")
                        for t in range(4):
                            tile = g * 4 + t
                            tsl = slice(t * 128, (t + 1) * 128)
                            for hf in range(2):
                                ps = bank()
                                for c2 in range(2):
                                    cg = hf * 2 + c2
                                    for cc in range(2):
                                        P.mm(ps[:, c2 * 256:(c2 + 1) * 256], dl[cg][:, cc, tsl], pw_sb[:, cg, cc, :],
                                             start=(cc == 0), stop=(cc == 1))
                                tmp = f512.get()
                                hs = slice(hf * 512, (hf + 1) * 512)
                                P.tt(tmp[:], ps[:, :], pb_bc[:, hs], ALU.add)
                                P.tt(tmp[:], tmp[:], psc_bc[:, hs], ALU.mult, eng="pool")
                                P.tt(xg[t][:, hs], xg[t][:, hs], tmp[:], ALU.add)
                            P.dma(out_d[b, tile * 128:(tile + 1) * 128, :], xg[t][:], queue="pool", writes=[okey(b, tile)])
            stage_pools(NS, 12)
            set_banks([[0, 1, 2, 3], [4, 5, 6, 7]] if NS > 1 else [list(range(8))], [[6, 7]])
            run_streams(P, [(lambda b=b: body(b)) for b in range(NS)])
            set_banks([list(range(8))], [[6, 7]])
            dbg_dump("mix1")
            stage_end()


        bregs = {}

        def breg(e, val):
            if val not in bregs:
                bregs[val] = e.to_reg(val)
            return bregs[val]

        def stage_moe_sparse(l):
          BIGV = 30000.0
          def tile_of(i):
              return i // NT, i % NT
          widx = gst.enter_context(nc.sbuf_tensor("widx_l%d" % l, [128, NBLK], I32))
          IDX = gst.enter_context(nc.sbuf_tensor("IDX_l%d" % l, [128, NTI, 2], I32))
          G01 = gst.enter_context(nc.sbuf_tensor("G01_l%d" % l, [128, NTI, 2], F32))
          with contextlib.ExitStack() as st:
            P.stack = st
            wr = P.sb("wr", [128, 8, 36], F32)
            P.dma(wr[:, :, 0:4], rg_w[l].rearrange("(kc p) f -> p kc f", p=128))
            P.dma(wr[:, :, 4:36], re_w[l].rearrange("(kc p) f -> p kc f", p=128))
            rb = P.sb("rb", [128, 36], F32)
            P.dma(rb[:, 0:4], rg_b[l].partition_broadcast(128))
            P.dma(rb[:, 4:36], re_b[l].partition_broadcast(128))
            g_ffn = load_bc("g_ffn", ln_ffn[l], D)
            utsf = P.sb("utsf", [128, 128]); P.dma(utsf[:], c_uts)
            thr = load_bc("thr", c_thr, 16); jidx = load_bc("jidx", c_jidx, NBLK)
            iota = P.sb("iota", [128, 1]); P.dma(iota[:], c_iota)
            zkeys = []
            RT = P.sb("RT", [128, NTI, 128], F32)
            NSTR = 4 if NTI % 4 == 0 and NTI >= 8 else 1
            TPS = NTI // NSTR
            carries = [P.sb("carry%d" % k, [128, 32], F32) for k in range(NSTR)]
            for c_ in carries:
                P.memset(c_[:], 0.0)

            def rkey(i):
                return ("RT", i)

            def body1(sidx):
                carry = carries[sidx]
                x_r = Rot(P, "xm1", [128, D], F32, 2)
                h32_r = Rot(P, "h32", [128, D], F32, 1)
                hb_r = Rot(P, "hbm", [128, D], BF16, 2)
                h32T_r = Rot(P, "h32T", [128, 8, 128], F32, 1)
                for i in range(sidx * TPS, (sidx + 1) * TPS):
                    b, tile = tile_of(i)
                    x1 = x_r.get()
                    P.dma(x1[:], out_d[b, tile * 128:(tile + 1) * 128, :], reads=[okey(b, tile)])
                    h32 = h32_r.get()
                    rmsnorm_full(x1[:], D, g_ffn[:], h32[:])
                    hb = hb_r.get()
                    P.copy(hb[:], h32[:], eng="pool")
                    P.dma(Hn_d[i * 128:(i + 1) * 128, :], hb[:], queue="pool", writes=[("Hn", i)])
                    h32T = h32T_r.get()
                    transpose_to(h32[:], 8, 128, h32T[:], dt=F32)
                    ps_r = bank()
                    for kc in range(8):
                        P.mm(ps_r[:, 0:36], h32T[:, kc, :], wr[:, kc, :], start=(kc == 0), stop=(kc == 7))
                    lg = small.get()
                    P.tt(lg[:, 0:36], ps_r[:, 0:36], rb[:], ALU.add)
                    gmax = small.get(); P.red(gmax[:, 0:1], lg[:, 0:4], ALU.max)
                    goh = small.get(); P.ts(goh[:, 0:4], lg[:, 0:4], gmax[:, 0:1], None, ALU.is_ge)
                    ngmax = small.get(); P.ts(ngmax[:, 0:1], gmax[:, 0:1], -1.0, None, ALU.mult)
                    gex = small.get(); gsum = small.get()
                    P.actf(gex[:, 0:4], lg[:, 0:4], AF.Exp, bias=ngmax[:, 0:1], accum_out=gsum[:, 0:1])
                    gp = small.get(); P.recip(gp[:, 0:1], gsum[:, 0:1])
                    pen = small.get(); P.ts(pen[:, 0:4], goh[:, 0:4], -1.0, BIGV, ALU.add, ALU.mult)
                    elm = small.get()
                    P.tt(elm[:, 0:32].rearrange("p (g e) -> p g e", g=4), lg[:, 4:36].rearrange("p (g e) -> p g e", g=4),
                         bc_last(pen[:, 0:4], 8), ALU.add)
                    m1 = small.get(); P.red(m1[:, 0:1], elm[:, 0:32], ALU.max)
                    rt = small.get()
                    P.ts(rt[:, 0:32], elm[:, 0:32], m1[:, 0:1], None, ALU.is_ge)
                    elm2 = small.get(); P.stt(elm2[:, 0:32], rt[:, 0:32], -BIGV, elm[:, 0:32], ALU.mult, ALU.add)
                    m2 = small.get(); P.red(m2[:, 0:1], elm2[:, 0:32], ALU.max)
                    P.ts(rt[:, 32:64], elm2[:, 0:32], m2[:, 0:1], None, ALU.is_ge)
                    sel = small.get(); P.tt(sel[:, 0:32], rt[:, 0:32], rt[:, 32:64], ALU.add)
                    nm1 = small.get(); P.ts(nm1[:, 0:1], m1[:, 0:1], -1.0, None, ALU.mult)
                    ex = small.get(); P.actf(ex[:, 0:32], elm[:, 0:32], AF.Exp, bias=nm1[:, 0:1])
                    wv = small.get(); P.tt(wv[:, 0:32], ex[:, 0:32], sel[:, 0:32], ALU.mult)
                    ws = small.get(); P.red(ws[:, 0:1], wv[:, 0:32], ALU.add)
                    rws = small.get(); P.recip(rws[:, 0:1], ws[:, 0:1])
                    coef = small.get(); P.tt(coef[:, 0:1], rws[:, 0:1], gp[:, 0:1], ALU.mult)
                    P.ts(rt[:, 64:96], wv[:, 0:32], coef[:, 0:1], None, ALU.mult)
                    ps_p = bank()
                    P.mm(ps_p[:, 0:32], utsf[:], sel[:, 0:32])
                    P.mm(ps_p[:, 32:64], onesf[:], sel[:, 0:32])
                    P.tt(rt[:, 96:128], ps_p[:, 0:32], carry[:], ALU.add)
                    P.tt(carry[:], carry[:], ps_p[:, 32:64], ALU.add)
                    P.op("pool", lambda e, i=i, rt=rt: e.tensor_copy(RT[:, i, :], rt[:, 0:128]), [rt], [rkey(i)])

            stage_pools(NSTR, 17)
            if NSTR > 1:
                set_banks([[0, 1], [2, 3], [4, 5], [6, 7]], [[6, 7]])
            run_streams(P, [(lambda k=k: body1(k)) for k in range(NSTR)])
            set_banks([list(range(8))], [[6, 7]])
            if os.environ.get("MK_MOE_STOP") == "1":
                P.barrier(); P.emit()
                return
            offs = [None]
            carry = P.sb("carry_tot", [128, 32], F32)
            P.copy(carry[:], carries[0][:])
            for k in range(1, NSTR):
                o = P.sb("off%d" % k, [128, 32], F32)
                P.copy(o[:], carry[:])
                offs.append(o)
                P.tt(carry[:], carry[:], carries[k][:], ALU.add)
            cmp = P.sb("cmp", [128, 32, 16], F32)
            P.tt(cmp[:], bc_last(carry[:], 16), bc_mid(thr[:], 32), ALU.is_gt)
            nb = P.sb("nb", [128, 32], F32)
            P.red(nb[:], cmp[:], ALU.add)
            sc = [P.sb("sc0", [128, 32], F32), P.sb("sc1", [128, 32], F32)]
            P.copy(sc[0][:], nb[:])
            cur = 0
            for sh in (1, 2, 4, 8, 16):
                a, bb = sc[cur], sc[1 - cur]
                P.copy(bb[:, 0:sh], a[:, 0:sh])
                P.tt(bb[:, sh:32], a[:, sh:32], a[:, 0:32 - sh], ALU.add)
                cur = 1 - cur
            bend = sc[cur]
            rowst = P.sb("rowst", [128, 32], F32)
            P.tt(rowst[:], bend[:], nb[:], ALU.subtract)
            P.ts(rowst[:], rowst[:], float(BLK), None, ALU.mult)
            cmp2 = P.sb("cmp2", [128, NBLK, 32], F32)
            P.tt(cmp2[:], bc_last(jidx[:], 32), bc_mid(bend[:], NBLK), ALU.is_ge)
            be = P.sb("be", [128, NBLK], F32)
            P.red(be[:], cmp2[:], ALU.add)
            P.ts(be[:], be[:], 128.0, iota[:, 0:1], ALU.mult, ALU.add)
            P.copy(widx[:], be[:])
            rowst_s = [rowst]
            for k in range(1, NSTR):
                rs = P.sb("rowst%d" % k, [128, 32], F32)
                P.tt(rs[:], rowst[:], offs[k][:], ALU.add)
                rowst_s.append(rs)
            hb2_r = Rot(P, "hb2", [128, D], BF16, 3)
            xs_keys = []
            for i in range(NTI):
                dall = small.get()
                rs_ = rowst_s[i // TPS]
                P.op("dve", lambda e, i=i, dall=dall, rs_=rs_: e.tensor_tensor(dall[:, 0:32], RT[:, i, 96:128], rs_[:], ALU.add),
                     [rkey(i), rs_], [dall])
                d4 = small.get()
                tmp = small.get()
                for k in range(2):
                    P.op("dve", lambda e, i=i, k=k, tmp=tmp, dall=dall: e.tensor_tensor(tmp[:, 0:32], RT[:, i, 32 * k:32 * k + 32], dall[:, 0:32], ALU.mult),
                         [rkey(i), dall], [tmp])
                    P.red(d4[:, k:k + 1], tmp[:, 0:32], ALU.add)
                    P.op("dve", lambda e, i=i, k=k, tmp=tmp: e.tensor_tensor(tmp[:, 32:64], RT[:, i, 32 * k:32 * k + 32], RT[:, i, 64:96], ALU.mult),
                         [rkey(i)], [tmp])
                    P.red(d4[:, 2 + k:3 + k], tmp[:, 32:64], ALU.add)
                P.op("dve", lambda e, i=i, d4=d4: e.tensor_copy(IDX[:, i, :], d4[:, 0:2]), [d4], [("IDX", i)])
                P.op("dve", lambda e, i=i, d4=d4: e.tensor_copy(G01[:, i, :], d4[:, 2:4]), [d4], [("G01", i)])
                hb2 = hb2_r.get()
                P.dma(hb2[:], Hn_d[i * 128:(i + 1) * 128, :], reads=[("Hn", i)])
                for k in range(2):
                    P.dma_fn(lambda e, i=i, k=k, hb2=hb2: e.indirect_dma_start(
                        out=Xs_d[:, :], out_offset=bass.IndirectOffsetOnAxis(ap=IDX[:, i, k:k + 1], axis=0),
                        in_=hb2[:], in_offset=None, bounds_check=breg(e, NROWS - 1), oob_is_err=False),
                        "pool", [("IDX", i), hb2] + zkeys, [("Xs", i, k)])
                    xs_keys.append(("Xs", i, k))
            P.barrier()
            P.emit()
          if os.environ.get("MK_MOE_STOP") == "2":
              return
          with contextlib.ExitStack() as st:
            P.stack = st
            stage_pools()
            Wg_r = Rot(P, "Wg", [128, 2048], BF16, 3); Wu_r = Rot(P, "Wu", [128, 2048], BF16, 3)
            Wd_r = Rot(P, "Wd", [128, 2048], BF16, 3)
            xb_r = Rot(P, "xbm", [128, D], BF16, 12)
            xT_r = Rot(P, "xTm", [128, 8, 512], BF16, 2)
            aT_r = Rot(P, "aTm", [128, 2, 512], BF16, 2)
            sg_r = Rot(P, "sgm", [128, 512], F32, 3)
            yb_r = Rot(P, "ybm", [128, D], BF16, 4)
            def loads3(j):
                Wg = Wg_r.get(); Wu = Wu_r.get(); Wd = Wd_r.get()
                for Wt, src in ((Wg, ewg_l), (Wu, ewu_l), (Wd, ewd_l)):
                    P.dma_fn(lambda e, Wt=Wt, src=src, j=j: e.indirect_dma_start(
                        out=Wt[:], out_offset=None, in_=src[l][:, :],
                        in_offset=bass.IndirectOffsetOnAxis(ap=widx[:, j:j + 1], axis=0),
                        bounds_check=breg(e, NEXP * 128 - 1), oob_is_err=False), "pool", [widx], [Wt])
                xbs = []
                for t in range(4):
                    xb = xb_r.get()
                    r0 = j * BLK + t * 128
                    P.dma(xb[:], Xs_d[r0:r0 + 128, :], reads=["Xs_all"])
                    xbs.append(xb)
                return Wg, Wu, Wd, xbs

            nxt = loads3(0)
            for j in range(NBLK):
                Wg, Wu, Wd, xbs = nxt
                if j + 1 < NBLK:
                    nxt = loads3(j + 1)
                Wg3 = Wg[:].rearrange("p (kc f) -> p kc f", kc=8)
                Wu3 = Wu[:].rearrange("p (kc f) -> p kc f", kc=8)
                Wd3 = Wd[:].rearrange("p (c f) -> p c f", c=2)
                xT = xT_r.get()
                for t in range(4):
                    transpose_to(xbs[t][:], 8, 128, xT[:, :, t * 128:(t + 1) * 128])
                pg = [bank(), bank()]
                pu = [bank(), bank()]
                for fc in range(2):
                    for kc in range(8):
                        P.mm(pg[fc][:, :], Wg3[:, kc, fc * 128:(fc + 1) * 128], xT[:, kc, :], start=(kc == 0), stop=(kc == 7))
                    for kc in range(8):
                        P.mm(pu[fc][:, :], Wu3[:, kc, fc * 128:(fc + 1) * 128], xT[:, kc, :], start=(kc == 0), stop=(kc == 7))
                aT = aT_r.get()
                for fc in range(2):
                    sg = sg_r.get()
                    P.actf(sg[:], pg[fc][:, :], AF.Silu)
                    P.tt(aT[:, fc, :], sg[:], pu[fc][:, :], ALU.mult)
                for t in range(4):
                    yb = yb_r.get()
                    for hf in range(2):
                        pd = bank()
                        for fc in range(2):
                            P.mm(pd[:, :], aT[:, fc, t * 128:(t + 1) * 128], Wd3[:, fc, hf * 512:(hf + 1) * 512],
                                 start=(fc == 0), stop=(fc == 1))
                        P.copy(yb[:, hf * 512:(hf + 1) * 512], pd[:, :], eng=("act" if hf == 0 else "dve"))
                    r0 = j * BLK + t * 128
                    P.dma(Ys_d[r0:r0 + 128, :], yb[:], queue="sp", writes=[("Ys", j, t)])
            P.barrier()
            P.emit()
          if os.environ.get("MK_MOE_STOP") == "3":
              return
          with contextlib.ExitStack() as st:
            P.stack = st
            stage_pools()
            x_r = Rot(P, "xm4", [128, D], F32, 4)
            y_r = Rot(P, "ym4", [128, D], BF16, 8)
            def loads4(i):
                b, tile = tile_of(i)
                x1 = x_r.get()
                P.dma(x1[:], out_d[b, tile * 128:(tile + 1) * 128, :], reads=[okey(b, tile)])
                ys = []
                for k in range(2):
                    y = y_r.get()
                    P.dma_fn(lambda e, i=i, k=k, y=y: e.indirect_dma_start(
                        out=y[:], out_offset=None, in_=Ys_d[:, :],
                        in_offset=bass.IndirectOffsetOnAxis(ap=IDX[:, i, k:k + 1], axis=0),
                        bounds_check=breg(e, NROWS - 1), oob_is_err=False), "pool", ["Ys_all", IDX], [y])
                    ys.append(y)
                return x1, ys

            nxt = loads4(0)
            for i in range(NTI):
                b, tile = tile_of(i)
                x1, ys = nxt
                if i + 1 < NTI:
                    nxt = loads4(i + 1)
                for k in range(2):
                    y = ys[k]
                    P.op("dve", lambda e, i=i, k=k, y=y, x1=x1: e.scalar_tensor_tensor(x1[:], y[:], G01[:, i, k:k + 1], x1[:], ALU.mult, ALU.add),
                         [y, x1, G01], [x1])
                P.dma(out_d[b, tile * 128:(tile + 1) * 128, :], x1[:], queue="sp", writes=[okey(b, tile)])
            if l == 0:
                dbg_dump("moe0")
            stage_end()

        if run("XA0"):
            stage_xa(0)
        SPARSE = bool(int(os.environ.get("MK_SPARSE", "1")))
        if run("MOE0"):
            (stage_moe_sparse if SPARSE else stage_moe)(0)
        if run("POOL"):
            stage_pool()
        if run("XA1"):
            stage_xa(1)
        if run("MOE1"):
            (stage_moe_sparse if SPARSE else stage_moe)(1)
        P.barrier(new_sems=False)
        P.emit()
        return nc, P, None


def host_consts():
    i = np.arange(128)
    c = {}
    c["c_ident"] = np.eye(128, dtype=np.float32)
    c["c_ut"] = (i[:, None] <= i[None, :]).astype(np.float32)
    c["c_ls"] = (i[:, None] > i[None, :]).astype(np.float32)
    c["c_invf"] = (THETA ** (-np.arange(0, ROPE // 2, dtype=np.float32) * 2.0 / ROPE)).astype(np.float32)
    c["c_uts"] = (i[:, None] < i[None, :]).astype(np.float32)
    c["c_thr"] = (np.arange(16) * 512).astype(np.float32)
    c["c_iota"] = np.arange(128, dtype=np.float32).reshape(128, 1)
    t = np.arange(512, dtype=np.float32)
    c["c_icnt"] = np.stack([1.0 / np.minimum(t + 1.0, float(w)) for w in (2, 4, 8, 16)]).astype(np.float32)
    return c


WEIGHT_KEYS = ["ln_mix", "w_in", "q_lat_norm", "w_uq", "kv_lat_norm", "w_ukv", "q_norm", "k_norm",
               "dt_bias", "a_log", "d_skip", "ssd_norm", "w_out", "pool_w", "pool_b", "pool_scale",
               "ln_xq", "ln_mem", "xq_w", "xkv_w", "xq_norm", "xk_norm", "xo_w", "ln_ffn", "rg_w", "rg_b",
               "re_w", "re_b"]


def make_in_maps(inputs, NS, S, n_cores):
    NT = S // 128
    shared = {k: np.ascontiguousarray(np.asarray(inputs[k], dtype=np.float32)) for k in WEIGHT_KEYS}
    cw = np.asarray(inputs["conv_w"], dtype=np.float32)[0]
    shared["conv_wl"] = np.ascontiguousarray(cw.reshape(CK, 8, 128).transpose(2, 1, 0))
    cb = np.asarray(inputs["conv_b"], dtype=np.float32)[0]
    shared["conv_bl"] = np.ascontiguousarray(cb.reshape(8, 128).T)
    shared.update(host_consts())
    nblk = (2 * NS * S) // 512 + NEXP
    shared["c_jidx"] = np.arange(nblk, dtype=np.float32)
    g = np.asarray(inputs["exp_w_gate"], dtype=np.float32); u = np.asarray(inputs["exp_w_up"], dtype=np.float32)
    dn = np.asarray(inputs["exp_w_down"], dtype=np.float32)
    for li in range(2):
        shared["ewg_l%d" % li] = np.ascontiguousarray(g[li].reshape(NEXP, 8, 128, EFF).transpose(0, 2, 1, 3)).reshape(NEXP * 128, 2048)
        shared["ewu_l%d" % li] = np.ascontiguousarray(u[li].reshape(NEXP, 8, 128, EFF).transpose(0, 2, 1, 3)).reshape(NEXP * 128, 2048)
        shared["ewd_l%d" % li] = np.ascontiguousarray(dn[li].reshape(NEXP, 2, 128, D).transpose(0, 2, 1, 3)).reshape(NEXP * 128, 2048)
    x = np.asarray(inputs["x"]); mem = np.asarray(inputs["mem"]); pos = np.asarray(inputs["positions"])
    maps = []
    for c in range(n_cores):
        sl = slice(c * NS, (c + 1) * NS)
        m = dict(shared)
        m["x"] = np.ascontiguousarray(x[sl], dtype=np.float32)
        m["mem"] = np.ascontiguousarray(mem[sl], dtype=np.float32)
        p = pos[sl].astype(np.int32).reshape(NS * NT, 128).T
        m["posT"] = np.ascontiguousarray(p)
        maps.append(m)
    return maps


_CACHE = {}


def kernel(**inputs):
    n_cores = 8
    x = np.asarray(inputs["x"])
    B, S, _ = x.shape
    NS = B // n_cores
    key = (NS, S)
    if key not in _CACHE:
        nc, _, _ = build_program(NS, S, dbg=False)
        _CACHE[key] = nc
    nc = _CACHE[key]
    maps = make_in_maps(inputs, NS, S, n_cores)
    res = run_bass_kernel_spmd(nc, maps, core_ids=list(range(n_cores)))
    out = np.concatenate([np.asarray(r["out"]) for r in res.results], axis=0)
    return out.astype(np.float32, copy=False)
```

```python
import contextlib
import os
import numpy as np
import ml_dtypes
import concourse.bass as bass
import concourse.mybir as mybir
from concourse.bass_utils import run_bass_kernel_spmd

F32 = mybir.dt.float32
BF16 = mybir.dt.bfloat16
I32 = mybir.dt.int32
AF = mybir.ActivationFunctionType
ALU = mybir.AluOpType
AX = mybir.AxisListType

RSTD_LNEXP = bool(int(os.environ.get('MK_LNEXP', '1')))
SILU_EXP = bool(int(os.environ.get('MK_SILUEXP', '0')))
NO_SAME_ENGINE_SYNC = bool(int(os.environ.get('MK_NOSAME', '0')))
COMPUTE = ("pe", "act", "dve", "pool")
ALLQ = ("pe", "act", "dve", "pool", "sp")

D = 1024
MEM = 256
XH, XD = 4, 64
AH, NOPE, ROPE, QK, VD = 8, 64, 32, 96, 64
QL, KVL = 256, 128
BH, HP, DI, SG, SN, CK = 8, 64, 512, 2, 128, 4
IN_DIM = 1960
NEXP, EFF = 32, 256
EPS = 1e-6
THETA = 10000.0


class Prog:
    def __init__(self, nc, n_dma_sems=16):
        self.nc = nc
        self.stack = None
        self.q = {e: [] for e in ALLQ}
        self.dsem = [nc.alloc_semaphore("dsem%d" % i) for i in range(n_dma_sems)]
        self.dtot = [0] * n_dma_sems
        self.dq = {"sp": list(range(0, n_dma_sems // 2)), "pool": list(range(n_dma_sems // 2, n_dma_sems)),
                   "act": list(range(0, n_dma_sems // 2))}
        self.drr = {"sp": 0, "pool": 0, "act": 3}
        self.semobj = {}
        for i, s in enumerate(self.dsem):
            self.semobj["dsem%d" % i] = s
        self.known = {e: {} for e in ALLQ}
        self.last_w = {}
        self.readers = {}
        self.epoch = -1
        self.n_inst = 0
        self.n_wait = 0
        self.esem = {}
        self.ecnt = {}
        self.stream = 0
        self.hook = None
        self._new_sems()

    def _new_sems(self):
        self.epoch += 1
        for e in COMPUTE:
            nm = "sem_%s_%d" % (e, self.epoch)
            s = self.nc.alloc_semaphore(nm)
            self.esem[e] = (nm, s)
            self.semobj[nm] = s
            self.ecnt[e] = 0

    def sb(self, name, shape, dtype=F32):
        self._uid = getattr(self, "_uid", 0) + 1
        return self.stack.enter_context(self.nc.sbuf_tensor("%s_u%d" % (name, self._uid), list(shape), dtype))

    @staticmethod
    def key(x):
        if isinstance(x, (str, tuple)):
            return x
        if hasattr(x, "tensor"):
            return x.tensor.name
        return x.name

    def _wait(self, eng, tok, force=False):
        semname, val = tok
        if val <= 0:
            return
        kn = self.known[eng]
        if kn.get(semname, 0) >= val:
            return
        if eng in COMPUTE and semname == self.esem[eng][0] and not force and (eng == "pe" or NO_SAME_ENGINE_SYNC):
            return
        kn[semname] = val
        sem = self.semobj[semname]
        self.q[eng].append(lambda e, sem=sem, val=val: e.wait_ge(sem, val))
        self.n_wait += 1

    def _deps(self, eng, reads, writes):
        deps = {}
        for k in reads:
            t = self.last_w.get(self.key(k))
            if t:
                deps[t[0]] = max(deps.get(t[0], 0), t[1])
        for k in writes:
            k = self.key(k)
            t = self.last_w.get(k)
            if t:
                deps[t[0]] = max(deps.get(t[0], 0), t[1])
            for t in self.readers.get(k, ()):
                deps[t[0]] = max(deps.get(t[0], 0), t[1])
        for s, v in deps.items():
            self._wait(eng, (s, v))

    def _record(self, tok, reads, writes):
        for k in reads:
            lst = self.readers.setdefault(self.key(k), [])
            for i, t in enumerate(lst):
                if t[0] == tok[0]:
                    lst[i] = tok
                    break
            else:
                lst.append(tok)
        for k in writes:
            k = self.key(k)
            self.last_w[k] = tok
            self.readers[k] = []

    def op(self, eng, fn, reads=(), writes=()):
        self._deps(eng, reads, writes)
        self.ecnt[eng] += 1
        nm, sem = self.esem[eng]
        self.q[eng].append(lambda e, fn=fn, sem=sem: fn(e).then_inc(sem, 1))
        tok = (nm, self.ecnt[eng])
        self._record(tok, reads, writes)
        self.n_inst += 1
        if self.hook:
            self.hook()
        return tok

    def dma(self, out, in_, queue="sp", reads=None, writes=None, **kw):
        reads = [in_] if reads is None else reads
        writes = [out] if writes is None else writes
        lst = self.dq[queue]
        i = lst[self.drr[queue] % len(lst)]
        self.drr[queue] += 1
        semname = "dsem%d" % i
        self._wait(queue, (semname, self.dtot[i]))
        self._deps(queue, reads, writes)
        self.dtot[i] += 16
        sem = self.dsem[i]
        self.q[queue].append(lambda e, sem=sem: e.dma_start(out=out, in_=in_, **kw).then_inc(sem, 16))
        tok = (semname, self.dtot[i])
        self._record(tok, reads, writes)
        self.n_inst += 1
        if self.hook:
            self.hook()
        return tok

    def dma_fn(self, fn, queue, reads, writes):
        lst = self.dq[queue]
        i = lst[self.drr[queue] % len(lst)]
        self.drr[queue] += 1
        semname = "dsem%d" % i
        self._wait(queue, (semname, self.dtot[i]))
        self._deps(queue, reads, writes)
        self.dtot[i] += 16
        sem = self.dsem[i]
        self.q[queue].append(lambda e, sem=sem: fn(e).then_inc(sem, 16))
        tok = (semname, self.dtot[i])
        self._record(tok, reads, writes)
        self.n_inst += 1
        return tok

    def barrier(self, new_sems=True):
        for e in ALLQ:
            for c in COMPUTE:
                self._wait(e, (self.esem[c][0], self.ecnt[c]), force=True)
            for i in range(len(self.dsem)):
                self._wait(e, ("dsem%d" % i, self.dtot[i]))
        self.last_w = {}
        self.readers = {}
        if new_sems:
            self._new_sems()

    def mm(self, out, lhsT, rhs, start=True, stop=True, reads=None, writes=None):
        reads = [lhsT, rhs] if reads is None else reads
        writes = [out] if writes is None else writes
        return self.op("pe", lambda e: e.matmul(out, lhsT, rhs, start=start, stop=stop), reads, writes)

    def tr(self, out, in_, ident):
        return self.op("pe", lambda e: e.transpose(out, in_, ident), [in_, ident], [out])

    def actf(self, out, in_, func, bias=None, scale=None, accum_out=None):
        r = [in_]
        kw = {}
        if bias is not None:
            kw["bias"] = bias
            if not isinstance(bias, (int, float)):
                r.append(bias)
        if scale is not None:
            kw["scale"] = scale
            if not isinstance(scale, (int, float)):
                r.append(scale)
        w = [out]
        if accum_out is not None:
            kw["accum_out"] = accum_out
            w.append(accum_out)
        return self.op("act", lambda e: e.activation(out, in_, func, **kw), r, w)

    def tt(self, out, in0, in1, op, eng="dve"):
        return self.op(eng, lambda e: e.tensor_tensor(out, in0, in1, op), [in0, in1], [out])

    def ts(self, out, in0, s1, s2, op0, op1=None, eng="dve"):
        r = [in0]
        if not isinstance(s1, (int, float)):
            r.append(s1)
        if s2 is not None and not isinstance(s2, (int, float)):
            r.append(s2)
        if op1 is None:
            return self.op(eng, lambda e: e.tensor_scalar(out, in0, s1, None, op0), r, [out])
        return self.op(eng, lambda e: e.tensor_scalar(out, in0, s1, s2, op0, op1), r, [out])

    def stt(self, out, in0, scalar, in1, op0, op1, eng="dve"):
        r = [in0, in1]
        if not isinstance(scalar, (int, float)):
            r.append(scalar)
        return self.op(eng, lambda e: e.scalar_tensor_tensor(out, in0, scalar, in1, op0, op1), r, [out])

    def copy(self, out, in_, eng="dve"):
        if eng == "act":
            return self.op("act", lambda e: e.copy(out, in_), [in_], [out])
        return self.op(eng, lambda e: e.tensor_copy(out, in_), [in_], [out])

    def red(self, out, in_, op, axis=None, eng="dve"):
        axis = AX.X if axis is None else axis
        return self.op(eng, lambda e: e.tensor_reduce(out, in_, axis, op), [in_], [out])

    def recip(self, out, in_):
        return self.op("dve", lambda e: e.reciprocal(out, in_), [in_], [out])

    def memset(self, ap, val, eng="pool"):
        return self.op(eng, lambda e: e.memset(ap, val), [], [ap])

    def emit(self):
        nc = self.nc
        q = self.q
        self.q = {e: [] for e in ALLQ}
        with nc.Block() as block:
            @block.sync
            def _(e):
                for f in q["sp"]:
                    f(e)

            @block.tensor
            def _(e):
                for f in q["pe"]:
                    f(e)

            @block.scalar
            def _(e):
                for f in q["act"]:
                    f(e)

            @block.vector
            def _(e):
                for f in q["dve"]:
                    f(e)

            @block.gpsimd
            def _(e):
                for f in q["pool"]:
                    f(e)


def run_streams(P, bodies):
    import threading
    n = len(bodies)
    if n == 1:
        P.stream = 0
        bodies[0]()
        return
    sems = [threading.Semaphore(0) for _ in range(n)]
    main = threading.Semaphore(0)
    done = [False] * n
    cur = [0]
    errs = []

    def nxt(i):
        for d in range(1, n + 1):
            j = (i + d) % n
            if not done[j]:
                return j
        return None

    def hook():
        i = cur[0]
        j = nxt(i)
        if j is None or j == i:
            return
        cur[0] = j
        P.stream = j
        sems[j].release()
        sems[i].acquire()
        P.stream = i

    def runner(i):
        sems[i].acquire()
        P.stream = i
        try:
            bodies[i]()
        except BaseException as ex:
            errs.append(ex)
        done[i] = True
        j = nxt(i)
        if j is None:
            main.release()
        else:
            cur[0] = j
            P.stream = j
            sems[j].release()

    ths = [threading.Thread(target=runner, args=(i,)) for i in range(n)]
    for t in ths:
        t.start()
    P.hook = hook
    cur[0] = 0
    sems[0].release()
    main.acquire()
    P.hook = None
    P.stream = 0
    for t in ths:
        t.join()
    if errs:
        raise errs[0]


class SRot:
    def __init__(self, P, name, shape, dtype):
        self.P, self.name, self.shape, self.dtype = P, name, shape, dtype
        self.pools = []

    def setup(self, nstreams, n):
        self.pools = [Rot(self.P, "%s_s%d" % (self.name, i), self.shape, self.dtype, n) for i in range(nstreams)]

    def get(self):
        return self.pools[self.P.stream].get()


class SplitRot:
    def __init__(self, pools, P):
        self.pools, self.P = pools, P

    def get(self):
        return self.pools[min(self.P.stream, len(self.pools) - 1)].get()


class Rot:
    def __init__(self, P, name, shape, dtype, n):
        self.t = [P.sb("%s_%d" % (name, i), shape, dtype) for i in range(n)]
        self.i = 0

    def get(self):
        t = self.t[self.i % len(self.t)]
        self.i += 1
        return t


def bc_mid(ap2d, n):
    p, f = ap2d.shape
    return ap2d.unsqueeze(1).to_broadcast([p, n, f])


def bc_last(ap2d, n):
    p, h = ap2d.shape
    return ap2d.unsqueeze(2).to_broadcast([p, h, n])


def build_program(NS, S, dbg=False, stages=None):
    NT = S // 128
    NG = S // 512
    NTT = NS * NT
    nc = bass.Bass("TRN2", target_bir_lowering=False)

    def din(name, shape, dt=F32):
        return nc.dram_tensor(name, list(shape), dt, kind="ExternalInput").ap()

    def dscr(name, shape, dt=F32):
        kind = "ExternalOutput" if dbg else "Internal"
        return nc.dram_tensor(name, list(shape), dt, kind=kind).ap()

    x_d = din("x", [NS, S, D])
    mem_d = din("mem", [NS, MEM, D])
    posT_d = din("posT", [128, NTT], I32)
    ln_mix = din("ln_mix", [2, D]); w_in = din("w_in", [1, D, IN_DIM])
    q_lat_norm = din("q_lat_norm", [1, QL]); w_uq = din("w_uq", [1, QL, AH * QK])
    kv_lat_norm = din("kv_lat_norm", [1, KVL]); w_ukv = din("w_ukv", [1, KVL, AH * (NOPE + VD)])
    q_norm = din("q_norm", [1, QK]); k_norm = din("k_norm", [1, QK])
    conv_wl = din("conv_wl", [128, 8, CK]); conv_bl = din("conv_bl", [128, 8])
    dt_bias = din("dt_bias", [1, BH]); a_log = din("a_log", [1, BH]); d_skip = din("d_skip", [1, BH])
    ssd_norm = din("ssd_norm", [1, DI]); w_out = din("w_out", [1, D, D])
    pool_w = din("pool_w", [1, 4, 256, 256]); pool_b = din("pool_b", [1, D]); pool_scale = din("pool_scale", [1, D])
    ln_xq = din("ln_xq", [2, D]); ln_mem = din("ln_mem", [2, D])
    xq_w = din("xq_w", [2, D, XH * XD]); xkv_w = din("xkv_w", [2, D, 2 * XH * XD])
    xq_norm = din("xq_norm", [2, XD]); xk_norm = din("xk_norm", [2, XD]); xo_w = din("xo_w", [2, XH * XD, D])
    ln_ffn = din("ln_ffn", [2, D]); rg_w = din("rg_w", [2, D, 4]); rg_b = din("rg_b", [2, 4])
    re_w = din("re_w", [2, D, NEXP]); re_b = din("re_b", [2, NEXP])
    c_ident = din("c_ident", [128, 128]); c_ut = din("c_ut", [128, 128]); c_ls = din("c_ls", [128, 128])
    c_invf = din("c_invf", [ROPE // 2]); c_icnt = din("c_icnt", [4, 512])
    NTOK = NS * S
    BLK = 512
    NBLK = (2 * NTOK) // BLK + NEXP
    NROWS = NBLK * BLK
    NTI = NTOK // 128
    c_uts = din("c_uts", [128, 128]); c_thr = din("c_thr", [16]); c_jidx = din("c_jidx", [NBLK]); c_iota = din("c_iota", [128, 1])
    ewg_l = [din("ewg_l%d" % i, [NEXP * 128, 2048]) for i in range(2)]
    ewu_l = [din("ewu_l%d" % i, [NEXP * 128, 2048]) for i in range(2)]
    ewd_l = [din("ewd_l%d" % i, [NEXP * 128, 2048]) for i in range(2)]
    Hn_d = nc.dram_tensor("Hn_s", [NTOK, D], BF16, kind="Internal").ap()
    Xs_l = [nc.dram_tensor("Xs_s%d" % i, [NROWS, D], BF16, kind="Internal").ap() for i in range(2)]
    Ys_d = nc.dram_tensor("Ys_s", [NROWS, D], BF16, kind="Internal").ap()

    out_d = nc.dram_tensor("out", [NS, S, D], F32, kind="ExternalOutput").ap()
    qT_d = dscr("qT_s", [NS, AH, QK, S], BF16)
    kT_d = dscr("kT_s", [NS, AH, QK, S], BF16)
    v_d = dscr("v_s", [NS, S, AH * VD], BF16)
    yT_d = dscr("yT_s", [NS, 4, 128, S], BF16)
    aT_d = dscr("aT_s", [NS, AH * VD, S], BF16)
    dbg_out = {}
    if dbg:
        for nm in ("mix0", "xa0", "moe0", "mix1", "xa1"):
            dbg_out[nm] = nc.dram_tensor("dbg_" + nm, [NS, S, D], F32, kind="ExternalOutput").ap()

    def okey(b, tile):
        return ("out", b, tile)

    with contextlib.ExitStack() as gst:
        P = Prog(nc)
        P.stack = gst
        banks = [gst.enter_context(nc.psum_tensor("pb%d" % i, [128, 512], F32)) for i in range(8)]
        bank_cfg = {"sets": [list(range(8))], "acc": [[6, 7]]}
        bank_i = {}
        acc_i = {}

        def set_banks(sets, acc):
            bank_cfg["sets"] = sets
            bank_cfg["acc"] = acc

        def bank():
            sidx = P.stream if P.stream < len(bank_cfg["sets"]) else 0
            ids = bank_cfg["sets"][sidx]
            k = bank_i.get(sidx, 0)
            bank_i[sidx] = k + 1
            return banks[ids[k % len(ids)]]

        def accbank():
            sidx = P.stream if P.stream < len(bank_cfg["acc"]) else 0
            ids = bank_cfg["acc"][sidx]
            k = acc_i.get(sidx, 0)
            acc_i[sidx] = k + 1
            return banks[ids[k % len(ids)]]

        identf = P.sb("identf", [128, 128]); identb = P.sb("identb", [128, 128], BF16)
        utf = P.sb("utf", [128, 128]); utb = P.sb("utb", [128, 128], BF16)
        lsf = P.sb("lsf", [128, 128]); onesf = P.sb("onesf", [128, 128])
        epsb = P.sb("epsb", [128, 1])
        cosT = P.sb("cosT", [128, NTT, 16]); sinT = P.sb("sinT", [128, NTT, 16])
        P.dma(identf[:], c_ident); P.dma(utf[:], c_ut); P.dma(lsf[:], c_ls)
        P.copy(identb[:], identf[:]); P.copy(utb[:], utf[:])
        P.memset(onesf[:], 1.0); P.memset(epsb[:], EPS)

        small = SRot(P, "small", [128, 128], F32)
        junk = SRot(P, "junk", [128, 1024], F32)
        junkb = SRot(P, "junkb", [128, 1024], BF16)

        def stage_pools(nstreams=1, nsmall=28, need_junk=False):
            small.setup(nstreams, nsmall)
            junkb.setup(nstreams, 1)
            if need_junk:
                junk.setup(nstreams, 1)

        def rstd_from_ss(ss, n, inv_d):
            r = small.get()
            r2 = small.get()
            if RSTD_LNEXP:
                P.actf(r[:, 0:n], ss, AF.Ln, bias=epsb[:, 0:1], scale=inv_d)
                P.actf(r2[:, 0:n], r[:, 0:n], AF.Exp, scale=-0.5)
            else:
                P.actf(r[:, 0:n], ss, AF.Sqrt, bias=epsb[:, 0:1], scale=inv_d)
                P.recip(r2[:, 0:n], r[:, 0:n])
            return r2[:, 0:n]

        def silu_exp(out_ap, x_ap, tmp_pool, n):
            e = tmp_pool.get()
            P.actf(e[:, 0:n], x_ap, AF.Exp, scale=-1.0)
            P.ts(e[:, 0:n], e[:, 0:n], 1.0, None, ALU.add)
            P.recip(e[:, 0:n], e[:, 0:n])
            P.tt(out_ap, x_ap, e[:, 0:n], ALU.mult)

        def rmsnorm_full(x_ap, dd, gain_bc, out_ap):
            ss = small.get()
            j = junkb.get()
            P.actf(j[:, 0:dd], x_ap, AF.Square, accum_out=ss[:, 0:1])
            r = rstd_from_ss(ss[:, 0:1], 1, 1.0 / dd)
            P.stt(out_ap, x_ap, r[:, 0:1], gain_bc, ALU.mult, ALU.mult)

        def transpose_to(in_ap, nchunk, csz, out_ap, dt=BF16, evac="dve"):
            done = 0
            per = (1024 if dt == BF16 else 512) // 128
            while done < nchunk:
                n = min(per, nchunk - done)
                bk = bank()
                bv = bk[:].bitcast(BF16) if dt == BF16 else bk[:]
                idn = identb if dt == BF16 else identf
                for c in range(n):
                    P.tr(bv[0:csz, c * 128:(c + 1) * 128], in_ap[:, (done + c) * csz:(done + c + 1) * csz], idn[:])
                src = bv[0:csz, 0:n * 128].rearrange("p (a c) -> p a c", a=n)
                P.copy(out_ap[:, done:done + n, :], src, eng=evac)
                done += n

        def load_bc(name, vec_ap, n, eng_q="sp"):
            t = P.sb(name, [128, n])
            P.dma(t[:], vec_ap.partition_broadcast(128), queue=eng_q)
            return t

        tmpst = contextlib.ExitStack()
        P.stack = tmpst
        posi = P.sb("posi", [128, NTT], I32); posf = P.sb("posf", [128, NTT])
        invf = load_bc("invf", c_invf, 16)
        P.dma(posi[:], posT_d)
        P.copy(posf[:], posi[:])
        ang = P.sb("ang", [128, NTT, 16]); a1 = P.sb("ang1", [128, NTT, 16]); ki = P.sb("angk", [128, NTT, 16], I32)
        kf = P.sb("angkf", [128, NTT, 16])
        P.tt(ang[:], bc_last(posf[:], 16), bc_mid(invf[:], NTT), ALU.mult)
        TWO_PI = float(2 * np.pi)

        def sin_of(dst, shift):
            P.ts(a1[:], ang[:], shift, 1.0 / TWO_PI, ALU.add, ALU.mult)
            P.copy(ki[:], a1[:])
            P.copy(kf[:], ki[:])
            P.ts(a1[:], ang[:], shift, None, ALU.add)
            P.stt(a1[:], kf[:], -TWO_PI, a1[:], ALU.mult, ALU.add)
            P.ts(kf[:], a1[:], float(np.pi), -TWO_PI, ALU.is_gt, ALU.mult)
            P.tt(a1[:], a1[:], kf[:], ALU.add)
            P.ts(kf[:], a1[:], float(-np.pi), TWO_PI, ALU.is_lt, ALU.mult)
            P.tt(a1[:], a1[:], kf[:], ALU.add)
            P.actf(dst, a1[:], AF.Sin)

        sin_of(sinT[:], 0.0)
        sin_of(cosT[:], float(np.pi / 2))
        P.barrier(new_sems=False)
        P.emit()
        tmpst.close()
        P.stack = gst

        def rope(u_ap, tile_idx, o1, o2, nh):
            u1 = u_ap[:, :, 0:16]; u2 = u_ap[:, :, 16:32]
            cb = bc_mid(cosT[:, tile_idx, :], nh); sb_ = bc_mid(sinT[:, tile_idx, :], nh)
            ta = small.get(); tb = small.get()
            tav = ta[:, 0:nh * 16].rearrange("p (h f) -> p h f", h=nh)
            tbv = tb[:, 0:nh * 16].rearrange("p (h f) -> p h f", h=nh)
            P.tt(tav, u1, cb, ALU.mult); P.tt(tbv, u2, sb_, ALU.mult)
            P.tt(o1, tav, tbv, ALU.subtract)
            tc_ = small.get(); td = small.get()
            tcv = tc_[:, 0:nh * 16].rearrange("p (h f) -> p h f", h=nh)
            tdv = td[:, 0:nh * 16].rearrange("p (h f) -> p h f", h=nh)
            P.tt(tcv, u2, cb, ALU.mult); P.tt(tdv, u1, sb_, ALU.mult)
            P.tt(o2, tcv, tdv, ALU.add)

        def softmax_finalize(ps_o, ncols, out_bf, P_osb, P_rl):
            osb = P_osb.get()
            P.copy(osb[0:65, 0:ncols], ps_o[0:65, 0:ncols], eng="act")
            rl = P_rl.get()
            P.recip(rl[64:65, 0:ncols], osb[64:65, 0:ncols])
            pb = bank()
            P.mm(pb[0:64, 0:ncols], onesf[64:65, 0:64], rl[64:65, 0:ncols])
            P.tt(out_bf, osb[0:64, 0:ncols], pb[0:64, 0:ncols], ALU.mult)


        def dbg_dump(name):
            if not dbg:
                return
            P.barrier(new_sems=False)
            for b in range(NS):
                P.dma(dbg_out[name][b], out_d[b], reads=["out_all"], writes=["dbg_" + name])
            P.barrier(new_sems=False)

        dumped = set()

        def dump(name, ap, dt=F32):
            if not dbg or name in dumped:
                return
            dumped.add(name)
            d = nc.dram_tensor("dmp_" + name, list(ap.shape), dt, kind="ExternalOutput").ap()
            P.dma(d, ap, queue="sp")

        def stage_end():
            P.barrier()
            P.emit()

        run = (lambda s: True) if stages is None else (lambda s: s in stages)

        if run("A"):
          with contextlib.ExitStack() as st:
            P.stack = st
            stage_pools(3, 9, need_junk=True)
            set_banks([[0, 1, 2], [3, 4], [5, 6, 7]], [[6, 7]])
            w_in_sb = P.sb("w_in_sb", [128, 8, IN_DIM], BF16)
            w_in_v = w_in[0].rearrange("(kc p) f -> p kc f", p=128)
            P.dma(w_in_sb[:, :, 0:416], w_in_v[:, :, 0:416], queue="pool")
            P.dma(w_in_sb[:, :, 416:424], w_in_v[:, :, 1952:1960], queue="pool")
            P.dma(w_in_sb[:, :, 424:1960], w_in_v[:, :, 416:1952], queue="pool")
            w_uq_sb = P.sb("w_uq_sb", [128, 2, AH * QK], BF16)
            P.dma(w_uq_sb[:], w_uq[0].rearrange("(kc p) f -> p kc f", p=128), queue="pool")
            w_ukv_sb = P.sb("w_ukv_sb", [128, 1024], BF16)
            P.dma(w_ukv_sb[:], w_ukv[0], queue="pool")
            g_mix = load_bc("g_mix", ln_mix[0], D)
            g_ql = load_bc("g_ql", q_lat_norm[0], QL); g_kvl = load_bc("g_kvl", kv_lat_norm[0], KVL)
            g_q = load_bc("g_q", q_norm[0], QK); g_k = load_bc("g_k", k_norm[0], QK)
            dtb = load_bc("dtb", dt_bias[0], BH); alog = load_bc("alog", a_log[0], BH)
            dsk8 = load_bc("dsk8", d_skip[0], BH); g_ssd = load_bc("g_ssd", ssd_norm[0], DI)
            abc = P.sb("abc", [128, BH])
            P.actf(abc[:], alog[:], AF.Exp)
            P.ts(abc[:], abc[:], -1.0, None, ALU.mult)
            cw = P.sb("cw", [128, 8, CK]); cb_ = P.sb("cb", [128, 8])
            P.dma(cw[:], conv_wl); P.dma(cb_[:], conv_bl)

            xt_r = Rot(P, "xt", [128, D], F32, 2)
            hb_r = Rot(P, "hb", [128, D], BF16, 1)
            hT_r = Rot(P, "hT", [128, 8, 512], BF16, 1)
            U = [P.sb("U%d" % c, [128, 515], F32) for c in range(8)]
            cacc_r = Rot(P, "cacc", [128, 512], F32, 1)
            xbcT_r = [Rot(P, "xbcT%d" % c, [128, 512], BF16, 1) for c in range(8)]
            qT_g_r = Rot(P, "qT_g", [96, AH, 512], BF16, 1)
            kT_g_r = Rot(P, "kT_g", [96, AH, 512], BF16, 1)
            v_g_r = Rot(P, "v_g", [128, 4, AH * VD], BF16, 1)
            yT_g_r = Rot(P, "yT_g", [128, 4, 512], BF16, 1)
            Sf = P.sb("Sf", [128, 512], F32); Sb = P.sb("Sb", [128, 512], BF16)
            f512 = SplitRot([Rot(P, "f512q", [128, 512], F32, 1), Rot(P, "f512kv", [128, 512], F32, 2), Rot(P, "f512s", [128, 512], F32, 3)], P)
            pa_r = Rot(P, "pa", [128, 424], F32, 2)
            se_r = Rot(P, "se", [128, 512], F32, 2) if SILU_EXP else None
            zs_r = Rot(P, "zs", [128, 512], F32, 2)
            b512 = SplitRot([Rot(P, "b512q", [128, 512], BF16, 2), Rot(P, "b512kv", [128, 512], BF16, 2), Rot(P, "b512s", [128, 512], BF16, 5)], P)
            f768 = Rot(P, "f768", [128, 768], F32, 2)
            f1k = Rot(P, "f1k", [128, 1024], F32, 1)
            b768 = SplitRot([Rot(P, "b768q", [128, 768], BF16, 1), Rot(P, "b768kv", [128, 768], BF16, 1)], P)
            lhs_r = Rot(P, "lhsh", [128, 128], F32, 4)
            dec_r = Rot(P, "dec", [128, 4, 128], F32, 1)
            MT_r = Rot(P, "MT", [128, 8, 128], BF16, 2)
            ps_keep = {}

            ASTOP = int(os.environ.get("A_STOP", "99"))

            class _Stop(Exception):
                pass

            def stop_if(k):
                if ASTOP == k:
                    raise _Stop()

            try:
              for b in range(NS):
                  for c in range(8):
                      P.memset(U[c][:, 0:3], 0.0)
                  P.memset(Sf[:], 0.0); P.memset(Sb[:], 0.0)
                  for g in range(NG):
                      hT = hT_r.get()
                      qT_g = qT_g_r.get(); kT_g = kT_g_r.get(); v_g = v_g_r.get(); yT_g = yT_g_r.get()
                      for t in range(4):
                          tile = g * 4 + t
                          xt = xt_r.get()
                          P.dma(xt[:], x_d[b, tile * 128:(tile + 1) * 128, :])
                          hb = hb_r.get()
                          rmsnorm_full(xt[:], D, g_mix[:], hb[:])
                          transpose_to(hb[:], 8, 128, hT[:, :, t * 128:(t + 1) * 128])
                      xbcT = []
                      for fc in range(8):
                          bk = bank()
                          for kc in range(8):
                              P.mm(bk[:, :], w_in_sb[:, kc, 936 + fc * 128:936 + (fc + 1) * 128], hT[:, kc, :],
                                   start=(kc == 0), stop=(kc == 7))
                          P.copy(U[fc][:, 3:515], bk[:, :], eng="act")
                          ca = cacc_r.get()
                          P.ts(ca[:], U[fc][:, 3:515], cw[:, fc, 3:4], None, ALU.mult)
                          for k in (2, 1, 0):
                              P.stt(ca[:], U[fc][:, k:k + 512], cw[:, fc, k:k + 1], ca[:], ALU.mult, ALU.add)
                          P.copy(U[fc][:, 0:3], U[fc][:, 512:515], eng="pool")
                          xo = xbcT_r[fc].get()
                          if SILU_EXP:
                              P.ts(ca[:], ca[:], cb_[:, fc:fc + 1], None, ALU.add)
                              silu_exp(xo[:], ca[:], se_r, 512)
                          else:
                              P.actf(xo[:], ca[:], AF.Silu, bias=cb_[:, fc:fc + 1])
                          xbcT.append(xo)
                          dump("xbcT%d" % fc, xo[:], BF16)
                      for t in range(4):
                          tile = g * 4 + t
                          gt = b * NT + tile
                          tsl = slice(t * 128, (t + 1) * 128)
                          ps_a = bank()
                          for kc in range(8):
                              P.mm(ps_a[:, 0:424], hT[:, kc, tsl], w_in_sb[:, kc, 0:424], start=(kc == 0), stop=(kc == 7))
                          ps_z = bank()
                          for kc in range(8):
                              P.mm(ps_z[:, :], hT[:, kc, tsl], w_in_sb[:, kc, 424:936], start=(kc == 0), stop=(kc == 7))
                          pa = pa_r.get()
                          P.copy(pa[:, 0:424], ps_a[:, 0:424], eng="act")
                          zs = zs_r.get()
                          if SILU_EXP:
                              silu_exp(zs[:], ps_z[:, :], se_r, 512)
                          else:
                              P.actf(zs[:], ps_z[:, :], AF.Silu)
                          def q_path():
                              qlb = b512.get()
                              rmsnorm_full(pa[:, 0:256], QL, g_ql[:], qlb[:, 0:256])
                              qlT = b512.get()
                              transpose_to(qlb[:, 0:256], 2, 128, qlT[:, 0:256].rearrange("p (a c) -> p a c", a=2))
                              q_sb = f768.get()
                              bq0 = bank(); bq1 = bank()
                              for kc in range(2):
                                  P.mm(bq0[:, :], qlT[:, kc * 128:(kc + 1) * 128], w_uq_sb[:, kc, 0:512], start=(kc == 0), stop=(kc == 1))
                              for kc in range(2):
                                  P.mm(bq1[:, 0:256], qlT[:, kc * 128:(kc + 1) * 128], w_uq_sb[:, kc, 512:768], start=(kc == 0), stop=(kc == 1))
                              P.copy(q_sb[:, 0:512], bq0[:, :], eng="act")
                              P.copy(q_sb[:, 512:768], bq1[:, 0:256], eng="act")
                              sq = junk.get()
                              P.actf(sq[:, 0:768], q_sb[:], AF.Square)
                              ssq = small.get()
                              P.red(ssq[:, 0:8], sq[:, 0:768].rearrange("p (h f) -> p h f", h=8), ALU.add)
                              rq = rstd_from_ss(ssq[:, 0:8], 8, 1.0 / QK)
                              qn = f768.get()
                              q3 = q_sb[:].rearrange("p (h f) -> p h f", h=8)
                              qn3 = qn[:].rearrange("p (h f) -> p h f", h=8)
                              P.tt(qn3, q3, bc_last(rq, QK), ALU.mult)
                              P.tt(qn3, qn3, bc_mid(g_q[:], 8), ALU.mult)
                              qf = b768.get()
                              qf3 = qf[:].rearrange("p (h f) -> p h f", h=8)
                              P.copy(qf3[:, :, 0:64], qn3[:, :, 0:64], eng=os.environ.get("MK_CAST", "act"))
                              rope(qn3[:, :, 64:96], gt, qf3[:, :, 64:80], qf3[:, :, 80:96], 8)
                              transpose_to(qf[:], 8, 96, qT_g[:, :, tsl])

                          def kv_path():
                              kvb = b512.get()
                              rmsnorm_full(pa[:, 256:384], KVL, g_kvl[:], kvb[:, 0:128])
                              kvT = b512.get()
                              transpose_to(kvb[:, 0:128], 1, 128, kvT[:, 0:128].rearrange("p (a c) -> p a c", a=1))
                              kv_sb = f1k.get()
                              for hf in range(2):
                                  bkv = bank()
                                  P.mm(bkv[:, :], kvT[:, 0:128], w_ukv_sb[:, hf * 512:(hf + 1) * 512])
                                  P.copy(kv_sb[:, hf * 512:(hf + 1) * 512], bkv[:, :], eng="act")
                              kv3 = kv_sb[:].rearrange("p (h f) -> p h f", h=8)
                              sqk = junk.get()
                              P.actf(sqk[:], kv_sb[:], AF.Square)
                              ssk = small.get()
                              P.red(ssk[:, 0:8], sqk[:].rearrange("p (h f) -> p h f", h=8)[:, :, 0:64], ALU.add)
                              ssr = small.get()
                              jr = small.get()
                              P.actf(jr[:, 0:32], pa[:, 384:416], AF.Square, accum_out=ssr[:, 0:1])
                              P.ts(ssk[:, 0:8], ssk[:, 0:8], ssr[:, 0:1], None, ALU.add)
                              rk = rstd_from_ss(ssk[:, 0:8], 8, 1.0 / QK)
                              kf_ = b768.get()
                              kf3 = kf_[:].rearrange("p (h f) -> p h f", h=8)
                              kn = f512.get()
                              kn3 = kn[:].rearrange("p (h f) -> p h f", h=8)
                              P.tt(kn3, kv3[:, :, 0:64], bc_last(rk, 64), ALU.mult)
                              P.tt(kf3[:, :, 0:64], kn3, bc_mid(g_k[:, 0:64], 8), ALU.mult)
                              krg = small.get()
                              P.tt(krg[:, 0:32], pa[:, 384:416], g_k[:, 64:96], ALU.mult)
                              kr = f512.get()
                              kr3 = kr[:, 0:256].rearrange("p (h f) -> p h f", h=8)
                              P.tt(kr3, bc_mid(krg[:, 0:32], 8), bc_last(rk, 32), ALU.mult)
                              rope(kr3, gt, kf3[:, :, 64:80], kf3[:, :, 80:96], 8)
                              transpose_to(kf_[:], 8, 96, kT_g[:, :, tsl])
                              P.copy(v_g[:, t, :].rearrange("p (h f) -> p h f", h=8), kv3[:, :, 64:128], eng=os.environ.get("MK_CAST", "act"))

                          def ssd_path():
                              xs_tm = b512.get(); B_tm = b512.get()
                              bk = bank(); bv = bk[:].bitcast(BF16)
                              for c in range(4):
                                  P.tr(bv[:, c * 128:(c + 1) * 128], xbcT[c][:, tsl], identb[:])
                              for c in range(2):
                                  P.tr(bv[:, 512 + c * 128:512 + (c + 1) * 128], xbcT[4 + c][:, tsl], identb[:])
                              P.copy(xs_tm[:], bv[:, 0:512])
                              P.copy(B_tm[:, 0:256], bv[:, 512:768])
                              dtr = small.get(); dte_ = small.get(); dtv = small.get(); adt = small.get()
                              P.tt(dtr[:, 0:8], pa[:, 416:424], dtb[:], ALU.add)
                              P.actf(dte_[:, 0:8], dtr[:, 0:8], AF.Exp)
                              P.actf(dtv[:, 0:8], dte_[:, 0:8], AF.Ln, bias=1.0)
                              P.tt(adt[:, 0:8], dtv[:, 0:8], abc[:], ALU.mult)
                              dump("dtv_%d" % t, dtv[:, 0:8]); dump("xs_tm_%d" % t, xs_tm[:], BF16)
                              ps_c = bank()
                              P.mm(ps_c[:, 0:8], utf[:], adt[:, 0:8])
                              P.mm(ps_c[:, 8:16], onesf[:], adt[:, 0:8])
                              ct = small.get()
                              P.copy(ct[:, 0:16], ps_c[:, 0:16])
                              acs = ct[:, 0:8]; tot = ct[:, 8:16]
                              MT = MT_r.get()
                              ps_cb = bank()
                              for gr in range(2):
                                  P.mm(ps_cb[:, gr * 128:(gr + 1) * 128], xbcT[4 + gr][:, tsl], xbcT[6 + gr][:, tsl])
                              cbm = f512.get()
                              cbm3 = cbm[:, 0:256].rearrange("p (g t) -> p g t", g=2)
                              P.tt(cbm3, ps_cb[:, 0:256].rearrange("p (g t) -> p g t", g=2), bc_mid(utf[:], 2), ALU.mult)
                              for gr in range(2):
                                  ps_d = bank()
                                  for hh in range(4):
                                      h = gr * 4 + hh
                                      lh = lhs_r.get()
                                      P.ts(lh[:], lsf[:], adt[:, h:h + 1], None, ALU.mult, eng=os.environ.get("MK_LH", "dve"))
                                      P.mm(ps_d[:, hh * 128:(hh + 1) * 128], lh[:], utf[:])
                                  dec = dec_r.get()
                                  P.actf(dec[:].rearrange("p a b -> p (a b)"), ps_d[:, :], AF.Exp)
                                  P.tt(MT[:, gr * 4:(gr + 1) * 4, :], dec[:], bc_mid(cbm3[:, gr, :], 4), ALU.mult)
                              xdt = b512.get()
                              P.tt(xdt[:].rearrange("p (h f) -> p h f", h=8), xs_tm[:].rearrange("p (h f) -> p h f", h=8),
                                   bc_last(dtv[:, 0:8], 64), ALU.mult)
                              e3 = small.get()
                              P.tt(e3[:, 0:8], tot, acs, ALU.subtract)
                              P.actf(e3[:, 0:8], e3[:, 0:8], AF.Exp)
                              P.actf(e3[:, 8:16], acs, AF.Exp)
                              P.actf(e3[:, 16:24], tot, AF.Exp)
                              xdte = b512.get()
                              P.tt(xdte[:].rearrange("p (h f) -> p h f", h=8), xdt[:].rearrange("p (h f) -> p h f", h=8),
                                   bc_last(e3[:, 0:8], 64), ALU.mult, eng="pool")
                              ps_yo = bank()
                              for gr in range(2):
                                  P.mm(ps_yo[:, gr * 256:(gr + 1) * 256], xbcT[6 + gr][:, tsl], Sb[:, gr * 256:(gr + 1) * 256])
                              ps_yd = bank()
                              for h in range(8):
                                  P.mm(ps_yd[:, h * 64:(h + 1) * 64], MT[:, h, :], xdt[:, h * 64:(h + 1) * 64])
                              y1 = f512.get()
                              P.tt(y1[:].rearrange("p (h f) -> p h f", h=8), ps_yo[:, :].rearrange("p (h f) -> p h f", h=8),
                                   bc_last(e3[:, 8:16], 64), ALU.mult)
                              P.tt(y1[:], y1[:], ps_yd[:, :], ALU.add)
                              y2 = f512.get()
                              P.tt(y2[:].rearrange("p (h f) -> p h f", h=8), xs_tm[:].rearrange("p (h f) -> p h f", h=8),
                                   bc_last(dsk8[:], 64), ALU.mult, eng="pool")
                              P.tt(y1[:], y1[:], y2[:], ALU.add)
                              dump("ydiag_%d" % t, ps_yd[:, :]) if False else None
                              dump("y1_%d" % t, y1[:]); dump("acs_%d" % t, ct[:, 0:16]); dump("MT_%d" % t, MT[:], BF16)
                              ps_s = bank()
                              for gr in range(2):
                                  P.mm(ps_s[:, gr * 256:(gr + 1) * 256], B_tm[:, gr * 128:(gr + 1) * 128], xdte[:, gr * 256:(gr + 1) * 256])
                              P.tt(Sf[:].rearrange("p (h f) -> p h f", h=8), Sf[:].rearrange("p (h f) -> p h f", h=8),
                                   bc_last(e3[:, 16:24], 64), ALU.mult)
                              P.tt(Sf[:], Sf[:], ps_s[:, :], ALU.add)
                              P.copy(Sb[:], Sf[:], eng="act")
                              P.tt(y1[:], y1[:], zs[:], ALU.mult)
                              ynb = b512.get()
                              rmsnorm_full(y1[:], DI, g_ssd[:], ynb[:])
                              transpose_to(ynb[:], 4, 128, yT_g[:, :, tsl])

                          run_streams(P, [q_path, kv_path, ssd_path])
                      gs = slice(g * 512, (g + 1) * 512)
                      P.dma(qT_d[b, :, :, gs].rearrange("h d s -> d h s"), qT_g[:], queue="pool", writes=[("qT", b)])
                      P.dma(kT_d[b, :, :, gs].rearrange("h d s -> d h s"), kT_g[:], queue="pool", writes=[("kT", b)])
                      P.dma(v_d[b, gs, :].rearrange("(t p) f -> p t f", p=128), v_g[:], queue="pool", writes=[("v", b)])
                      P.dma(yT_d[b, :, :, gs].rearrange("c p s -> p c s"), yT_g[:], queue="pool", writes=[("yT", b)])
            except _Stop:
                pass
            set_banks([list(range(8))], [[6, 7]])
            stage_end()

        if run("B"):
          with contextlib.ExitStack() as st:
            P.stack = st
            ztile = P.sb("ztile", [128, D], BF16)
            P.memset(ztile[:], 0.0)
            zch = 2048 if NROWS % 2048 == 0 else 512
            for li in range(2):
                for c in range(NROWS // zch):
                    P.dma(Xs_l[li][c * zch:(c + 1) * zch, :].rearrange("(n p) d -> p n d", p=128),
                          ztile[:].unsqueeze(1).to_broadcast([128, zch // 128, D]), queue="act", writes=[("Xsz", li, c)])
            def body(b):
                kT_r = Rot(P, "kTh", [96, S], BF16, 2); qT_r = Rot(P, "qTh", [96, S], BF16, 2)
                Vx_r = Rot(P, "Vx", [128, NT, 65], BF16, 2)
                for t_ in Vx_r.t:
                    P.memset(t_[:, :, 64:65], 1.0)
                pt_r = Rot(P, "pt", [128, 512], BF16, 4)
                at_r = Rot(P, "at", [64, 512], BF16, 2)
                osb_r = Rot(P, "osb", [128, 512], F32, 2); rl_r = Rot(P, "rl", [128, 512], F32, 2)
                scale = float(QK ** -0.5)
                if True:
                    for h in range(AH):
                        kT = kT_r.get(); qT = qT_r.get(); Vx = Vx_r.get()
                        P.dma(kT[:], kT_d[b, h], reads=[("kT", b)])
                        P.dma(qT[:], qT_d[b, h], reads=[("qT", b)])
                        P.dma(Vx[:, :, 0:64], v_d[b, :, h * 64:(h + 1) * 64].rearrange("(n p) d -> p n d", p=128),
                              reads=[("v", b)])
                        for qg in range(NG):
                            ps_o = accbank()
                            nkb = 4 * qg + 4
                            SKEW = 2
                            pend = {}
                            for kk in range(nkb + SKEW):
                                if kk < nkb:
                                    kb = kk
                                    i = kb - 4 * qg
                                    q0 = 128 * i if i > 0 else 0
                                    ps_s = bank()
                                    P.mm(ps_s[:, q0:512], kT[:, kb * 128:(kb + 1) * 128], qT[:, qg * 512 + q0:(qg + 1) * 512])
                                    pend[kb] = (ps_s, q0, i)
                                kb = kk - SKEW
                                if kb >= 0:
                                    ps_s, q0, i = pend.pop(kb)
                                    pt = pt_r.get()
                                    P.actf(pt[:, q0:512], ps_s[:, q0:512], AF.Exp, scale=scale)
                                    if q0 > 0:
                                        P.memset(pt[:, 0:q0], 0.0)
                                    if i >= 0:
                                        P.tt(pt[:, q0:q0 + 128], pt[:, q0:q0 + 128], utb[:], ALU.mult, eng="pool")
                                    P.mm(ps_o[0:65, :], Vx[:, kb, :], pt[:, :], start=(kb == 0), stop=(kb == nkb - 1))
                            at = at_r.get()
                            softmax_finalize(ps_o, 512, at[:], osb_r, rl_r)
                            P.dma(aT_d[b, h * 64:(h + 1) * 64, qg * 512:(qg + 1) * 512], at[:], queue="pool",
                                  writes=[("aT", b)])

            stage_pools(NS)
            set_banks([[0, 1, 2], [4, 5, 6]] if NS > 1 else [list(range(6))], [[3], [7]] if NS > 1 else [[6, 7]])
            run_streams(P, [(lambda b=b: body(b)) for b in range(NS)])
            set_banks([list(range(8))], [[6, 7]])
            stage_end()

        if run("C"):
          with contextlib.ExitStack() as st:
            P.stack = st
            w_out_sb = P.sb("w_out_sb", [128, 8, D], BF16)
            P.dma(w_out_sb[:], w_out[0].rearrange("(kc p) f -> p kc f", p=128), queue="pool")
            def body(b):
                mixT_r = Rot(P, "mixT", [128, 8, 512], BF16, 2)
                xt_r = Rot(P, "xtc", [128, D], F32, 3)
                if True:
                    for g in range(NG):
                        mixT = mixT_r.get()
                        gs = slice(g * 512, (g + 1) * 512)
                        P.dma(mixT[:, 0:4, :], aT_d[b, :, gs].rearrange("(c p) s -> p c s", p=128), reads=[("aT", b)])
                        P.dma(mixT[:, 4:8, :], yT_d[b, :, :, gs].rearrange("c p s -> p c s"), reads=[("yT", b)])
                        for t in range(4):
                            tile = g * 4 + t
                            xt = xt_r.get()
                            P.dma(xt[:], x_d[b, tile * 128:(tile + 1) * 128, :])
                            for hf in range(2):
                                ps = bank()
                                for c in range(8):
                                    P.mm(ps[:, :], mixT[:, c, t * 128:(t + 1) * 128], w_out_sb[:, c, hf * 512:(hf + 1) * 512],
                                         start=(c == 0), stop=(c == 7))
                                P.tt(xt[:, hf * 512:(hf + 1) * 512], xt[:, hf * 512:(hf + 1) * 512], ps[:, :], ALU.add)
                            P.dma(out_d[b, tile * 128:(tile + 1) * 128, :], xt[:], queue="pool", writes=[okey(b, tile)])
            stage_pools(NS)
            set_banks([[0, 1, 2, 3], [4, 5, 6, 7]] if NS > 1 else [list(range(8))], [[6, 7]])
            run_streams(P, [(lambda b=b: body(b)) for b in range(NS)])
            set_banks([list(range(8))], [[6, 7]])
            dbg_dump("mix0")
            stage_end()

        def stage_xa(l):
          with contextlib.ExitStack() as st:
            P.stack = st
            xq_sb = P.sb("xq_sb", [128, 8, XH * XD], BF16)
            P.dma(xq_sb[:], xq_w[l].rearrange("(kc p) f -> p kc f", p=128), queue="pool")
            xkv_sb = P.sb("xkv_sb", [128, 8, 2 * XH * XD], BF16)
            P.dma(xkv_sb[:], xkv_w[l].rearrange("(kc p) f -> p kc f", p=128), queue="pool")
            xo_sb = P.sb("xo_sb", [64, XH, D], BF16)
            P.dma(xo_sb[:], xo_w[l].rearrange("(h p) f -> p h f", p=64), queue="pool")
            g_xq = load_bc("g_xq", ln_xq[l], D); g_mem = load_bc("g_mem", ln_mem[l], D)
            g_q = load_bc("g_xqn", xq_norm[l], XD); g_k = load_bc("g_xkn", xk_norm[l], XD)
            memk = [P.sb("memk%d" % b, [64, XH, MEM], BF16) for b in range(NS)]
            memV = [P.sb("memV%d" % b, [128, 2, XH, 65], BF16) for b in range(NS)]
            def body(b):
                xg_r = Rot(P, "xg", [128, D], F32, 5)
                hb_r = Rot(P, "hbx", [128, D], BF16, 2)
                hT_r = Rot(P, "hTx", [128, 8, 512], BF16, 1)
                mT = P.sb("mT", [128, 8, MEM], BF16)
                f512 = Rot(P, "f512x", [128, 512], F32, 3)
                b256 = Rot(P, "b256x", [128, 256], BF16, 3)
                qT_r = Rot(P, "qTx", [64, XH, 512], BF16, 1)
                oT_r = Rot(P, "oTx", [64, XH, 512], BF16, 1)
                pt_r = Rot(P, "ptx", [128, 512], BF16, 3)
                osb_r = Rot(P, "osbx", [128, 512], F32, 1); rl_r = Rot(P, "rlx", [128, 512], F32, 1)
                scale = float(XD ** -0.5)

                def head_norm(src_ap, g_bc, out_bf):
                    sq = f512.get()
                    P.actf(sq[:, 0:256], src_ap, AF.Square)
                    ss = small.get()
                    P.red(ss[:, 0:4], sq[:, 0:256].rearrange("p (h f) -> p h f", h=4), ALU.add)
                    r = rstd_from_ss(ss[:, 0:4], 4, 1.0 / XD)
                    qn = f512.get()
                    qn3 = qn[:, 0:256].rearrange("p (h f) -> p h f", h=4)
                    P.tt(qn3, src_ap.rearrange("p (h f) -> p h f", h=4), bc_last(r, XD), ALU.mult)
                    P.tt(out_bf.rearrange("p (h f) -> p h f", h=4), qn3, bc_mid(g_bc[:], 4), ALU.mult)

                if True:
                    P.memset(memV[b][:, :, :, 64:65], 1.0)
                    for mt in range(2):
                        xm = xg_r.get()
                        P.dma(xm[:], mem_d[b, mt * 128:(mt + 1) * 128, :])
                        mb = hb_r.get()
                        rmsnorm_full(xm[:], D, g_mem[:], mb[:])
                        transpose_to(mb[:], 8, 128, mT[:, :, mt * 128:(mt + 1) * 128])
                    for mt in range(2):
                        ps = bank()
                        for kc in range(8):
                            P.mm(ps[:, :], mT[:, kc, mt * 128:(mt + 1) * 128], xkv_sb[:, kc, :], start=(kc == 0), stop=(kc == 7))
                        kv = f512.get()
                        P.copy(kv[:], ps[:, :], eng="act")
                        knb = b256.get()
                        head_norm(kv[:, 0:256], g_k, knb[:])
                        transpose_to(knb[:], 4, 64, memk[b][:, :, mt * 128:(mt + 1) * 128])
                        P.copy(memV[b][:, mt, :, 0:64], kv[:, 256:512].rearrange("p (h f) -> p h f", h=4), eng="pool")

                if True:
                    for g in range(NG):
                        hT = hT_r.get()
                        xg = []
                        for t in range(4):
                            tile = g * 4 + t
                            x1 = xg_r.get()
                            P.dma(x1[:], out_d[b, tile * 128:(tile + 1) * 128, :], reads=[okey(b, tile)])
                            hb = hb_r.get()
                            rmsnorm_full(x1[:], D, g_xq[:], hb[:])
                            transpose_to(hb[:], 8, 128, hT[:, :, t * 128:(t + 1) * 128])
                            xg.append(x1)
                        qT = qT_r.get()
                        for t in range(4):
                            tsl = slice(t * 128, (t + 1) * 128)
                            ps_q = bank()
                            for kc in range(8):
                                P.mm(ps_q[:, 0:256], hT[:, kc, tsl], xq_sb[:, kc, :], start=(kc == 0), stop=(kc == 7))
                            q_sb = f512.get()
                            P.copy(q_sb[:, 0:256], ps_q[:, 0:256], eng="act")
                            qb = b256.get()
                            head_norm(q_sb[:, 0:256], g_q, qb[:])
                            transpose_to(qb[:], 4, 64, qT[:, :, tsl])
                        oT = oT_r.get()
                        for hh in range(XH):
                            ps_o = accbank()
                            for mt in range(2):
                                ps_s = bank()
                                P.mm(ps_s[:, :], memk[b][:, hh, mt * 128:(mt + 1) * 128], qT[:, hh, :])
                                pt = pt_r.get()
                                P.actf(pt[:], ps_s[:, :], AF.Exp, scale=scale)
                                P.mm(ps_o[0:65, :], memV[b][:, mt, hh, :], pt[:], start=(mt == 0), stop=(mt == 1))
                            softmax_finalize(ps_o, 512, oT[:, hh, :], osb_r, rl_r)
                        for t in range(4):
                            tile = g * 4 + t
                            tsl = slice(t * 128, (t + 1) * 128)
                            for hf in range(2):
                                ps = bank()
                                for hh in range(XH):
                                    P.mm(ps[:, :], oT[:, hh, tsl], xo_sb[:, hh, hf * 512:(hf + 1) * 512],
                                         start=(hh == 0), stop=(hh == XH - 1))
                                P.tt(xg[t][:, hf * 512:(hf + 1) * 512], xg[t][:, hf * 512:(hf + 1) * 512], ps[:, :], ALU.add)
                            P.dma(out_d[b, tile * 128:(tile + 1) * 128, :], xg[t][:], queue="pool", writes=[okey(b, tile)])
            stage_pools(NS, 20)
            set_banks([[0, 1, 2], [4, 5, 6]] if NS > 1 else [list(range(6))], [[3], [7]] if NS > 1 else [[6, 7]])
            run_streams(P, [(lambda b=b: body(b)) for b in range(NS)])
            set_banks([list(range(8))], [[6, 7]])
            dbg_dump("xa%d" % l)
            stage_end()

        def stage_moe(l):
          NTOK = NS * S
          SBT = min(2048, NTOK)
          nsb = NTOK // SBT
          tsb = SBT // 128
          gsb = SBT // 512
          BIG = 30000.0
          for sbi in range(nsb):
           with contextlib.ExitStack() as st:
            P.stack = st
            stage_pools()
            wr = P.sb("wr", [128, 8, 36], F32)
            P.dma(wr[:, :, 0:4], rg_w[l].rearrange("(kc p) f -> p kc f", p=128))
            P.dma(wr[:, :, 4:36], re_w[l].rearrange("(kc p) f -> p kc f", p=128))
            rb = P.sb("rb", [128, 36], F32)
            P.dma(rb[:, 0:4], rg_b[l].partition_broadcast(128))
            P.dma(rb[:, 4:36], re_b[l].partition_broadcast(128))
            g_ffn = load_bc("g_ffn", ln_ffn[l], D)
            acc = [P.sb("acc%d" % i, [128, D], F32) for i in range(tsb)]
            h2T = [P.sb("h2T%d" % i, [128, 8, 512], BF16) for i in range(gsb)]
            G = [P.sb("G%d" % i, [128, 32], F32) for i in range(tsb)]
            h32_r = Rot(P, "h32", [128, D], F32, 2)
            h32T_r = Rot(P, "h32T", [128, 8, 128], F32, 2)
            Wg_r = Rot(P, "Wg", [128, 8, EFF], BF16, 2); Wu_r = Rot(P, "Wu", [128, 8, EFF], BF16, 2)
            Wd_r = Rot(P, "Wd", [128, 2, D], BF16, 2)
            aT_r = Rot(P, "aTm", [128, 2, 512], BF16, 2)
            sg_r = Rot(P, "sgm", [128, 512], F32, 3)

            def tile_of(i):
                gtile = sbi * tsb + i
                return gtile // NT, gtile % NT

            for i in range(tsb):
                b, tile = tile_of(i)
                P.dma(acc[i][:], out_d[b, tile * 128:(tile + 1) * 128, :], reads=[okey(b, tile)])
                h32 = h32_r.get()
                rmsnorm_full(acc[i][:], D, g_ffn[:], h32[:])
                h32T = h32T_r.get()
                transpose_to(h32[:], 8, 128, h32T[:], dt=F32)
                P.copy(h2T[i // 4][:, :, (i % 4) * 128:(i % 4 + 1) * 128], h32T[:], eng="pool")
                ps_r = bank()
                for kc in range(8):
                    P.mm(ps_r[:, 0:36], h32T[:, kc, :], wr[:, kc, :], start=(kc == 0), stop=(kc == 7))
                lg = small.get()
                P.tt(lg[:, 0:36], ps_r[:, 0:36], rb[:], ALU.add)
                gmax = small.get(); P.red(gmax[:, 0:1], lg[:, 0:4], ALU.max)
                goh = small.get(); P.ts(goh[:, 0:4], lg[:, 0:4], gmax[:, 0:1], None, ALU.is_ge)
                ngmax = small.get(); P.ts(ngmax[:, 0:1], gmax[:, 0:1], -1.0, None, ALU.mult)
                gex = small.get(); gsum = small.get()
                P.actf(gex[:, 0:4], lg[:, 0:4], AF.Exp, bias=ngmax[:, 0:1], accum_out=gsum[:, 0:1])
                gp = small.get(); P.recip(gp[:, 0:1], gsum[:, 0:1])
                pen = small.get(); P.ts(pen[:, 0:4], goh[:, 0:4], -1.0, BIG, ALU.add, ALU.mult)
                elm = small.get()
                P.tt(elm[:, 0:32].rearrange("p (g e) -> p g e", g=4), lg[:, 4:36].rearrange("p (g e) -> p g e", g=4),
                     bc_last(pen[:, 0:4], 8), ALU.add)
                m1 = small.get(); P.red(m1[:, 0:1], elm[:, 0:32], ALU.max)
                oh1 = small.get(); P.ts(oh1[:, 0:32], elm[:, 0:32], m1[:, 0:1], None, ALU.is_ge)
                elm2 = small.get(); P.stt(elm2[:, 0:32], oh1[:, 0:32], -BIG, elm[:, 0:32], ALU.mult, ALU.add)
                m2 = small.get(); P.red(m2[:, 0:1], elm2[:, 0:32], ALU.max)
                sel = small.get(); P.ts(sel[:, 0:32], elm2[:, 0:32], m2[:, 0:1], None, ALU.is_ge)
                P.tt(sel[:, 0:32], sel[:, 0:32], oh1[:, 0:32], ALU.add)
                nm1 = small.get(); P.ts(nm1[:, 0:1], m1[:, 0:1], -1.0, None, ALU.mult)
                ex = small.get(); P.actf(ex[:, 0:32], elm[:, 0:32], AF.Exp, bias=nm1[:, 0:1])
                wv = small.get(); P.tt(wv[:, 0:32], ex[:, 0:32], sel[:, 0:32], ALU.mult)
                ws = small.get(); P.red(ws[:, 0:1], wv[:, 0:32], ALU.add)
                rws = small.get(); P.recip(rws[:, 0:1], ws[:, 0:1])
                coef = small.get(); P.tt(coef[:, 0:1], rws[:, 0:1], gp[:, 0:1], ALU.mult)
                P.ts(G[i][:], wv[:, 0:32], coef[:, 0:1], None, ALU.mult)
                dump("G_%d_%d" % (l, i), G[i][:])
            for e in range(NEXP):
                Wg = Wg_r.get(); Wu = Wu_r.get(); Wd = Wd_r.get()
                P.dma(Wg[:], ewg_l[l][e * 128:(e + 1) * 128, :].rearrange("p (kc f) -> p kc f", kc=8), queue="pool")
                P.dma(Wu[:], ewu_l[l][e * 128:(e + 1) * 128, :].rearrange("p (kc f) -> p kc f", kc=8), queue="pool")
                P.dma(Wd[:], ewd_l[l][e * 128:(e + 1) * 128, :].rearrange("p (c f) -> p c f", c=2), queue="pool")
                for gi in range(gsb):
                    pg = [bank(), bank()]
                    pu = [bank(), bank()]
                    for fc in range(2):
                        for kc in range(8):
                            P.mm(pg[fc][:, :], Wg[:, kc, fc * 128:(fc + 1) * 128], h2T[gi][:, kc, :], start=(kc == 0), stop=(kc == 7))
                        for kc in range(8):
                            P.mm(pu[fc][:, :], Wu[:, kc, fc * 128:(fc + 1) * 128], h2T[gi][:, kc, :], start=(kc == 0), stop=(kc == 7))
                    aT = aT_r.get()
                    for fc in range(2):
                        sg = sg_r.get()
                        P.actf(sg[:], pg[fc][:, :], AF.Silu)
                        P.tt(aT[:, fc, :], sg[:], pu[fc][:, :], ALU.mult)
                    for t in range(4):
                        i = gi * 4 + t
                        for hf in range(2):
                            pd = bank()
                            for fc in range(2):
                                P.mm(pd[:, :], aT[:, fc, t * 128:(t + 1) * 128], Wd[:, fc, hf * 512:(hf + 1) * 512],
                                     start=(fc == 0), stop=(fc == 1))
                            P.stt(acc[i][:, hf * 512:(hf + 1) * 512], pd[:, :], G[i][:, e:e + 1],
                                  acc[i][:, hf * 512:(hf + 1) * 512], ALU.mult, ALU.add)
            for i in range(tsb):
                b, tile = tile_of(i)
                P.dma(out_d[b, tile * 128:(tile + 1) * 128, :], acc[i][:], queue="pool", writes=[okey(b, tile)])
            if sbi == nsb - 1:
                dbg_dump("moe%d" % l) if l == 0 else None
            stage_end()

        def stage_pool():
          with contextlib.ExitStack() as st:
            P.stack = st
            pw_sb = P.sb("pw_sb", [128, 4, 2, 256], BF16)
            for cg in range(4):
                P.dma(pw_sb[:, cg, :, :], pool_w[0, cg].rearrange("(cc p) d -> p cc d", p=128), queue="pool")
            pb_bc = load_bc("pb_bc", pool_b[0], D); psc_bc = load_bc("psc_bc", pool_scale[0], D)
            g_m1 = load_bc("g_m1", ln_mix[1], D)
            icnt = load_bc("icnt", c_icnt.rearrange("a b -> (a b)"), 4 * 512)
            def body(b):
                H = P.sb("H", [128, 8, 527], F32)
                xg_r = Rot(P, "xgp", [128, D], F32, 5)
                hb_r = Rot(P, "hbp", [128, D], BF16, 2)
                lv_r = Rot(P, "lv", [128, 2, 527], F32, 3)
                dl_r = [Rot(P, "dl%d" % c, [128, 2, 512], BF16, 1) for c in range(4)]
                f512 = Rot(P, "f512p", [128, 512], F32, 3)
                if True:
                    P.memset(H[:, :, 0:15], 0.0)
                    for g in range(NG):
                        xg = []
                        for t in range(4):
                            tile = g * 4 + t
                            x1 = xg_r.get()
                            P.dma(x1[:], out_d[b, tile * 128:(tile + 1) * 128, :], reads=[okey(b, tile)])
                            hb = hb_r.get()
                            rmsnorm_full(x1[:], D, g_m1[:], hb[:])
                            transpose_to(hb[:], 8, 128, H[:, :, 15 + t * 128:15 + (t + 1) * 128])
                            xg.append(x1)
                        dl = []
                        for cg in range(4):
                            w = 2 ** (cg + 1)
                            cur = H[:, 2 * cg:2 * cg + 2, :]
                            for k in range(cg + 1):
                                sh = 2 ** k
                                lo = 2 * sh - 1
                                nx = lv_r.get()
                                P.tt(nx[:, :, lo:527], cur[:, :, lo:527], cur[:, :, lo - sh:527 - sh], ALU.add,
                                     eng=("pool" if k % 2 == 0 else "dve"))
                                cur = nx[:]
                            d_ = dl_r[cg].get()
                            if g == 0:
                                tmp = lv_r.get()
                                P.tt(tmp[:, :, 0:512], cur[:, :, 15:527], bc_mid(icnt[:, cg * 512:(cg + 1) * 512], 2), ALU.mult)
                                P.tt(d_[:], tmp[:, :, 0:512], H[:, 2 * cg:2 * cg + 2, 15:527], ALU.subtract)
                            else:
                                P.stt(d_[:], cur[:, :, 15:527], 1.0 / w, H[:, 2 * cg:2 * cg + 2, 15:527], ALU.mult, ALU.subtract)
                            dl.append(d_)
                        P.copy(H[:, :, 0:15], H[:, :, 512:527], eng="pool")
                        for t in range(4):
                            tile = g * 4 + t
                            tsl = slice(t * 128, (t + 1) * 128)
                            for hf in range(2):
                                ps = bank()
                                for c2 in range(2):
                                    cg = hf * 2 + c2
                                    for cc in range(2):
                                        P.mm(ps[:, c2 * 256:(c2 + 1) * 256], dl[cg][:, cc, tsl], pw_sb[:, cg, cc, :],
                                             start=(cc == 0), stop=(cc == 1))
                                tmp = f512.get()
                                hs = slice(hf * 512, (hf + 1) * 512)
                                P.tt(tmp[:], ps[:, :], pb_bc[:, hs], ALU.add)
                                P.tt(tmp[:], tmp[:], psc_bc[:, hs], ALU.mult, eng="pool")
                                P.tt(xg[t][:, hs], xg[t][:, hs], tmp[:], ALU.add)
                            P.dma(out_d[b, tile * 128:(tile + 1) * 128, :], xg[t][:], queue="pool", writes=[okey(b, tile)])
            stage_pools(NS, 12)
            set_banks([[0, 1, 2, 3], [4, 5, 6, 7]] if NS > 1 else [list(range(8))], [[6, 7]])
            run_streams(P, [(lambda b=b: body(b)) for b in range(NS)])
            set_banks([list(range(8))], [[6, 7]])
            dbg_dump("mix1")
            stage_end()


        bregs = {}

        def breg(e, val):
            if val not in bregs:
                bregs[val] = e.to_reg(val)
            return bregs[val]

        def stage_moe_sparse(l):
          BIGV = 30000.0
          def tile_of(i):
              return i // NT, i % NT
          widx = gst.enter_context(nc.sbuf_tensor("widx_l%d" % l, [128, NBLK], I32))
          IDX = gst.enter_context(nc.sbuf_tensor("IDX_l%d" % l, [128, NTI, 2], I32))
          G01 = gst.enter_context(nc.sbuf_tensor("G01_l%d" % l, [128, NTI, 2], F32))
          with contextlib.ExitStack() as st:
            P.stack = st
            wr = P.sb("wr", [128, 8, 36], F32)
            P.dma(wr[:, :, 0:4], rg_w[l].rearrange("(kc p) f -> p kc f", p=128))
            P.dma(wr[:, :, 4:36], re_w[l].rearrange("(kc p) f -> p kc f", p=128))
            rb = P.sb("rb", [128, 36], F32)
            P.dma(rb[:, 0:4], rg_b[l].partition_broadcast(128))
            P.dma(rb[:, 4:36], re_b[l].partition_broadcast(128))
            g_ffn = load_bc("g_ffn", ln_ffn[l], D)
            utsf = P.sb("utsf", [128, 128]); P.dma(utsf[:], c_uts)
            thr = load_bc("thr", c_thr, 16); jidx = load_bc("jidx", c_jidx, NBLK)
            iota = P.sb("iota", [128, 1]); P.dma(iota[:], c_iota)
            zkeys = []
            RT = P.sb("RT", [128, NTI, 128], F32)
            NSTR = 4 if NTI % 4 == 0 and NTI >= 8 else 1
            TPS = NTI // NSTR
            carries = [P.sb("carry%d" % k, [128, 32], F32) for k in range(NSTR)]
            for c_ in carries:
                P.memset(c_[:], 0.0)

            def rkey(i):
                return ("RT", i)

            def body1(sidx):
                carry = carries[sidx]
                x_r = Rot(P, "xm1", [128, D], F32, 2)
                h32_r = Rot(P, "h32", [128, D], F32, 1)
                hb_r = Rot(P, "hbm", [128, D], BF16, 2)
                h32T_r = Rot(P, "h32T", [128, 8, 128], F32, 1)
                for i in range(sidx * TPS, (sidx + 1) * TPS):
                    b, tile = tile_of(i)
                    x1 = x_r.get()
                    P.dma(x1[:], out_d[b, tile * 128:(tile + 1) * 128, :], reads=[okey(b, tile)])
                    h32 = h32_r.get()
                    rmsnorm_full(x1[:], D, g_ffn[:], h32[:])
                    hb = hb_r.get()
                    P.copy(hb[:], h32[:], eng="pool")
                    P.dma(Hn_d[i * 128:(i + 1) * 128, :], hb[:], queue="pool", writes=[("Hn", i)])
                    h32T = h32T_r.get()
                    transpose_to(h32[:], 8, 128, h32T[:], dt=F32)
                    ps_r = bank()
                    for kc in range(8):
                        P.mm(ps_r[:, 0:36], h32T[:, kc, :], wr[:, kc, :], start=(kc == 0), stop=(kc == 7))
                    lg = small.get()
                    P.tt(lg[:, 0:36], ps_r[:, 0:36], rb[:], ALU.add)
                    gmax = small.get(); P.red(gmax[:, 0:1], lg[:, 0:4], ALU.max)
                    goh = small.get(); P.ts(goh[:, 0:4], lg[:, 0:4], gmax[:, 0:1], None, ALU.is_ge)
                    ngmax = small.get(); P.ts(ngmax[:, 0:1], gmax[:, 0:1], -1.0, None, ALU.mult)
                    gex = small.get(); gsum = small.get()
                    P.actf(gex[:, 0:4], lg[:, 0:4], AF.Exp, bias=ngmax[:, 0:1], accum_out=gsum[:, 0:1])
                    gp = small.get(); P.recip(gp[:, 0:1], gsum[:, 0:1])
                    pen = small.get(); P.ts(pen[:, 0:4], goh[:, 0:4], -1.0, BIGV, ALU.add, ALU.mult)
                    elm = small.get()
                    P.tt(elm[:, 0:32].rearrange("p (g e) -> p g e", g=4), lg[:, 4:36].rearrange("p (g e) -> p g e", g=4),
                         bc_last(pen[:, 0:4], 8), ALU.add)
                    m1 = small.get(); P.red(m1[:, 0:1], elm[:, 0:32], ALU.max)
                    rt = small.get()
                    P.ts(rt[:, 0:32], elm[:, 0:32], m1[:, 0:1], None, ALU.is_ge)
                    elm2 = small.get(); P.stt(elm2[:, 0:32], rt[:, 0:32], -BIGV, elm[:, 0:32], ALU.mult, ALU.add)
                    m2 = small.get(); P.red(m2[:, 0:1], elm2[:, 0:32], ALU.max)
                    P.ts(rt[:, 32:64], elm2[:, 0:32], m2[:, 0:1], None, ALU.is_ge)
                    sel = small.get(); P.tt(sel[:, 0:32], rt[:, 0:32], rt[:, 32:64], ALU.add)
                    nm1 = small.get(); P.ts(nm1[:, 0:1], m1[:, 0:1], -1.0, None, ALU.mult)
                    ex = small.get(); P.actf(ex[:, 0:32], elm[:, 0:32], AF.Exp, bias=nm1[:, 0:1])
                    wv = small.get(); P.tt(wv[:, 0:32], ex[:, 0:32], sel[:, 0:32], ALU.mult)
                    ws = small.get(); P.red(ws[:, 0:1], wv[:, 0:32], ALU.add)
                    rws = small.get(); P.recip(rws[:, 0:1], ws[:, 0:1])
                    coef = small.get(); P.tt(coef[:, 0:1], rws[:, 0:1], gp[:, 0:1], ALU.mult)
                    P.ts(rt[:, 64:96], wv[:, 0:32], coef[:, 0:1], None, ALU.mult)
                    ps_p = bank()
                    P.mm(ps_p[:, 0:32], utsf[:], sel[:, 0:32])
                    P.mm(ps_p[:, 32:64], onesf[:], sel[:, 0:32])
                    P.tt(rt[:, 96:128], ps_p[:, 0:32], carry[:], ALU.add)
                    P.tt(carry[:], carry[:], ps_p[:, 32:64], ALU.add)
                    P.op("pool", lambda e, i=i, rt=rt: e.tensor_copy(RT[:, i, :], rt[:, 0:128]), [rt], [rkey(i)])

            stage_pools(NSTR, 17)
            if NSTR > 1:
                set_banks([[0, 1], [2, 3], [4, 5], [6, 7]], [[6, 7]])
            run_streams(P, [(lambda k=k: body1(k)) for k in range(NSTR)])
            set_banks([list(range(8))], [[6, 7]])
            if os.environ.get("MK_MOE_STOP") == "1":
                P.barrier(); P.emit()
                return
            offs = [None]
            carry = P.sb("carry_tot", [128, 32], F32)
            P.copy(carry[:], carries[0][:])
            for k in range(1, NSTR):
                o = P.sb("off%d" % k, [128, 32], F32)
                P.copy(o[:], carry[:])
                offs.append(o)
                P.tt(carry[:], carry[:], carries[k][:], ALU.add)
            cmp = P.sb("cmp", [128, 32, 16], F32)
            P.tt(cmp[:], bc_last(carry[:], 16), bc_mid(thr[:], 32), ALU.is_gt)
            nb = P.sb("nb", [128, 32], F32)
            P.red(nb[:], cmp[:], ALU.add)
            sc = [P.sb("sc0", [128, 32], F32), P.sb("sc1", [128, 32], F32)]
            P.copy(sc[0][:], nb[:])
            cur = 0
            for sh in (1, 2, 4, 8, 16):
                a, bb = sc[cur], sc[1 - cur]
                P.copy(bb[:, 0:sh], a[:, 0:sh])
                P.tt(bb[:, sh:32], a[:, sh:32], a[:, 0:32 - sh], ALU.add)
                cur = 1 - cur
            bend = sc[cur]
            rowst = P.sb("rowst", [128, 32], F32)
            P.tt(rowst[:], bend[:], nb[:], ALU.subtract)
            P.ts(rowst[:], rowst[:], float(BLK), None, ALU.mult)
            cmp2 = P.sb("cmp2", [128, NBLK, 32], F32)
            P.tt(cmp2[:], bc_last(jidx[:], 32), bc_mid(bend[:], NBLK), ALU.is_ge)
            be = P.sb("be", [128, NBLK], F32)
            P.red(be[:], cmp2[:], ALU.add)
            P.ts(be[:], be[:], 128.0, iota[:, 0:1], ALU.mult, ALU.add)
            P.copy(widx[:], be[:])
            rowst_s = [rowst]
            for k in range(1, NSTR):
                rs = P.sb("rowst%d" % k, [128, 32], F32)
                P.tt(rs[:], rowst[:], offs[k][:], ALU.add)
                rowst_s.append(rs)
            hb2_r = Rot(P, "hb2", [128, D], BF16, 3)
            xs_keys = []
            for i in range(NTI):
                dall = small.get()
                rs_ = rowst_s[i // TPS]
                P.op("dve", lambda e, i=i, dall=dall, rs_=rs_: e.tensor_tensor(dall[:, 0:32], RT[:, i, 96:128], rs_[:], ALU.add),
                     [rkey(i), rs_], [dall])
                d4 = small.get()
                tmp = small.get()
                for k in range(2):
                    P.op("dve", lambda e, i=i, k=k, tmp=tmp, dall=dall: e.tensor_tensor(tmp[:, 0:32], RT[:, i, 32 * k:32 * k + 32], dall[:, 0:32], ALU.mult),
                         [rkey(i), dall], [tmp])
                    P.red(d4[:, k:k + 1], tmp[:, 0:32], ALU.add)
                    P.op("dve", lambda e, i=i, k=k, tmp=tmp: e.tensor_tensor(tmp[:, 32:64], RT[:, i, 32 * k:32 * k + 32], RT[:, i, 64:96], ALU.mult),
                         [rkey(i)], [tmp])
                    P.red(d4[:, 2 + k:3 + k], tmp[:, 32:64], ALU.add)
                P.op("dve", lambda e, i=i, d4=d4: e.tensor_copy(IDX[:, i, :], d4[:, 0:2]), [d4], [("IDX", i)])
                P.op("dve", lambda e, i=i, d4=d4: e.tensor_copy(G01[:, i, :], d4[:, 2:4]), [d4], [("G01", i)])
                hb2 = hb2_r.get()
                P.dma(hb2[:], Hn_d[i * 128:(i + 1) * 128, :], reads=[("Hn", i)])
                for k in range(2):
                    P.dma_fn(lambda e, i=i, k=k, hb2=hb2: e.indirect_dma_start(
                        out=Xs_l[l][:, :], out_offset=bass.IndirectOffsetOnAxis(ap=IDX[:, i, k:k + 1], axis=0),
                        in_=hb2[:], in_offset=None, bounds_check=breg(e, NROWS - 1), oob_is_err=False),
                        "pool", [("IDX", i), hb2] + zkeys, [("Xs", i, k)])
                    xs_keys.append(("Xs", i, k))
            P.barrier()
            P.emit()
          if os.environ.get("MK_MOE_STOP") == "2":
              return
          with contextlib.ExitStack() as st:
            P.stack = st
            stage_pools()
            Wg_r = Rot(P, "Wg", [128, 2048], BF16, 3); Wu_r = Rot(P, "Wu", [128, 2048], BF16, 3)
            Wd_r = Rot(P, "Wd", [128, 2048], BF16, 3)
            xb_r = Rot(P, "xbm", [128, D], BF16, 12)
            xT_r = Rot(P, "xTm", [128, 8, 512], BF16, 2)
            aT_r = Rot(P, "aTm", [128, 2, 512], BF16, 2)
            sg_r = Rot(P, "sgm", [128, 512], F32, 3)
            yb_r = Rot(P, "ybm", [128, D], BF16, 4)
            def loads3(j):
                Wg = Wg_r.get(); Wu = Wu_r.get(); Wd = Wd_r.get()
                for Wt, src in ((Wg, ewg_l), (Wu, ewu_l), (Wd, ewd_l)):
                    P.dma_fn(lambda e, Wt=Wt, src=src, j=j: e.indirect_dma_start(
                        out=Wt[:], out_offset=None, in_=src[l][:, :],
                        in_offset=bass.IndirectOffsetOnAxis(ap=widx[:, j:j + 1], axis=0),
                        bounds_check=breg(e, NEXP * 128 - 1), oob_is_err=False), "pool", [widx], [Wt])
                xbs = []
                for t in range(4):
                    xb = xb_r.get()
                    r0 = j * BLK + t * 128
                    P.dma(xb[:], Xs_l[l][r0:r0 + 128, :], reads=["Xs_all"])
                    xbs.append(xb)
                return Wg, Wu, Wd, xbs

            nxt = loads3(0)
            for j in range(NBLK):
                Wg, Wu, Wd, xbs = nxt
                if j + 1 < NBLK:
                    nxt = loads3(j + 1)
                Wg3 = Wg[:].rearrange("p (kc f) -> p kc f", kc=8)
                Wu3 = Wu[:].rearrange("p (kc f) -> p kc f", kc=8)
                Wd3 = Wd[:].rearrange("p (c f) -> p c f", c=2)
                xT = xT_r.get()
                for t in range(4):
                    transpose_to(xbs[t][:], 8, 128, xT[:, :, t * 128:(t + 1) * 128])
                pg = [bank(), bank()]
                pu = [bank(), bank()]
                for fc in range(2):
                    for kc in range(8):
                        P.mm(pg[fc][:, :], Wg3[:, kc, fc * 128:(fc + 1) * 128], xT[:, kc, :], start=(kc == 0), stop=(kc == 7))
                    for kc in range(8):
                        P.mm(pu[fc][:, :], Wu3[:, kc, fc * 128:(fc + 1) * 128], xT[:, kc, :], start=(kc == 0), stop=(kc == 7))
                aT = aT_r.get()
                for fc in range(2):
                    sg = sg_r.get()
                    P.actf(sg[:], pg[fc][:, :], AF.Silu)
                    P.tt(aT[:, fc, :], sg[:], pu[fc][:, :], ALU.mult)
                for t in range(4):
                    yb = yb_r.get()
                    for hf in range(2):
                        pd = bank()
                        for fc in range(2):
                            P.mm(pd[:, :], aT[:, fc, t * 128:(t + 1) * 128], Wd3[:, fc, hf * 512:(hf + 1) * 512],
                                 start=(fc == 0), stop=(fc == 1))
                        P.copy(yb[:, hf * 512:(hf + 1) * 512], pd[:, :], eng=("act" if hf == 0 else "dve"))
                    r0 = j * BLK + t * 128
                    P.dma(Ys_d[r0:r0 + 128, :], yb[:], queue="sp", writes=[("Ys", j, t)])
            P.barrier()
            P.emit()
          if os.environ.get("MK_MOE_STOP") == "3":
              return
          with contextlib.ExitStack() as st:
            P.stack = st
            stage_pools()
            x_r = Rot(P, "xm4", [128, D], F32, 4)
            y_r = Rot(P, "ym4", [128, D], BF16, 8)
            def loads4(i):
                b, tile = tile_of(i)
                x1 = x_r.get()
                P.dma(x1[:], out_d[b, tile * 128:(tile + 1) * 128, :], reads=[okey(b, tile)])
                ys = []
                for k in range(2):
                    y = y_r.get()
                    P.dma_fn(lambda e, i=i, k=k, y=y: e.indirect_dma_start(
                        out=y[:], out_offset=None, in_=Ys_d[:, :],
                        in_offset=bass.IndirectOffsetOnAxis(ap=IDX[:, i, k:k + 1], axis=0),
                        bounds_check=breg(e, NROWS - 1), oob_is_err=False), "pool", ["Ys_all", IDX], [y])
                    ys.append(y)
                return x1, ys

            nxt = loads4(0)
            for i in range(NTI):
                b, tile = tile_of(i)
                x1, ys = nxt
                if i + 1 < NTI:
                    nxt = loads4(i + 1)
                for k in range(2):
                    y = ys[k]
                    P.op("dve", lambda e, i=i, k=k, y=y, x1=x1: e.scalar_tensor_tensor(x1[:], y[:], G01[:, i, k:k + 1], x1[:], ALU.mult, ALU.add),
                         [y, x1, G01], [x1])
                P.dma(out_d[b, tile * 128:(tile + 1) * 128, :], x1[:], queue="sp", writes=[okey(b, tile)])
            if l == 0:
                dbg_dump("moe0")
            stage_end()

        if run("XA0"):
            stage_xa(0)
        SPARSE = bool(int(os.environ.get("MK_SPARSE", "1")))
        if run("MOE0"):
            (stage_moe_sparse if SPARSE else stage_moe)(0)
        if run("POOL"):
            stage_pool()
        if run("XA1"):
            stage_xa(1)
        if run("MOE1"):
            (stage_moe_sparse if SPARSE else stage_moe)(1)
        P.barrier(new_sems=False)
        P.emit()
        return nc, P, None


def host_consts():
    i = np.arange(128)
    c = {}
    c["c_ident"] = np.eye(128, dtype=np.float32)
    c["c_ut"] = (i[:, None] <= i[None, :]).astype(np.float32)
    c["c_ls"] = (i[:, None] > i[None, :]).astype(np.float32)
    c["c_invf"] = (THETA ** (-np.arange(0, ROPE // 2, dtype=np.float32) * 2.0 / ROPE)).astype(np.float32)
    c["c_uts"] = (i[:, None] < i[None, :]).astype(np.float32)
    c["c_thr"] = (np.arange(16) * 512).astype(np.float32)
    c["c_iota"] = np.arange(128, dtype=np.float32).reshape(128, 1)
    t = np.arange(512, dtype=np.float32)
    c["c_icnt"] = np.stack([1.0 / np.minimum(t + 1.0, float(w)) for w in (2, 4, 8, 16)]).astype(np.float32)
    return c


WEIGHT_KEYS = ["ln_mix", "w_in", "q_lat_norm", "w_uq", "kv_lat_norm", "w_ukv", "q_norm", "k_norm",
               "dt_bias", "a_log", "d_skip", "ssd_norm", "w_out", "pool_w", "pool_b", "pool_scale",
               "ln_xq", "ln_mem", "xq_w", "xkv_w", "xq_norm", "xk_norm", "xo_w", "ln_ffn", "rg_w", "rg_b",
               "re_w", "re_b"]


def make_in_maps(inputs, NS, S, n_cores):
    NT = S // 128
    shared = {k: np.ascontiguousarray(np.asarray(inputs[k], dtype=np.float32)) for k in WEIGHT_KEYS}
    cw = np.asarray(inputs["conv_w"], dtype=np.float32)[0]
    shared["conv_wl"] = np.ascontiguousarray(cw.reshape(CK, 8, 128).transpose(2, 1, 0))
    cb = np.asarray(inputs["conv_b"], dtype=np.float32)[0]
    shared["conv_bl"] = np.ascontiguousarray(cb.reshape(8, 128).T)
    shared.update(host_consts())
    nblk = (2 * NS * S) // 512 + NEXP
    shared["c_jidx"] = np.arange(nblk, dtype=np.float32)
    g = np.asarray(inputs["exp_w_gate"], dtype=np.float32); u = np.asarray(inputs["exp_w_up"], dtype=np.float32)
    dn = np.asarray(inputs["exp_w_down"], dtype=np.float32)
    for li in range(2):
        shared["ewg_l%d" % li] = np.ascontiguousarray(g[li].reshape(NEXP, 8, 128, EFF).transpose(0, 2, 1, 3)).reshape(NEXP * 128, 2048)
        shared["ewu_l%d" % li] = np.ascontiguousarray(u[li].reshape(NEXP, 8, 128, EFF).transpose(0, 2, 1, 3)).reshape(NEXP * 128, 2048)
        shared["ewd_l%d" % li] = np.ascontiguousarray(dn[li].reshape(NEXP, 2, 128, D).transpose(0, 2, 1, 3)).reshape(NEXP * 128, 2048)
    x = np.asarray(inputs["x"]); mem = np.asarray(inputs["mem"]); pos = np.asarray(inputs["positions"])
    maps = []
    for c in range(n_cores):
        sl = slice(c * NS, (c + 1) * NS)
        m = dict(shared)
        m["x"] = np.ascontiguousarray(x[sl], dtype=np.float32)
        m["mem"] = np.ascontiguousarray(mem[sl], dtype=np.float32)
        p = pos[sl].astype(np.int32).reshape(NS * NT, 128).T
        m["posT"] = np.ascontiguousarray(p)
        maps.append(m)
    return maps


_CACHE = {}


def kernel(**inputs):
    n_cores = 8
    x = np.asarray(inputs["x"])
    B, S, _ = x.shape
    NS = B // n_cores
    key = (NS, S)
    if key not in _CACHE:
        nc, _, _ = build_program(NS, S, dbg=False)
        _CACHE[key] = nc
    nc = _CACHE[key]
    maps = make_in_maps(inputs, NS, S, n_cores)
    res = run_bass_kernel_spmd(nc, maps, core_ids=list(range(n_cores)))
    out = np.concatenate([np.asarray(r["out"]) for r in res.results], axis=0)
    return out.astype(np.float32, copy=False)
```

```python
import contextlib
import os
import numpy as np
import ml_dtypes
import concourse.bass as bass
import concourse.mybir as mybir
from concourse.bass_utils import run_bass_kernel_spmd

F32 = mybir.dt.float32
BF16 = mybir.dt.bfloat16
I32 = mybir.dt.int32
AF = mybir.ActivationFunctionType
ALU = mybir.AluOpType
AX = mybir.AxisListType

RSTD_LNEXP = bool(int(os.environ.get('MK_LNEXP', '1')))
SILU_EXP = bool(int(os.environ.get('MK_SILUEXP', '0')))
MASK_ENG = os.environ.get('MK_MASK', 'dve')
FIN_ENG = os.environ.get('MK_FIN', 'act')
A_POOL_ENG = os.environ.get('MK_APOOL', 'pool')
NO_SAME_ENGINE_SYNC = bool(int(os.environ.get('MK_NOSAME', '0')))
COMPUTE = ("pe", "act", "dve", "pool")
ALLQ = ("pe", "act", "dve", "pool", "sp")

D = 1024
MEM = 256
XH, XD = 4, 64
AH, NOPE, ROPE, QK, VD = 8, 64, 32, 96, 64
QL, KVL = 256, 128
BH, HP, DI, SG, SN, CK = 8, 64, 512, 2, 128, 4
IN_DIM = 1960
NEXP, EFF = 32, 256
EPS = 1e-6
THETA = 10000.0


class Prog:
    def __init__(self, nc, n_dma_sems=16):
        self.nc = nc
        self.stack = None
        self.q = {e: [] for e in ALLQ}
        self.dsem = [nc.alloc_semaphore("dsem%d" % i) for i in range(n_dma_sems)]
        self.dtot = [0] * n_dma_sems
        self.dq = {"sp": list(range(0, n_dma_sems // 2)), "pool": list(range(n_dma_sems // 2, n_dma_sems)),
                   "act": list(range(0, n_dma_sems // 2))}
        self.drr = {"sp": 0, "pool": 0, "act": 3}
        self.semobj = {}
        for i, s in enumerate(self.dsem):
            self.semobj["dsem%d" % i] = s
        self.known = {e: {} for e in ALLQ}
        self.last_w = {}
        self.readers = {}
        self.epoch = -1
        self.n_inst = 0
        self.n_wait = 0
        self.esem = {}
        self.ecnt = {}
        self.stream = 0
        self.hook = None
        self._new_sems()

    def _new_sems(self):
        self.epoch += 1
        for e in COMPUTE:
            nm = "sem_%s_%d" % (e, self.epoch)
            s = self.nc.alloc_semaphore(nm)
            self.esem[e] = (nm, s)
            self.semobj[nm] = s
            self.ecnt[e] = 0

    def sb(self, name, shape, dtype=F32):
        self._uid = getattr(self, "_uid", 0) + 1
        return self.stack.enter_context(self.nc.sbuf_tensor("%s_u%d" % (name, self._uid), list(shape), dtype))

    @staticmethod
    def key(x):
        if isinstance(x, (str, tuple)):
            return x
        if hasattr(x, "tensor"):
            return x.tensor.name
        return x.name

    def _wait(self, eng, tok, force=False):
        semname, val = tok
        if val <= 0:
            return
        kn = self.known[eng]
        if kn.get(semname, 0) >= val:
            return
        if eng in COMPUTE and semname == self.esem[eng][0] and not force and (eng == "pe" or NO_SAME_ENGINE_SYNC):
            return
        kn[semname] = val
        sem = self.semobj[semname]
        self.q[eng].append(lambda e, sem=sem, val=val: e.wait_ge(sem, val))
        self.n_wait += 1

    def _deps(self, eng, reads, writes):
        deps = {}
        for k in reads:
            t = self.last_w.get(self.key(k))
            if t:
                deps[t[0]] = max(deps.get(t[0], 0), t[1])
        for k in writes:
            k = self.key(k)
            t = self.last_w.get(k)
            if t:
                deps[t[0]] = max(deps.get(t[0], 0), t[1])
            for t in self.readers.get(k, ()):
                deps[t[0]] = max(deps.get(t[0], 0), t[1])
        for s, v in deps.items():
            self._wait(eng, (s, v))

    def _record(self, tok, reads, writes):
        for k in reads:
            lst = self.readers.setdefault(self.key(k), [])
            for i, t in enumerate(lst):
                if t[0] == tok[0]:
                    lst[i] = tok
                    break
            else:
                lst.append(tok)
        for k in writes:
            k = self.key(k)
            self.last_w[k] = tok
            self.readers[k] = []

    def op(self, eng, fn, reads=(), writes=()):
        self._deps(eng, reads, writes)
        self.ecnt[eng] += 1
        nm, sem = self.esem[eng]
        self.q[eng].append(lambda e, fn=fn, sem=sem: fn(e).then_inc(sem, 1))
        tok = (nm, self.ecnt[eng])
        self._record(tok, reads, writes)
        self.n_inst += 1
        if self.hook:
            self.hook()
        return tok

    def dma(self, out, in_, queue="sp", reads=None, writes=None, **kw):
        reads = [in_] if reads is None else reads
        writes = [out] if writes is None else writes
        lst = self.dq[queue]
        i = lst[self.drr[queue] % len(lst)]
        self.drr[queue] += 1
        semname = "dsem%d" % i
        self._wait(queue, (semname, self.dtot[i]))
        self._deps(queue, reads, writes)
        self.dtot[i] += 16
        sem = self.dsem[i]
        self.q[queue].append(lambda e, sem=sem: e.dma_start(out=out, in_=in_, **kw).then_inc(sem, 16))
        tok = (semname, self.dtot[i])
        self._record(tok, reads, writes)
        self.n_inst += 1
        if self.hook:
            self.hook()
        return tok

    def dma_fn(self, fn, queue, reads, writes):
        lst = self.dq[queue]
        i = lst[self.drr[queue] % len(lst)]
        self.drr[queue] += 1
        semname = "dsem%d" % i
        self._wait(queue, (semname, self.dtot[i]))
        self._deps(queue, reads, writes)
        self.dtot[i] += 16
        sem = self.dsem[i]
        self.q[queue].append(lambda e, sem=sem: fn(e).then_inc(sem, 16))
        tok = (semname, self.dtot[i])
        self._record(tok, reads, writes)
        self.n_inst += 1
        return tok

    def barrier(self, new_sems=True):
        for e in ALLQ:
            for c in COMPUTE:
                self._wait(e, (self.esem[c][0], self.ecnt[c]), force=True)
            for i in range(len(self.dsem)):
                self._wait(e, ("dsem%d" % i, self.dtot[i]))
        self.last_w = {}
        self.readers = {}
        if new_sems:
            self._new_sems()

    def mm(self, out, lhsT, rhs, start=True, stop=True, reads=None, writes=None):
        reads = [lhsT, rhs] if reads is None else reads
        writes = [out] if writes is None else writes
        return self.op("pe", lambda e: e.matmul(out, lhsT, rhs, start=start, stop=stop), reads, writes)

    def tr(self, out, in_, ident):
        return self.op("pe", lambda e: e.transpose(out, in_, ident), [in_, ident], [out])

    def actf(self, out, in_, func, bias=None, scale=None, accum_out=None):
        r = [in_]
        kw = {}
        if bias is not None:
            kw["bias"] = bias
            if not isinstance(bias, (int, float)):
                r.append(bias)
        if scale is not None:
            kw["scale"] = scale
            if not isinstance(scale, (int, float)):
                r.append(scale)
        w = [out]
        if accum_out is not None:
            kw["accum_out"] = accum_out
            w.append(accum_out)
        return self.op("act", lambda e: e.activation(out, in_, func, **kw), r, w)

    def tt(self, out, in0, in1, op, eng="dve"):
        return self.op(eng, lambda e: e.tensor_tensor(out, in0, in1, op), [in0, in1], [out])

    def ts(self, out, in0, s1, s2, op0, op1=None, eng="dve"):
        r = [in0]
        if not isinstance(s1, (int, float)):
            r.append(s1)
        if s2 is not None and not isinstance(s2, (int, float)):
            r.append(s2)
        if op1 is None:
            return self.op(eng, lambda e: e.tensor_scalar(out, in0, s1, None, op0), r, [out])
        return self.op(eng, lambda e: e.tensor_scalar(out, in0, s1, s2, op0, op1), r, [out])

    def stt(self, out, in0, scalar, in1, op0, op1, eng="dve"):
        r = [in0, in1]
        if not isinstance(scalar, (int, float)):
            r.append(scalar)
        return self.op(eng, lambda e: e.scalar_tensor_tensor(out, in0, scalar, in1, op0, op1), r, [out])

    def copy(self, out, in_, eng="dve"):
        if eng == "act":
            return self.op("act", lambda e: e.copy(out, in_), [in_], [out])
        return self.op(eng, lambda e: e.tensor_copy(out, in_), [in_], [out])

    def red(self, out, in_, op, axis=None, eng="dve"):
        axis = AX.X if axis is None else axis
        return self.op(eng, lambda e: e.tensor_reduce(out, in_, axis, op), [in_], [out])

    def recip(self, out, in_):
        return self.op("dve", lambda e: e.reciprocal(out, in_), [in_], [out])

    def memset(self, ap, val, eng="pool"):
        return self.op(eng, lambda e: e.memset(ap, val), [], [ap])

    def emit(self):
        nc = self.nc
        q = self.q
        self.q = {e: [] for e in ALLQ}
        with nc.Block() as block:
            @block.sync
            def _(e):
                for f in q["sp"]:
                    f(e)

            @block.tensor
            def _(e):
                for f in q["pe"]:
                    f(e)

            @block.scalar
            def _(e):
                for f in q["act"]:
                    f(e)

            @block.vector
            def _(e):
                for f in q["dve"]:
                    f(e)

            @block.gpsimd
            def _(e):
                for f in q["pool"]:
                    f(e)


def run_streams(P, bodies):
    import threading
    n = len(bodies)
    if n == 1:
        P.stream = 0
        bodies[0]()
        return
    sems = [threading.Semaphore(0) for _ in range(n)]
    main = threading.Semaphore(0)
    done = [False] * n
    cur = [0]
    errs = []

    def nxt(i):
        for d in range(1, n + 1):
            j = (i + d) % n
            if not done[j]:
                return j
        return None

    def hook():
        i = cur[0]
        j = nxt(i)
        if j is None or j == i:
            return
        cur[0] = j
        P.stream = j
        sems[j].release()
        sems[i].acquire()
        P.stream = i

    def runner(i):
        sems[i].acquire()
        P.stream = i
        try:
            bodies[i]()
        except BaseException as ex:
            errs.append(ex)
        done[i] = True
        j = nxt(i)
        if j is None:
            main.release()
        else:
            cur[0] = j
            P.stream = j
            sems[j].release()

    ths = [threading.Thread(target=runner, args=(i,)) for i in range(n)]
    for t in ths:
        t.start()
    P.hook = hook
    cur[0] = 0
    sems[0].release()
    main.acquire()
    P.hook = None
    P.stream = 0
    for t in ths:
        t.join()
    if errs:
        raise errs[0]


class SRot:
    def __init__(self, P, name, shape, dtype):
        self.P, self.name, self.shape, self.dtype = P, name, shape, dtype
        self.pools = []

    def setup(self, nstreams, n):
        self.pools = [Rot(self.P, "%s_s%d" % (self.name, i), self.shape, self.dtype, n) for i in range(nstreams)]

    def get(self):
        return self.pools[self.P.stream].get()


class SplitRot:
    def __init__(self, pools, P):
        self.pools, self.P = pools, P

    def get(self):
        return self.pools[min(self.P.stream, len(self.pools) - 1)].get()


class Rot:
    def __init__(self, P, name, shape, dtype, n):
        self.t = [P.sb("%s_%d" % (name, i), shape, dtype) for i in range(n)]
        self.i = 0

    def get(self):
        t = self.t[self.i % len(self.t)]
        self.i += 1
        return t


def bc_mid(ap2d, n):
    p, f = ap2d.shape
    return ap2d.unsqueeze(1).to_broadcast([p, n, f])


def bc_last(ap2d, n):
    p, h = ap2d.shape
    return ap2d.unsqueeze(2).to_broadcast([p, h, n])


def build_program(NS, S, dbg=False, stages=None):
    NT = S // 128
    NG = S // 512
    NTT = NS * NT
    nc = bass.Bass("TRN2", target_bir_lowering=False)

    def din(name, shape, dt=F32):
        return nc.dram_tensor(name, list(shape), dt, kind="ExternalInput").ap()

    def dscr(name, shape, dt=F32):
        kind = "ExternalOutput" if dbg else "Internal"
        return nc.dram_tensor(name, list(shape), dt, kind=kind).ap()

    x_d = din("x", [NS, S, D])
    mem_d = din("mem", [NS, MEM, D])
    posT_d = din("posT", [128, NTT], I32)
    ln_mix = din("ln_mix", [2, D]); w_in = din("w_in", [1, D, IN_DIM])
    q_lat_norm = din("q_lat_norm", [1, QL]); w_uq = din("w_uq", [1, QL, AH * QK])
    kv_lat_norm = din("kv_lat_norm", [1, KVL]); w_ukv = din("w_ukv", [1, KVL, AH * (NOPE + VD)])
    q_norm = din("q_norm", [1, QK]); k_norm = din("k_norm", [1, QK])
    conv_wl = din("conv_wl", [128, 8, CK]); conv_bl = din("conv_bl", [128, 8])
    dt_bias = din("dt_bias", [1, BH]); a_log = din("a_log", [1, BH]); d_skip = din("d_skip", [1, BH])
    ssd_norm = din("ssd_norm", [1, DI]); w_out = din("w_out", [1, D, D])
    pool_w = din("pool_w", [1, 4, 256, 256]); pool_b = din("pool_b", [1, D]); pool_scale = din("pool_scale", [1, D])
    ln_xq = din("ln_xq", [2, D]); ln_mem = din("ln_mem", [2, D])
    xq_w = din("xq_w", [2, D, XH * XD]); xkv_w = din("xkv_w", [2, D, 2 * XH * XD])
    xq_norm = din("xq_norm", [2, XD]); xk_norm = din("xk_norm", [2, XD]); xo_w = din("xo_w", [2, XH * XD, D])
    ln_ffn = din("ln_ffn", [2, D]); rg_w = din("rg_w", [2, D, 4]); rg_b = din("rg_b", [2, 4])
    re_w = din("re_w", [2, D, NEXP]); re_b = din("re_b", [2, NEXP])
    c_ident = din("c_ident", [128, 128]); c_ut = din("c_ut", [128, 128]); c_ls = din("c_ls", [128, 128])
    c_invf = din("c_invf", [ROPE // 2]); c_icnt = din("c_icnt", [4, 512])
    NTOK = NS * S
    BLK = 512
    NBLK = (2 * NTOK) // BLK + NEXP
    NROWS = NBLK * BLK
    NTI = NTOK // 128
    c_uts = din("c_uts", [128, 128]); c_thr = din("c_thr", [16]); c_jidx = din("c_jidx", [NBLK]); c_iota = din("c_iota", [128, 1])
    ewg_l = [din("ewg_l%d" % i, [NEXP * 128, 2048]) for i in range(2)]
    ewu_l = [din("ewu_l%d" % i, [NEXP * 128, 2048]) for i in range(2)]
    ewd_l = [din("ewd_l%d" % i, [NEXP * 128, 2048]) for i in range(2)]
    Hn_d = nc.dram_tensor("Hn_s", [NTOK, D], BF16, kind="Internal").ap()
    Xs_d = nc.dram_tensor("Xs_s", [NROWS, D], BF16, kind="Internal").ap()
    Ys_d = nc.dram_tensor("Ys_s", [NROWS, D], BF16, kind="Internal").ap()

    out_d = nc.dram_tensor("out", [NS, S, D], F32, kind="ExternalOutput").ap()
    qT_d = dscr("qT_s", [NS, AH, QK, S], BF16)
    kT_d = dscr("kT_s", [NS, AH, QK, S], BF16)
    v_d = dscr("v_s", [NS, S, AH * VD], BF16)
    yT_d = dscr("yT_s", [NS, 4, 128, S], BF16)
    aT_d = dscr("aT_s", [NS, AH * VD, S], BF16)
    dbg_out = {}
    if dbg:
        for nm in ("mix0", "xa0", "moe0", "mix1", "xa1"):
            dbg_out[nm] = nc.dram_tensor("dbg_" + nm, [NS, S, D], F32, kind="ExternalOutput").ap()

    def okey(b, tile):
        return ("out", b, tile)

    with contextlib.ExitStack() as gst:
        P = Prog(nc)
        P.stack = gst
        banks = [gst.enter_context(nc.psum_tensor("pb%d" % i, [128, 512], F32)) for i in range(8)]
        bank_cfg = {"sets": [list(range(8))], "acc": [[6, 7]]}
        bank_i = {}
        acc_i = {}

        def set_banks(sets, acc):
            bank_cfg["sets"] = sets
            bank_cfg["acc"] = acc

        def bank():
            sidx = P.stream if P.stream < len(bank_cfg["sets"]) else 0
            ids = bank_cfg["sets"][sidx]
            k = bank_i.get(sidx, 0)
            bank_i[sidx] = k + 1
            return banks[ids[k % len(ids)]]

        def accbank():
            sidx = P.stream if P.stream < len(bank_cfg["acc"]) else 0
            ids = bank_cfg["acc"][sidx]
            k = acc_i.get(sidx, 0)
            acc_i[sidx] = k + 1
            return banks[ids[k % len(ids)]]

        identf = P.sb("identf", [128, 128]); identb = P.sb("identb", [128, 128], BF16)
        utf = P.sb("utf", [128, 128]); utb = P.sb("utb", [128, 128], BF16)
        lsf = P.sb("lsf", [128, 128]); onesf = P.sb("onesf", [128, 128])
        epsb = P.sb("epsb", [128, 1])
        cosT = P.sb("cosT", [128, NTT, 16]); sinT = P.sb("sinT", [128, NTT, 16])
        P.dma(identf[:], c_ident); P.dma(utf[:], c_ut); P.dma(lsf[:], c_ls)
        P.copy(identb[:], identf[:]); P.copy(utb[:], utf[:])
        P.memset(onesf[:], 1.0); P.memset(epsb[:], EPS)

        small = SRot(P, "small", [128, 128], F32)
        junk = SRot(P, "junk", [128, 1024], F32)
        junkb = SRot(P, "junkb", [128, 1024], BF16)

        def stage_pools(nstreams=1, nsmall=28, need_junk=False):
            small.setup(nstreams, nsmall)
            junkb.setup(nstreams, 1)
            if need_junk:
                junk.setup(nstreams, 1)

        def rstd_from_ss(ss, n, inv_d):
            r = small.get()
            r2 = small.get()
            if RSTD_LNEXP:
                P.actf(r[:, 0:n], ss, AF.Ln, bias=epsb[:, 0:1], scale=inv_d)
                P.actf(r2[:, 0:n], r[:, 0:n], AF.Exp, scale=-0.5)
            else:
                P.actf(r[:, 0:n], ss, AF.Sqrt, bias=epsb[:, 0:1], scale=inv_d)
                P.recip(r2[:, 0:n], r[:, 0:n])
            return r2[:, 0:n]

        def silu_exp(out_ap, x_ap, tmp_pool, n):
            e = tmp_pool.get()
            P.actf(e[:, 0:n], x_ap, AF.Exp, scale=-1.0)
            P.ts(e[:, 0:n], e[:, 0:n], 1.0, None, ALU.add)
            P.recip(e[:, 0:n], e[:, 0:n])
            P.tt(out_ap, x_ap, e[:, 0:n], ALU.mult)

        def rmsnorm_full(x_ap, dd, gain_bc, out_ap):
            ss = small.get()
            j = junkb.get()
            P.actf(j[:, 0:dd], x_ap, AF.Square, accum_out=ss[:, 0:1])
            r = rstd_from_ss(ss[:, 0:1], 1, 1.0 / dd)
            P.stt(out_ap, x_ap, r[:, 0:1], gain_bc, ALU.mult, ALU.mult)

        def transpose_to(in_ap, nchunk, csz, out_ap, dt=BF16, evac="dve"):
            done = 0
            per = (1024 if dt == BF16 else 512) // 128
            while done < nchunk:
                n = min(per, nchunk - done)
                bk = bank()
                bv = bk[:].bitcast(BF16) if dt == BF16 else bk[:]
                idn = identb if dt == BF16 else identf
                for c in range(n):
                    P.tr(bv[0:csz, c * 128:(c + 1) * 128], in_ap[:, (done + c) * csz:(done + c + 1) * csz], idn[:])
                src = bv[0:csz, 0:n * 128].rearrange("p (a c) -> p a c", a=n)
                P.copy(out_ap[:, done:done + n, :], src, eng=evac)
                done += n

        def load_bc(name, vec_ap, n, eng_q="sp"):
            t = P.sb(name, [128, n])
            P.dma(t[:], vec_ap.partition_broadcast(128), queue=eng_q)
            return t

        tmpst = contextlib.ExitStack()
        P.stack = tmpst
        posi = P.sb("posi", [128, NTT], I32); posf = P.sb("posf", [128, NTT])
        invf = load_bc("invf", c_invf, 16)
        P.dma(posi[:], posT_d)
        P.copy(posf[:], posi[:])
        ang = P.sb("ang", [128, NTT, 16]); a1 = P.sb("ang1", [128, NTT, 16]); ki = P.sb("angk", [128, NTT, 16], I32)
        kf = P.sb("angkf", [128, NTT, 16])
        P.tt(ang[:], bc_last(posf[:], 16), bc_mid(invf[:], NTT), ALU.mult)
        TWO_PI = float(2 * np.pi)

        def sin_of(dst, shift):
            P.ts(a1[:], ang[:], shift, 1.0 / TWO_PI, ALU.add, ALU.mult)
            P.copy(ki[:], a1[:])
            P.copy(kf[:], ki[:])
            P.ts(a1[:], ang[:], shift, None, ALU.add)
            P.stt(a1[:], kf[:], -TWO_PI, a1[:], ALU.mult, ALU.add)
            P.ts(kf[:], a1[:], float(np.pi), -TWO_PI, ALU.is_gt, ALU.mult)
            P.tt(a1[:], a1[:], kf[:], ALU.add)
            P.ts(kf[:], a1[:], float(-np.pi), TWO_PI, ALU.is_lt, ALU.mult)
            P.tt(a1[:], a1[:], kf[:], ALU.add)
            P.actf(dst, a1[:], AF.Sin)

        sin_of(sinT[:], 0.0)
        sin_of(cosT[:], float(np.pi / 2))
        P.barrier(new_sems=False)
        P.emit()
        tmpst.close()
        P.stack = gst

        def rope(u_ap, tile_idx, o1, o2, nh):
            u1 = u_ap[:, :, 0:16]; u2 = u_ap[:, :, 16:32]
            cb = bc_mid(cosT[:, tile_idx, :], nh); sb_ = bc_mid(sinT[:, tile_idx, :], nh)
            ta = small.get(); tb = small.get()
            tav = ta[:, 0:nh * 16].rearrange("p (h f) -> p h f", h=nh)
            tbv = tb[:, 0:nh * 16].rearrange("p (h f) -> p h f", h=nh)
            P.tt(tav, u1, cb, ALU.mult); P.tt(tbv, u2, sb_, ALU.mult)
            P.tt(o1, tav, tbv, ALU.subtract)
            tc_ = small.get(); td = small.get()
            tcv = tc_[:, 0:nh * 16].rearrange("p (h f) -> p h f", h=nh)
            tdv = td[:, 0:nh * 16].rearrange("p (h f) -> p h f", h=nh)
            P.tt(tcv, u2, cb, ALU.mult); P.tt(tdv, u1, sb_, ALU.mult)
            P.tt(o2, tcv, tdv, ALU.add)

        def softmax_finalize(ps_o, ncols, out_bf, P_osb, P_rl):
            osb = P_osb.get()
            P.copy(osb[0:65, 0:ncols], ps_o[0:65, 0:ncols], eng=FIN_ENG)
            rl = P_rl.get()
            P.recip(rl[64:65, 0:ncols], osb[64:65, 0:ncols])
            pb = bank()
            P.mm(pb[0:64, 0:ncols], onesf[64:65, 0:64], rl[64:65, 0:ncols])
            P.tt(out_bf, osb[0:64, 0:ncols], pb[0:64, 0:ncols], ALU.mult)


        def dbg_dump(name):
            if not dbg:
                return
            P.barrier(new_sems=False)
            for b in range(NS):
                P.dma(dbg_out[name][b], out_d[b], reads=["out_all"], writes=["dbg_" + name])
            P.barrier(new_sems=False)

        dumped = set()

        def dump(name, ap, dt=F32):
            if not dbg or name in dumped:
                return
            dumped.add(name)
            d = nc.dram_tensor("dmp_" + name, list(ap.shape), dt, kind="ExternalOutput").ap()
            P.dma(d, ap, queue="sp")

        def stage_end():
            P.barrier()
            P.emit()

        run = (lambda s: True) if stages is None else (lambda s: s in stages)

        if run("A"):
          with contextlib.ExitStack() as st:
            P.stack = st
            stage_pools(3, 9, need_junk=True)
            set_banks([[0, 1, 2], [3, 4], [5, 6, 7]], [[6, 7]])
            w_in_sb = P.sb("w_in_sb", [128, 8, IN_DIM], BF16)
            w_in_v = w_in[0].rearrange("(kc p) f -> p kc f", p=128)
            P.dma(w_in_sb[:, :, 0:416], w_in_v[:, :, 0:416], queue="pool")
            P.dma(w_in_sb[:, :, 416:424], w_in_v[:, :, 1952:1960], queue="pool")
            P.dma(w_in_sb[:, :, 424:1960], w_in_v[:, :, 416:1952], queue="pool")
            w_uq_sb = P.sb("w_uq_sb", [128, 2, AH * QK], BF16)
            P.dma(w_uq_sb[:], w_uq[0].rearrange("(kc p) f -> p kc f", p=128), queue="pool")
            w_ukv_sb = P.sb("w_ukv_sb", [128, 1024], BF16)
            P.dma(w_ukv_sb[:], w_ukv[0], queue="pool")
            g_mix = load_bc("g_mix", ln_mix[0], D)
            g_ql = load_bc("g_ql", q_lat_norm[0], QL); g_kvl = load_bc("g_kvl", kv_lat_norm[0], KVL)
            g_q = load_bc("g_q", q_norm[0], QK); g_k = load_bc("g_k", k_norm[0], QK)
            dtb = load_bc("dtb", dt_bias[0], BH); alog = load_bc("alog", a_log[0], BH)
            dsk8 = load_bc("dsk8", d_skip[0], BH); g_ssd = load_bc("g_ssd", ssd_norm[0], DI)
            abc = P.sb("abc", [128, BH])
            P.actf(abc[:], alog[:], AF.Exp)
            P.ts(abc[:], abc[:], -1.0, None, ALU.mult)
            cw = P.sb("cw", [128, 8, CK]); cb_ = P.sb("cb", [128, 8])
            P.dma(cw[:], conv_wl); P.dma(cb_[:], conv_bl)

            xt_r = Rot(P, "xt", [128, D], F32, 2)
            hb_r = Rot(P, "hb", [128, D], BF16, 1)
            hT_r = Rot(P, "hT", [128, 8, 512], BF16, 1)
            U = [P.sb("U%d" % c, [128, 515], F32) for c in range(8)]
            cacc_r = Rot(P, "cacc", [128, 512], F32, 1)
            xbcT_r = [Rot(P, "xbcT%d" % c, [128, 512], BF16, 1) for c in range(8)]
            qT_g_r = Rot(P, "qT_g", [96, AH, 512], BF16, 1)
            kT_g_r = Rot(P, "kT_g", [96, AH, 512], BF16, 1)
            v_g_r = Rot(P, "v_g", [128, 4, AH * VD], BF16, 1)
            yT_g_r = Rot(P, "yT_g", [128, 4, 512], BF16, 1)
            Sf = P.sb("Sf", [128, 512], F32); Sb = P.sb("Sb", [128, 512], BF16)
            f512 = SplitRot([Rot(P, "f512q", [128, 512], F32, 1), Rot(P, "f512kv", [128, 512], F32, 2), Rot(P, "f512s", [128, 512], F32, 3)], P)
            pa_r = Rot(P, "pa", [128, 424], F32, 2)
            se_r = Rot(P, "se", [128, 512], F32, 2) if SILU_EXP else None
            zs_r = Rot(P, "zs", [128, 512], F32, 2)
            b512 = SplitRot([Rot(P, "b512q", [128, 512], BF16, 2), Rot(P, "b512kv", [128, 512], BF16, 2), Rot(P, "b512s", [128, 512], BF16, 5)], P)
            f768 = Rot(P, "f768", [128, 768], F32, 2)
            f1k = Rot(P, "f1k", [128, 1024], F32, 1)
            b768 = SplitRot([Rot(P, "b768q", [128, 768], BF16, 1), Rot(P, "b768kv", [128, 768], BF16, 1)], P)
            lhs_r = Rot(P, "lhsh", [128, 128], F32, 4)
            dec_r = Rot(P, "dec", [128, 4, 128], F32, 1)
            MT_r = Rot(P, "MT", [128, 8, 128], BF16, 2)
            ps_keep = {}

            ASTOP = int(os.environ.get("A_STOP", "99"))

            class _Stop(Exception):
                pass

            def stop_if(k):
                if ASTOP == k:
                    raise _Stop()

            try:
              for b in range(NS):
                  for c in range(8):
                      P.memset(U[c][:, 0:3], 0.0)
                  P.memset(Sf[:], 0.0); P.memset(Sb[:], 0.0)
                  for g in range(NG):
                      hT = hT_r.get()
                      qT_g = qT_g_r.get(); kT_g = kT_g_r.get(); v_g = v_g_r.get(); yT_g = yT_g_r.get()
                      for t in range(4):
                          tile = g * 4 + t
                          xt = xt_r.get()
                          P.dma(xt[:], x_d[b, tile * 128:(tile + 1) * 128, :])
                          hb = hb_r.get()
                          rmsnorm_full(xt[:], D, g_mix[:], hb[:])
                          transpose_to(hb[:], 8, 128, hT[:, :, t * 128:(t + 1) * 128])
                      xbcT = []
                      for fc in range(8):
                          bk = bank()
                          for kc in range(8):
                              P.mm(bk[:, :], w_in_sb[:, kc, 936 + fc * 128:936 + (fc + 1) * 128], hT[:, kc, :],
                                   start=(kc == 0), stop=(kc == 7))
                          P.copy(U[fc][:, 3:515], bk[:, :], eng="act")
                          ca = cacc_r.get()
                          P.ts(ca[:], U[fc][:, 3:515], cw[:, fc, 3:4], None, ALU.mult)
                          for k in (2, 1, 0):
                              P.stt(ca[:], U[fc][:, k:k + 512], cw[:, fc, k:k + 1], ca[:], ALU.mult, ALU.add)
                          P.copy(U[fc][:, 0:3], U[fc][:, 512:515], eng="pool")
                          xo = xbcT_r[fc].get()
                          if SILU_EXP:
                              P.ts(ca[:], ca[:], cb_[:, fc:fc + 1], None, ALU.add)
                              silu_exp(xo[:], ca[:], se_r, 512)
                          else:
                              P.actf(xo[:], ca[:], AF.Silu, bias=cb_[:, fc:fc + 1])
                          xbcT.append(xo)
                          dump("xbcT%d" % fc, xo[:], BF16)
                      for t in range(4):
                          tile = g * 4 + t
                          gt = b * NT + tile
                          tsl = slice(t * 128, (t + 1) * 128)
                          ps_a = bank()
                          for kc in range(8):
                              P.mm(ps_a[:, 0:424], hT[:, kc, tsl], w_in_sb[:, kc, 0:424], start=(kc == 0), stop=(kc == 7))
                          ps_z = bank()
                          for kc in range(8):
                              P.mm(ps_z[:, :], hT[:, kc, tsl], w_in_sb[:, kc, 424:936], start=(kc == 0), stop=(kc == 7))
                          pa = pa_r.get()
                          P.copy(pa[:, 0:424], ps_a[:, 0:424], eng="act")
                          zs = zs_r.get()
                          if SILU_EXP:
                              silu_exp(zs[:], ps_z[:, :], se_r, 512)
                          else:
                              P.actf(zs[:], ps_z[:, :], AF.Silu)
                          def q_path():
                              qlb = b512.get()
                              rmsnorm_full(pa[:, 0:256], QL, g_ql[:], qlb[:, 0:256])
                              qlT = b512.get()
                              transpose_to(qlb[:, 0:256], 2, 128, qlT[:, 0:256].rearrange("p (a c) -> p a c", a=2))
                              q_sb = f768.get()
                              bq0 = bank(); bq1 = bank()
                              for kc in range(2):
                                  P.mm(bq0[:, :], qlT[:, kc * 128:(kc + 1) * 128], w_uq_sb[:, kc, 0:512], start=(kc == 0), stop=(kc == 1))
                              for kc in range(2):
                                  P.mm(bq1[:, 0:256], qlT[:, kc * 128:(kc + 1) * 128], w_uq_sb[:, kc, 512:768], start=(kc == 0), stop=(kc == 1))
                              P.copy(q_sb[:, 0:512], bq0[:, :], eng="act")
                              P.copy(q_sb[:, 512:768], bq1[:, 0:256], eng="act")
                              sq = junk.get()
                              P.actf(sq[:, 0:768], q_sb[:], AF.Square)
                              ssq = small.get()
                              P.red(ssq[:, 0:8], sq[:, 0:768].rearrange("p (h f) -> p h f", h=8), ALU.add)
                              rq = rstd_from_ss(ssq[:, 0:8], 8, 1.0 / QK)
                              qn = f768.get()
                              q3 = q_sb[:].rearrange("p (h f) -> p h f", h=8)
                              qn3 = qn[:].rearrange("p (h f) -> p h f", h=8)
                              P.tt(qn3, q3, bc_last(rq, QK), ALU.mult)
                              P.tt(qn3, qn3, bc_mid(g_q[:], 8), ALU.mult)
                              qf = b768.get()
                              qf3 = qf[:].rearrange("p (h f) -> p h f", h=8)
                              P.copy(qf3[:, :, 0:64], qn3[:, :, 0:64], eng=os.environ.get("MK_CAST", "act"))
                              rope(qn3[:, :, 64:96], gt, qf3[:, :, 64:80], qf3[:, :, 80:96], 8)
                              transpose_to(qf[:], 8, 96, qT_g[:, :, tsl])

                          def kv_path():
                              kvb = b512.get()
                              rmsnorm_full(pa[:, 256:384], KVL, g_kvl[:], kvb[:, 0:128])
                              kvT = b512.get()
                              transpose_to(kvb[:, 0:128], 1, 128, kvT[:, 0:128].rearrange("p (a c) -> p a c", a=1))
                              kv_sb = f1k.get()
                              for hf in range(2):
                                  bkv = bank()
                                  P.mm(bkv[:, :], kvT[:, 0:128], w_ukv_sb[:, hf * 512:(hf + 1) * 512])
                                  P.copy(kv_sb[:, hf * 512:(hf + 1) * 512], bkv[:, :], eng="act")
                              kv3 = kv_sb[:].rearrange("p (h f) -> p h f", h=8)
                              sqk = junk.get()
                              P.actf(sqk[:], kv_sb[:], AF.Square)
                              ssk = small.get()
                              P.red(ssk[:, 0:8], sqk[:].rearrange("p (h f) -> p h f", h=8)[:, :, 0:64], ALU.add)
                              ssr = small.get()
                              jr = small.get()
                              P.actf(jr[:, 0:32], pa[:, 384:416], AF.Square, accum_out=ssr[:, 0:1])
                              P.ts(ssk[:, 0:8], ssk[:, 0:8], ssr[:, 0:1], None, ALU.add)
                              rk = rstd_from_ss(ssk[:, 0:8], 8, 1.0 / QK)
                              kf_ = b768.get()
                              kf3 = kf_[:].rearrange("p (h f) -> p h f", h=8)
                              kn = f512.get()
                              kn3 = kn[:].rearrange("p (h f) -> p h f", h=8)
                              P.tt(kn3, kv3[:, :, 0:64], bc_last(rk, 64), ALU.mult)
                              P.tt(kf3[:, :, 0:64], kn3, bc_mid(g_k[:, 0:64], 8), ALU.mult)
                              krg = small.get()
                              P.tt(krg[:, 0:32], pa[:, 384:416], g_k[:, 64:96], ALU.mult)
                              kr = f512.get()
                              kr3 = kr[:, 0:256].rearrange("p (h f) -> p h f", h=8)
                              P.tt(kr3, bc_mid(krg[:, 0:32], 8), bc_last(rk, 32), ALU.mult)
                              rope(kr3, gt, kf3[:, :, 64:80], kf3[:, :, 80:96], 8)
                              transpose_to(kf_[:], 8, 96, kT_g[:, :, tsl])
                              P.copy(v_g[:, t, :].rearrange("p (h f) -> p h f", h=8), kv3[:, :, 64:128], eng=os.environ.get("MK_CAST", "act"))

                          def ssd_path():
                              xs_tm = b512.get(); B_tm = b512.get()
                              bk = bank(); bv = bk[:].bitcast(BF16)
                              for c in range(4):
                                  P.tr(bv[:, c * 128:(c + 1) * 128], xbcT[c][:, tsl], identb[:])
                              for c in range(2):
                                  P.tr(bv[:, 512 + c * 128:512 + (c + 1) * 128], xbcT[4 + c][:, tsl], identb[:])
                              P.copy(xs_tm[:], bv[:, 0:512])
                              P.copy(B_tm[:, 0:256], bv[:, 512:768])
                              dtr = small.get(); dte_ = small.get(); dtv = small.get(); adt = small.get()
                              P.tt(dtr[:, 0:8], pa[:, 416:424], dtb[:], ALU.add)
                              P.actf(dte_[:, 0:8], dtr[:, 0:8], AF.Exp)
                              P.actf(dtv[:, 0:8], dte_[:, 0:8], AF.Ln, bias=1.0)
                              P.tt(adt[:, 0:8], dtv[:, 0:8], abc[:], ALU.mult)
                              dump("dtv_%d" % t, dtv[:, 0:8]); dump("xs_tm_%d" % t, xs_tm[:], BF16)
                              ps_c = bank()
                              P.mm(ps_c[:, 0:8], utf[:], adt[:, 0:8])
                              P.mm(ps_c[:, 8:16], onesf[:], adt[:, 0:8])
                              ct = small.get()
                              P.copy(ct[:, 0:16], ps_c[:, 0:16])
                              acs = ct[:, 0:8]; tot = ct[:, 8:16]
                              MT = MT_r.get()
                              ps_cb = bank()
                              for gr in range(2):
                                  P.mm(ps_cb[:, gr * 128:(gr + 1) * 128], xbcT[4 + gr][:, tsl], xbcT[6 + gr][:, tsl])
                              cbm = f512.get()
                              cbm3 = cbm[:, 0:256].rearrange("p (g t) -> p g t", g=2)
                              P.tt(cbm3, ps_cb[:, 0:256].rearrange("p (g t) -> p g t", g=2), bc_mid(utf[:], 2), ALU.mult)
                              for gr in range(2):
                                  ps_d = bank()
                                  for hh in range(4):
                                      h = gr * 4 + hh
                                      lh = lhs_r.get()
                                      P.ts(lh[:], lsf[:], adt[:, h:h + 1], None, ALU.mult, eng=os.environ.get("MK_LH", "dve"))
                                      P.mm(ps_d[:, hh * 128:(hh + 1) * 128], lh[:], utf[:])
                                  dec = dec_r.get()
                                  P.actf(dec[:].rearrange("p a b -> p (a b)"), ps_d[:, :], AF.Exp)
                                  P.tt(MT[:, gr * 4:(gr + 1) * 4, :], dec[:], bc_mid(cbm3[:, gr, :], 4), ALU.mult)
                              xdt = b512.get()
                              P.tt(xdt[:].rearrange("p (h f) -> p h f", h=8), xs_tm[:].rearrange("p (h f) -> p h f", h=8),
                                   bc_last(dtv[:, 0:8], 64), ALU.mult)
                              e3 = small.get()
                              P.tt(e3[:, 0:8], tot, acs, ALU.subtract)
                              P.actf(e3[:, 0:8], e3[:, 0:8], AF.Exp)
                              P.actf(e3[:, 8:16], acs, AF.Exp)
                              P.actf(e3[:, 16:24], tot, AF.Exp)
                              xdte = b512.get()
                              P.tt(xdte[:].rearrange("p (h f) -> p h f", h=8), xdt[:].rearrange("p (h f) -> p h f", h=8),
                                   bc_last(e3[:, 0:8], 64), ALU.mult, eng=A_POOL_ENG)
                              ps_yo = bank()
                              for gr in range(2):
                                  P.mm(ps_yo[:, gr * 256:(gr + 1) * 256], xbcT[6 + gr][:, tsl], Sb[:, gr * 256:(gr + 1) * 256])
                              ps_yd = bank()
                              for h in range(8):
                                  P.mm(ps_yd[:, h * 64:(h + 1) * 64], MT[:, h, :], xdt[:, h * 64:(h + 1) * 64])
                              y1 = f512.get()
                              P.tt(y1[:].rearrange("p (h f) -> p h f", h=8), ps_yo[:, :].rearrange("p (h f) -> p h f", h=8),
                                   bc_last(e3[:, 8:16], 64), ALU.mult)
                              P.tt(y1[:], y1[:], ps_yd[:, :], ALU.add)
                              y2 = f512.get()
                              P.tt(y2[:].rearrange("p (h f) -> p h f", h=8), xs_tm[:].rearrange("p (h f) -> p h f", h=8),
                                   bc_last(dsk8[:], 64), ALU.mult, eng=A_POOL_ENG)
                              P.tt(y1[:], y1[:], y2[:], ALU.add)
                              dump("ydiag_%d" % t, ps_yd[:, :]) if False else None
                              dump("y1_%d" % t, y1[:]); dump("acs_%d" % t, ct[:, 0:16]); dump("MT_%d" % t, MT[:], BF16)
                              ps_s = bank()
                              for gr in range(2):
                                  P.mm(ps_s[:, gr * 256:(gr + 1) * 256], B_tm[:, gr * 128:(gr + 1) * 128], xdte[:, gr * 256:(gr + 1) * 256])
                              P.tt(Sf[:].rearrange("p (h f) -> p h f", h=8), Sf[:].rearrange("p (h f) -> p h f", h=8),
                                   bc_last(e3[:, 16:24], 64), ALU.mult)
                              P.tt(Sf[:], Sf[:], ps_s[:, :], ALU.add)
                              P.copy(Sb[:], Sf[:], eng="act")
                              P.tt(y1[:], y1[:], zs[:], ALU.mult)
                              ynb = b512.get()
                              rmsnorm_full(y1[:], DI, g_ssd[:], ynb[:])
                              transpose_to(ynb[:], 4, 128, yT_g[:, :, tsl])

                          run_streams(P, [q_path, kv_path, ssd_path])
                      gs = slice(g * 512, (g + 1) * 512)
                      P.dma(qT_d[b, :, :, gs].rearrange("h d s -> d h s"), qT_g[:], queue="pool", writes=[("qT", b)])
                      P.dma(kT_d[b, :, :, gs].rearrange("h d s -> d h s"), kT_g[:], queue="pool", writes=[("kT", b)])
                      P.dma(v_d[b, gs, :].rearrange("(t p) f -> p t f", p=128), v_g[:], queue="pool", writes=[("v", b)])
                      P.dma(yT_d[b, :, :, gs].rearrange("c p s -> p c s"), yT_g[:], queue="pool", writes=[("yT", b)])
            except _Stop:
                pass
            set_banks([list(range(8))], [[6, 7]])
            stage_end()

        if run("B"):
          with contextlib.ExitStack() as st:
            P.stack = st
            def body(b):
                kT_r = Rot(P, "kTh", [96, S], BF16, 2); qT_r = Rot(P, "qTh", [96, S], BF16, 2)
                Vx_r = Rot(P, "Vx", [128, NT, 65], BF16, 2)
                for t_ in Vx_r.t:
                    P.memset(t_[:, :, 64:65], 1.0)
                pt_r = Rot(P, "pt", [128, 512], BF16, 4)
                at_r = Rot(P, "at", [64, 512], BF16, 2)
                osb_r = Rot(P, "osb", [128, 512], F32, 2); rl_r = Rot(P, "rl", [128, 512], F32, 2)
                scale = float(QK ** -0.5)
                if True:
                    for h in range(AH):
                        kT = kT_r.get(); qT = qT_r.get(); Vx = Vx_r.get()
                        P.dma(kT[:], kT_d[b, h], reads=[("kT", b)])
                        P.dma(qT[:], qT_d[b, h], reads=[("qT", b)])
                        P.dma(Vx[:, :, 0:64], v_d[b, :, h * 64:(h + 1) * 64].rearrange("(n p) d -> p n d", p=128),
                              reads=[("v", b)])
                        for qg in range(NG):
                            ps_o = accbank()
                            nkb = 4 * qg + 4
                            SKEW = 2
                            pend = {}
                            for kk in range(nkb + SKEW):
                                if kk < nkb:
                                    kb = kk
                                    i = kb - 4 * qg
                                    q0 = 128 * i if i > 0 else 0
                                    ps_s = bank()
                                    P.mm(ps_s[:, q0:512], kT[:, kb * 128:(kb + 1) * 128], qT[:, qg * 512 + q0:(qg + 1) * 512])
                                    pend[kb] = (ps_s, q0, i)
                                kb = kk - SKEW
                                if kb >= 0:
                                    ps_s, q0, i = pend.pop(kb)
                                    pt = pt_r.get()
                                    P.actf(pt[:, q0:512], ps_s[:, q0:512], AF.Exp, scale=scale)
                                    if q0 > 0:
                                        P.memset(pt[:, 0:q0], 0.0, eng=MASK_ENG)
                                    if i >= 0:
                                        P.tt(pt[:, q0:q0 + 128], pt[:, q0:q0 + 128], utb[:], ALU.mult, eng=MASK_ENG)
                                    P.mm(ps_o[0:65, :], Vx[:, kb, :], pt[:, :], start=(kb == 0), stop=(kb == nkb - 1))
                            at = at_r.get()
                            softmax_finalize(ps_o, 512, at[:], osb_r, rl_r)
                            P.dma(aT_d[b, h * 64:(h + 1) * 64, qg * 512:(qg + 1) * 512], at[:], queue="pool",
                                  writes=[("aT", b)])

            stage_pools(NS)
            set_banks([[0, 1, 2], [4, 5, 6]] if NS > 1 else [list(range(6))], [[3], [7]] if NS > 1 else [[6, 7]])
            run_streams(P, [(lambda b=b: body(b)) for b in range(NS)])
            set_banks([list(range(8))], [[6, 7]])
            stage_end()

        if run("C"):
          with contextlib.ExitStack() as st:
            P.stack = st
            w_out_sb = P.sb("w_out_sb", [128, 8, D], BF16)
            P.dma(w_out_sb[:], w_out[0].rearrange("(kc p) f -> p kc f", p=128), queue="pool")
            def body(b):
                mixT_r = Rot(P, "mixT", [128, 8, 512], BF16, 2)
                xt_r = Rot(P, "xtc", [128, D], F32, 3)
                if True:
                    for g in range(NG):
                        mixT = mixT_r.get()
                        gs = slice(g * 512, (g + 1) * 512)
                        P.dma(mixT[:, 0:4, :], aT_d[b, :, gs].rearrange("(c p) s -> p c s", p=128), reads=[("aT", b)])
                        P.dma(mixT[:, 4:8, :], yT_d[b, :, :, gs].rearrange("c p s -> p c s"), reads=[("yT", b)])
                        for t in range(4):
                            tile = g * 4 + t
                            xt = xt_r.get()
                            P.dma(xt[:], x_d[b, tile * 128:(tile + 1) * 128, :])
                            for hf in range(2):
                                ps = bank()
                                for c in range(8):
                                    P.mm(ps[:, :], mixT[:, c, t * 128:(t + 1) * 128], w_out_sb[:, c, hf * 512:(hf + 1) * 512],
                                         start=(c == 0), stop=(c == 7))
                                P.tt(xt[:, hf * 512:(hf + 1) * 512], xt[:, hf * 512:(hf + 1) * 512], ps[:, :], ALU.add)
                            P.dma(out_d[b, tile * 128:(tile + 1) * 128, :], xt[:], queue="pool", writes=[okey(b, tile)])
            stage_pools(NS)
            set_banks([[0, 1, 2, 3], [4, 5, 6, 7]] if NS > 1 else [list(range(8))], [[6, 7]])
            run_streams(P, [(lambda b=b: body(b)) for b in range(NS)])
            set_banks([list(range(8))], [[6, 7]])
            dbg_dump("mix0")
            stage_end()

        def stage_xa(l):
          with contextlib.ExitStack() as st:
            P.stack = st
            ztile = P.sb("ztile", [128, D], BF16)
            P.memset(ztile[:], 0.0)
            zch = 2048 if NROWS % 2048 == 0 else 512
            for c in range(NROWS // zch):
                P.dma(Xs_d[c * zch:(c + 1) * zch, :].rearrange("(n p) d -> p n d", p=128),
                      ztile[:].unsqueeze(1).to_broadcast([128, zch // 128, D]), queue="act", writes=[("Xsz", c)])
            xq_sb = P.sb("xq_sb", [128, 8, XH * XD], BF16)
            P.dma(xq_sb[:], xq_w[l].rearrange("(kc p) f -> p kc f", p=128), queue="pool")
            xkv_sb = P.sb("xkv_sb", [128, 8, 2 * XH * XD], BF16)
            P.dma(xkv_sb[:], xkv_w[l].rearrange("(kc p) f -> p kc f", p=128), queue="pool")
            xo_sb = P.sb("xo_sb", [64, XH, D], BF16)
            P.dma(xo_sb[:], xo_w[l].rearrange("(h p) f -> p h f", p=64), queue="pool")
            g_xq = load_bc("g_xq", ln_xq[l], D); g_mem = load_bc("g_mem", ln_mem[l], D)
            g_q = load_bc("g_xqn", xq_norm[l], XD); g_k = load_bc("g_xkn", xk_norm[l], XD)
            memk = [P.sb("memk%d" % b, [64, XH, MEM], BF16) for b in range(NS)]
            memV = [P.sb("memV%d" % b, [128, 2, XH, 65], BF16) for b in range(NS)]
            def body(b):
                xg_r = Rot(P, "xg", [128, D], F32, 5)
                hb_r = Rot(P, "hbx", [128, D], BF16, 2)
                hT_r = Rot(P, "hTx", [128, 8, 512], BF16, 1)
                mT = P.sb("mT", [128, 8, MEM], BF16)
                f512 = Rot(P, "f512x", [128, 512], F32, 3)
                b256 = Rot(P, "b256x", [128, 256], BF16, 3)
                qT_r = Rot(P, "qTx", [64, XH, 512], BF16, 1)
                oT_r = Rot(P, "oTx", [64, XH, 512], BF16, 1)
                pt_r = Rot(P, "ptx", [128, 512], BF16, 3)
                osb_r = Rot(P, "osbx", [128, 512], F32, 1); rl_r = Rot(P, "rlx", [128, 512], F32, 1)
                scale = float(XD ** -0.5)

                def head_norm(src_ap, g_bc, out_bf):
                    sq = f512.get()
                    P.actf(sq[:, 0:256], src_ap, AF.Square)
                    ss = small.get()
                    P.red(ss[:, 0:4], sq[:, 0:256].rearrange("p (h f) -> p h f", h=4), ALU.add)
                    r = rstd_from_ss(ss[:, 0:4], 4, 1.0 / XD)
                    qn = f512.get()
                    qn3 = qn[:, 0:256].rearrange("p (h f) -> p h f", h=4)
                    P.tt(qn3, src_ap.rearrange("p (h f) -> p h f", h=4), bc_last(r, XD), ALU.mult)
                    P.tt(out_bf.rearrange("p (h f) -> p h f", h=4), qn3, bc_mid(g_bc[:], 4), ALU.mult)

                if True:
                    P.memset(memV[b][:, :, :, 64:65], 1.0)
                    for mt in range(2):
                        xm = xg_r.get()
                        P.dma(xm[:], mem_d[b, mt * 128:(mt + 1) * 128, :])
                        mb = hb_r.get()
                        rmsnorm_full(xm[:], D, g_mem[:], mb[:])
                        transpose_to(mb[:], 8, 128, mT[:, :, mt * 128:(mt + 1) * 128])
                    for mt in range(2):
                        ps = bank()
                        for kc in range(8):
                            P.mm(ps[:, :], mT[:, kc, mt * 128:(mt + 1) * 128], xkv_sb[:, kc, :], start=(kc == 0), stop=(kc == 7))
                        kv = f512.get()
                        P.copy(kv[:], ps[:, :], eng="act")
                        knb = b256.get()
                        head_norm(kv[:, 0:256], g_k, knb[:])
                        transpose_to(knb[:], 4, 64, memk[b][:, :, mt * 128:(mt + 1) * 128])
                        P.copy(memV[b][:, mt, :, 0:64], kv[:, 256:512].rearrange("p (h f) -> p h f", h=4), eng="pool")

                if True:
                    for g in range(NG):
                        hT = hT_r.get()
                        xg = []
                        for t in range(4):
                            tile = g * 4 + t
                            x1 = xg_r.get()
                            P.dma(x1[:], out_d[b, tile * 128:(tile + 1) * 128, :], reads=[okey(b, tile)])
                            hb = hb_r.get()
                            rmsnorm_full(x1[:], D, g_xq[:], hb[:])
                            transpose_to(hb[:], 8, 128, hT[:, :, t * 128:(t + 1) * 128])
                            xg.append(x1)
                        qT = qT_r.get()
                        for t in range(4):
                            tsl = slice(t * 128, (t + 1) * 128)
                            ps_q = bank()
                            for kc in range(8):
                                P.mm(ps_q[:, 0:256], hT[:, kc, tsl], xq_sb[:, kc, :], start=(kc == 0), stop=(kc == 7))
                            q_sb = f512.get()
                            P.copy(q_sb[:, 0:256], ps_q[:, 0:256], eng="act")
                            qb = b256.get()
                            head_norm(q_sb[:, 0:256], g_q, qb[:])
                            transpose_to(qb[:], 4, 64, qT[:, :, tsl])
                        oT = oT_r.get()
                        for hh in range(XH):
                            ps_o = accbank()
                            for mt in range(2):
                                ps_s = bank()
                                P.mm(ps_s[:, :], memk[b][:, hh, mt * 128:(mt + 1) * 128], qT[:, hh, :])
                                pt = pt_r.get()
                                P.actf(pt[:], ps_s[:, :], AF.Exp, scale=scale)
                                P.mm(ps_o[0:65, :], memV[b][:, mt, hh, :], pt[:], start=(mt == 0), stop=(mt == 1))
                            softmax_finalize(ps_o, 512, oT[:, hh, :], osb_r, rl_r)
                        for t in range(4):
                            tile = g * 4 + t
                            tsl = slice(t * 128, (t + 1) * 128)
                            for hf in range(2):
                                ps = bank()
                                for hh in range(XH):
                                    P.mm(ps[:, :], oT[:, hh, tsl], xo_sb[:, hh, hf * 512:(hf + 1) * 512],
                                         start=(hh == 0), stop=(hh == XH - 1))
                                P.tt(xg[t][:, hf * 512:(hf + 1) * 512], xg[t][:, hf * 512:(hf + 1) * 512], ps[:, :], ALU.add)
                            P.dma(out_d[b, tile * 128:(tile + 1) * 128, :], xg[t][:], queue="pool", writes=[okey(b, tile)])
            stage_pools(NS, 20)
            set_banks([[0, 1, 2], [4, 5, 6]] if NS > 1 else [list(range(6))], [[3], [7]] if NS > 1 else [[6, 7]])
            run_streams(P, [(lambda b=b: body(b)) for b in range(NS)])
            set_banks([list(range(8))], [[6, 7]])
            dbg_dump("xa%d" % l)
            stage_end()

        def stage_moe(l):
          NTOK = NS * S
          SBT = min(2048, NTOK)
          nsb = NTOK // SBT
          tsb = SBT // 128
          gsb = SBT // 512
          BIG = 30000.0
          for sbi in range(nsb):
           with contextlib.ExitStack() as st:
            P.stack = st
            stage_pools()
            wr = P.sb("wr", [128, 8, 36], F32)
            P.dma(wr[:, :, 0:4], rg_w[l].rearrange("(kc p) f -> p kc f", p=128))
            P.dma(wr[:, :, 4:36], re_w[l].rearrange("(kc p) f -> p kc f", p=128))
            rb = P.sb("rb", [128, 36], F32)
            P.dma(rb[:, 0:4], rg_b[l].partition_broadcast(128))
            P.dma(rb[:, 4:36], re_b[l].partition_broadcast(128))
            g_ffn = load_bc("g_ffn", ln_ffn[l], D)
            acc = [P.sb("acc%d" % i, [128, D], F32) for i in range(tsb)]
            h2T = [P.sb("h2T%d" % i, [128, 8, 512], BF16) for i in range(gsb)]
            G = [P.sb("G%d" % i, [128, 32], F32) for i in range(tsb)]
            h32_r = Rot(P, "h32", [128, D], F32, 2)
            h32T_r = Rot(P, "h32T", [128, 8, 128], F32, 2)
            Wg_r = Rot(P, "Wg", [128, 8, EFF], BF16, 2); Wu_r = Rot(P, "Wu", [128, 8, EFF], BF16, 2)
            Wd_r = Rot(P, "Wd", [128, 2, D], BF16, 2)
            aT_r = Rot(P, "aTm", [128, 2, 512], BF16, 2)
            sg_r = Rot(P, "sgm", [128, 512], F32, 3)

            def tile_of(i):
                gtile = sbi * tsb + i
                return gtile // NT, gtile % NT

            for i in range(tsb):
                b, tile = tile_of(i)
                P.dma(acc[i][:], out_d[b, tile * 128:(tile + 1) * 128, :], reads=[okey(b, tile)])
                h32 = h32_r.get()
                rmsnorm_full(acc[i][:], D, g_ffn[:], h32[:])
                h32T = h32T_r.get()
                transpose_to(h32[:], 8, 128, h32T[:], dt=F32)
                P.copy(h2T[i // 4][:, :, (i % 4) * 128:(i % 4 + 1) * 128], h32T[:], eng="pool")
                ps_r = bank()
                for kc in range(8):
                    P.mm(ps_r[:, 0:36], h32T[:, kc, :], wr[:, kc, :], start=(kc == 0), stop=(kc == 7))
                lg = small.get()
                P.tt(lg[:, 0:36], ps_r[:, 0:36], rb[:], ALU.add)
                gmax = small.get(); P.red(gmax[:, 0:1], lg[:, 0:4], ALU.max)
                goh = small.get(); P.ts(goh[:, 0:4], lg[:, 0:4], gmax[:, 0:1], None, ALU.is_ge)
                ngmax = small.get(); P.ts(ngmax[:, 0:1], gmax[:, 0:1], -1.0, None, ALU.mult)
                gex = small.get(); gsum = small.get()
                P.actf(gex[:, 0:4], lg[:, 0:4], AF.Exp, bias=ngmax[:, 0:1], accum_out=gsum[:, 0:1])
                gp = small.get(); P.recip(gp[:, 0:1], gsum[:, 0:1])
                pen = small.get(); P.ts(pen[:, 0:4], goh[:, 0:4], -1.0, BIG, ALU.add, ALU.mult)
                elm = small.get()
                P.tt(elm[:, 0:32].rearrange("p (g e) -> p g e", g=4), lg[:, 4:36].rearrange("p (g e) -> p g e", g=4),
                     bc_last(pen[:, 0:4], 8), ALU.add)
                m1 = small.get(); P.red(m1[:, 0:1], elm[:, 0:32], ALU.max)
                oh1 = small.get(); P.ts(oh1[:, 0:32], elm[:, 0:32], m1[:, 0:1], None, ALU.is_ge)
                elm2 = small.get(); P.stt(elm2[:, 0:32], oh1[:, 0:32], -BIG, elm[:, 0:32], ALU.mult, ALU.add)
                m2 = small.get(); P.red(m2[:, 0:1], elm2[:, 0:32], ALU.max)
                sel = small.get(); P.ts(sel[:, 0:32], elm2[:, 0:32], m2[:, 0:1], None, ALU.is_ge)
                P.tt(sel[:, 0:32], sel[:, 0:32], oh1[:, 0:32], ALU.add)
                nm1 = small.get(); P.ts(nm1[:, 0:1], m1[:, 0:1], -1.0, None, ALU.mult)
                ex = small.get(); P.actf(ex[:, 0:32], elm[:, 0:32], AF.Exp, bias=nm1[:, 0:1])
                wv = small.get(); P.tt(wv[:, 0:32], ex[:, 0:32], sel[:, 0:32], ALU.mult)
                ws = small.get(); P.red(ws[:, 0:1], wv[:, 0:32], ALU.add)
                rws = small.get(); P.recip(rws[:, 0:1], ws[:, 0:1])
                coef = small.get(); P.tt(coef[:, 0:1], rws[:, 0:1], gp[:, 0:1], ALU.mult)
                P.ts(G[i][:], wv[:, 0:32], coef[:, 0:1], None, ALU.mult)
                dump("G_%d_%d" % (l, i), G[i][:])
            for e in range(NEXP):
                Wg = Wg_r.get(); Wu = Wu_r.get(); Wd = Wd_r.get()
                P.dma(Wg[:], ewg_l[l][e * 128:(e + 1) * 128, :].rearrange("p (kc f) -> p kc f", kc=8), queue="pool")
                P.dma(Wu[:], ewu_l[l][e * 128:(e + 1) * 128, :].rearrange("p (kc f) -> p kc f", kc=8), queue="pool")
                P.dma(Wd[:], ewd_l[l][e * 128:(e + 1) * 128, :].rearrange("p (c f) -> p c f", c=2), queue="pool")
                for gi in range(gsb):
                    pg = [bank(), bank()]
                    pu = [bank(), bank()]
                    for fc in range(2):
                        for kc in range(8):
                            P.mm(pg[fc][:, :], Wg[:, kc, fc * 128:(fc + 1) * 128], h2T[gi][:, kc, :], start=(kc == 0), stop=(kc == 7))
                        for kc in range(8):
                            P.mm(pu[fc][:, :], Wu[:, kc, fc * 128:(fc + 1) * 128], h2T[gi][:, kc, :], start=(kc == 0), stop=(kc == 7))
                    aT = aT_r.get()
                    for fc in range(2):
                        sg = sg_r.get()
                        P.actf(sg[:], pg[fc][:, :], AF.Silu)
                        P.tt(aT[:, fc, :], sg[:], pu[fc][:, :], ALU.mult)
                    for t in range(4):
                        i = gi * 4 + t
                        for hf in range(2):
                            pd = bank()
                            for fc in range(2):
                                P.mm(pd[:, :], aT[:, fc, t * 128:(t + 1) * 128], Wd[:, fc, hf * 512:(hf + 1) * 512],
                                     start=(fc == 0), stop=(fc == 1))
                            P.stt(acc[i][:, hf * 512:(hf + 1) * 512], pd[:, :], G[i][:, e:e + 1],
                                  acc[i][:, hf * 512:(hf + 1) * 512], ALU.mult, ALU.add)
            for i in range(tsb):
                b, tile = tile_of(i)
                P.dma(out_d[b, tile * 128:(tile + 1) * 128, :], acc[i][:], queue="pool", writes=[okey(b, tile)])
            if sbi == nsb - 1:
                dbg_dump("moe%d" % l) if l == 0 else None
            stage_end()

        def stage_pool():
          with contextlib.ExitStack() as st:
            P.stack = st
            pw_sb = P.sb("pw_sb", [128, 4, 2, 256], BF16)
            for cg in range(4):
                P.dma(pw_sb[:, cg, :, :], pool_w[0, cg].rearrange("(cc p) d -> p cc d", p=128), queue="pool")
            pb_bc = load_bc("pb_bc", pool_b[0], D); psc_bc = load_bc("psc_bc", pool_scale[0], D)
            g_m1 = load_bc("g_m1", ln_mix[1], D)
            icnt = load_bc("icnt", c_icnt.rearrange("a b -> (a b)"), 4 * 512)
            def body(b):
                H = P.sb("H", [128, 8, 527], F32)
                xg_r = Rot(P, "xgp", [128, D], F32, 5)
                hb_r = Rot(P, "hbp", [128, D], BF16, 2)
                lv_r = Rot(P, "lv", [128, 2, 527], F32, 3)
                dl_r = [Rot(P, "dl%d" % c, [128, 2, 512], BF16, 1) for c in range(4)]
                f512 = Rot(P, "f512p", [128, 512], F32, 3)
                if True:
                    P.memset(H[:, :, 0:15], 0.0)
                    for g in range(NG):
                        xg = []
                        for t in range(4):
                            tile = g * 4 + t
                            x1 = xg_r.get()
                            P.dma(x1[:], out_d[b, tile * 128:(tile + 1) * 128, :], reads=[okey(b, tile)])
                            hb = hb_r.get()
                            rmsnorm_full(x1[:], D, g_m1[:], hb[:])
                            transpose_to(hb[:], 8, 128, H[:, :, 15 + t * 128:15 + (t + 1) * 128])
                            xg.append(x1)
                        dl = []
                        for cg in range(4):
                            w = 2 ** (cg + 1)
                            cur = H[:, 2 * cg:2 * cg + 2, :]
                            for k in range(cg + 1):
                                sh = 2 ** k
                                lo = 2 * sh - 1
                                nx = lv_r.get()
                                P.tt(nx[:, :, lo:527], cur[:, :, lo:527], cur[:, :, lo - sh:527 - sh], ALU.add,
                                     eng=("pool" if k % 2 == 0 else "dve"))
                                cur = nx[:]
                            d_ = dl_r[cg].get()
                            if g == 0:
                                tmp = lv_r.get()
                                P.tt(tmp[:, :, 0:512], cur[:, :, 15:527], bc_mid(icnt[:, cg * 512:(cg + 1) * 512], 2), ALU.mult)
                                P.tt(d_[:], tmp[:, :, 0:512], H[:, 2 * cg:2 * cg + 2, 15:527], ALU.subtract)
                            else:
                                P.stt(d_[:], cur[:, :, 15:527], 1.0 / w, H[:, 2 * cg:2 * cg + 2, 15:527], ALU.mult, ALU.subtract)
                            dl.append(d_)
                        P.copy(H[:, :, 0:15], H[:, :, 512:527], eng="pool")
                        for t in range(4):
                            tile = g * 4 + t
                            tsl = slice(t * 128, (t + 1) * 128)
                            for hf in range(2):
                                ps = bank()
                                for c2 in range(2):
                                    cg = hf * 2 + c2
                                    for cc in range(2):
                                        P.mm(ps[:, c2 * 256:(c2 + 1) * 256], dl[cg][:, cc, tsl], pw_sb[:, cg, cc, :],
                                             start=(cc == 0), stop=(cc == 1))
                                tmp = f512.get()
                                hs = slice(hf * 512, (hf + 1) * 512)
                                P.tt(tmp[:], ps[:, :], pb_bc[:, hs], ALU.add)
                                P.tt(tmp[:], tmp[:], psc_bc[:, hs], ALU.mult, eng="pool")
                                P.tt(xg[t][:, hs], xg[t][:, hs], tmp[:], ALU.add)
                            P.dma(out_d[b, tile * 128:(tile + 1) * 128, :], xg[t][:], queue="pool", writes=[okey(b, tile)])
            stage_pools(NS, 12)
            set_banks([[0, 1, 2, 3], [4, 5, 6, 7]] if NS > 1 else [list(range(8))], [[6, 7]])
            run_streams(P, [(lambda b=b: body(b)) for b in range(NS)])
            set_banks([list(range(8))], [[6, 7]])
            dbg_dump("mix1")
            stage_end()


        bregs = {}

        def breg(e, val):
            if val not in bregs:
                bregs[val] = e.to_reg(val)
            return bregs[val]

        def stage_moe_sparse(l):
          BIGV = 30000.0
          def tile_of(i):
              return i // NT, i % NT
          widx = gst.enter_context(nc.sbuf_tensor("widx_l%d" % l, [128, NBLK], I32))
          IDX = gst.enter_context(nc.sbuf_tensor("IDX_l%d" % l, [128, NTI, 2], I32))
          G01 = gst.enter_context(nc.sbuf_tensor("G01_l%d" % l, [128, NTI, 2], F32))
          with contextlib.ExitStack() as st:
            P.stack = st
            wr = P.sb("wr", [128, 8, 36], F32)
            P.dma(wr[:, :, 0:4], rg_w[l].rearrange("(kc p) f -> p kc f", p=128))
            P.dma(wr[:, :, 4:36], re_w[l].rearrange("(kc p) f -> p kc f", p=128))
            rb = P.sb("rb", [128, 36], F32)
            P.dma(rb[:, 0:4], rg_b[l].partition_broadcast(128))
            P.dma(rb[:, 4:36], re_b[l].partition_broadcast(128))
            g_ffn = load_bc("g_ffn", ln_ffn[l], D)
            utsf = P.sb("utsf", [128, 128]); P.dma(utsf[:], c_uts)
            thr = load_bc("thr", c_thr, 16); jidx = load_bc("jidx", c_jidx, NBLK)
            iota = P.sb("iota", [128, 1]); P.dma(iota[:], c_iota)
            zkeys = []
            RT = P.sb("RT", [128, NTI, 128], F32)
            NSTR = 4 if NTI % 4 == 0 and NTI >= 8 else 1
            TPS = NTI // NSTR
            carries = [P.sb("carry%d" % k, [128, 32], F32) for k in range(NSTR)]
            for c_ in carries:
                P.memset(c_[:], 0.0)

            def rkey(i):
                return ("RT", i)

            def body1(sidx):
                carry = carries[sidx]
                x_r = Rot(P, "xm1", [128, D], F32, 2)
                h32_r = Rot(P, "h32", [128, D], F32, 1)
                hb_r = Rot(P, "hbm", [128, D], BF16, 2)
                h32T_r = Rot(P, "h32T", [128, 8, 128], F32, 1)
                for i in range(sidx * TPS, (sidx + 1) * TPS):
                    b, tile = tile_of(i)
                    x1 = x_r.get()
                    P.dma(x1[:], out_d[b, tile * 128:(tile + 1) * 128, :], reads=[okey(b, tile)])
                    h32 = h32_r.get()
                    rmsnorm_full(x1[:], D, g_ffn[:], h32[:])
                    hb = hb_r.get()
                    P.copy(hb[:], h32[:], eng="pool")
                    P.dma(Hn_d[i * 128:(i + 1) * 128, :], hb[:], queue="pool", writes=[("Hn", i)])
                    h32T = h32T_r.get()
                    transpose_to(h32[:], 8, 128, h32T[:], dt=F32)
                    ps_r = bank()
                    for kc in range(8):
                        P.mm(ps_r[:, 0:36], h32T[:, kc, :], wr[:, kc, :], start=(kc == 0), stop=(kc == 7))
                    lg = small.get()
                    P.tt(lg[:, 0:36], ps_r[:, 0:36], rb[:], ALU.add)
                    gmax = small.get(); P.red(gmax[:, 0:1], lg[:, 0:4], ALU.max)
                    goh = small.get(); P.ts(goh[:, 0:4], lg[:, 0:4], gmax[:, 0:1], None, ALU.is_ge)
                    ngmax = small.get(); P.ts(ngmax[:, 0:1], gmax[:, 0:1], -1.0, None, ALU.mult)
                    gex = small.get(); gsum = small.get()
                    P.actf(gex[:, 0:4], lg[:, 0:4], AF.Exp, bias=ngmax[:, 0:1], accum_out=gsum[:, 0:1])
                    gp = small.get(); P.recip(gp[:, 0:1], gsum[:, 0:1])
                    pen = small.get(); P.ts(pen[:, 0:4], goh[:, 0:4], -1.0, BIGV, ALU.add, ALU.mult)
                    elm = small.get()
                    P.tt(elm[:, 0:32].rearrange("p (g e) -> p g e", g=4), lg[:, 4:36].rearrange("p (g e) -> p g e", g=4),
                         bc_last(pen[:, 0:4], 8), ALU.add)
                    m1 = small.get(); P.red(m1[:, 0:1], elm[:, 0:32], ALU.max)
                    rt = small.get()
                    P.ts(rt[:, 0:32], elm[:, 0:32], m1[:, 0:1], None, ALU.is_ge)
                    elm2 = small.get(); P.stt(elm2[:, 0:32], rt[:, 0:32], -BIGV, elm[:, 0:32], ALU.mult, ALU.add)
                    m2 = small.get(); P.red(m2[:, 0:1], elm2[:, 0:32], ALU.max)
                    P.ts(rt[:, 32:64], elm2[:, 0:32], m2[:, 0:1], None, ALU.is_ge)
                    sel = small.get(); P.tt(sel[:, 0:32], rt[:, 0:32], rt[:, 32:64], ALU.add)
                    nm1 = small.get(); P.ts(nm1[:, 0:1], m1[:, 0:1], -1.0, None, ALU.mult)
                    ex = small.get(); P.actf(ex[:, 0:32], elm[:, 0:32], AF.Exp, bias=nm1[:, 0:1])
                    wv = small.get(); P.tt(wv[:, 0:32], ex[:, 0:32], sel[:, 0:32], ALU.mult)
                    ws = small.get(); P.red(ws[:, 0:1], wv[:, 0:32], ALU.add)
                    rws = small.get(); P.recip(rws[:, 0:1], ws[:, 0:1])
                    coef = small.get(); P.tt(coef[:, 0:1], rws[:, 0:1], gp[:, 0:1], ALU.mult)
                    P.ts(rt[:, 64:96], wv[:, 0:32], coef[:, 0:1], None, ALU.mult)
                    ps_p = bank()
                    P.mm(ps_p[:, 0:32], utsf[:], sel[:, 0:32])
                    P.mm(ps_p[:, 32:64], onesf[:], sel[:, 0:32])
                    P.tt(rt[:, 96:128], ps_p[:, 0:32], carry[:], ALU.add)
                    P.tt(carry[:], carry[:], ps_p[:, 32:64], ALU.add)
                    P.op("pool", lambda e, i=i, rt=rt: e.tensor_copy(RT[:, i, :], rt[:, 0:128]), [rt], [rkey(i)])

            stage_pools(NSTR, 17)
            if NSTR > 1:
                set_banks([[0, 1], [2, 3], [4, 5], [6, 7]], [[6, 7]])
            run_streams(P, [(lambda k=k: body1(k)) for k in range(NSTR)])
            set_banks([list(range(8))], [[6, 7]])
            if os.environ.get("MK_MOE_STOP") == "1":
                P.barrier(); P.emit()
                return
            offs = [None]
            carry = P.sb("carry_tot", [128, 32], F32)
            P.copy(carry[:], carries[0][:])
            for k in range(1, NSTR):
                o = P.sb("off%d" % k, [128, 32], F32)
                P.copy(o[:], carry[:])
                offs.append(o)
                P.tt(carry[:], carry[:], carries[k][:], ALU.add)
            cmp = P.sb("cmp", [128, 32, 16], F32)
            P.tt(cmp[:], bc_last(carry[:], 16), bc_mid(thr[:], 32), ALU.is_gt)
            nb = P.sb("nb", [128, 32], F32)
            P.red(nb[:], cmp[:], ALU.add)
            sc = [P.sb("sc0", [128, 32], F32), P.sb("sc1", [128, 32], F32)]
            P.copy(sc[0][:], nb[:])
            cur = 0
            for sh in (1, 2, 4, 8, 16):
                a, bb = sc[cur], sc[1 - cur]
                P.copy(bb[:, 0:sh], a[:, 0:sh])
                P.tt(bb[:, sh:32], a[:, sh:32], a[:, 0:32 - sh], ALU.add)
                cur = 1 - cur
            bend = sc[cur]
            rowst = P.sb("rowst", [128, 32], F32)
            P.tt(rowst[:], bend[:], nb[:], ALU.subtract)
            P.ts(rowst[:], rowst[:], float(BLK), None, ALU.mult)
            cmp2 = P.sb("cmp2", [128, NBLK, 32], F32)
            P.tt(cmp2[:], bc_last(jidx[:], 32), bc_mid(bend[:], NBLK), ALU.is_ge)
            be = P.sb("be", [128, NBLK], F32)
            P.red(be[:], cmp2[:], ALU.add)
            P.ts(be[:], be[:], 128.0, iota[:, 0:1], ALU.mult, ALU.add)
            P.copy(widx[:], be[:])
            rowst_s = [rowst]
            for k in range(1, NSTR):
                rs = P.sb("rowst%d" % k, [128, 32], F32)
                P.tt(rs[:], rowst[:], offs[k][:], ALU.add)
                rowst_s.append(rs)
            hb2_r = Rot(P, "hb2", [128, D], BF16, 3)
            xs_keys = []
            for i in range(NTI):
                dall = small.get()
                rs_ = rowst_s[i // TPS]
                P.op("dve", lambda e, i=i, dall=dall, rs_=rs_: e.tensor_tensor(dall[:, 0:32], RT[:, i, 96:128], rs_[:], ALU.add),
                     [rkey(i), rs_], [dall])
                d4 = small.get()
                tmp = small.get()
                for k in range(2):
                    P.op("dve", lambda e, i=i, k=k, tmp=tmp, dall=dall: e.tensor_tensor(tmp[:, 0:32], RT[:, i, 32 * k:32 * k + 32], dall[:, 0:32], ALU.mult),
                         [rkey(i), dall], [tmp])
                    P.red(d4[:, k:k + 1], tmp[:, 0:32], ALU.add)
                    P.op("dve", lambda e, i=i, k=k, tmp=tmp: e.tensor_tensor(tmp[:, 32:64], RT[:, i, 32 * k:32 * k + 32], RT[:, i, 64:96], ALU.mult),
                         [rkey(i)], [tmp])
                    P.red(d4[:, 2 + k:3 + k], tmp[:, 32:64], ALU.add)
                P.op("dve", lambda e, i=i, d4=d4: e.tensor_copy(IDX[:, i, :], d4[:, 0:2]), [d4], [("IDX", i)])
                P.op("dve", lambda e, i=i, d4=d4: e.tensor_copy(G01[:, i, :], d4[:, 2:4]), [d4], [("G01", i)])
                hb2 = hb2_r.get()
                P.dma(hb2[:], Hn_d[i * 128:(i + 1) * 128, :], reads=[("Hn", i)])
                for k in range(2):
                    P.dma_fn(lambda e, i=i, k=k, hb2=hb2: e.indirect_dma_start(
                        out=Xs_d[:, :], out_offset=bass.IndirectOffsetOnAxis(ap=IDX[:, i, k:k + 1], axis=0),
                        in_=hb2[:], in_offset=None, bounds_check=breg(e, NROWS - 1), oob_is_err=False),
                        "pool", [("IDX", i), hb2] + zkeys, [("Xs", i, k)])
                    xs_keys.append(("Xs", i, k))
            P.barrier()
            P.emit()
          if os.environ.get("MK_MOE_STOP") == "2":
              return
          with contextlib.ExitStack() as st:
            P.stack = st
            stage_pools()
            Wg_r = Rot(P, "Wg", [128, 2048], BF16, 3); Wu_r = Rot(P, "Wu", [128, 2048], BF16, 3)
            Wd_r = Rot(P, "Wd", [128, 2048], BF16, 3)
            xb_r = Rot(P, "xbm", [128, D], BF16, 12)
            xT_r = Rot(P, "xTm", [128, 8, 512], BF16, 2)
            aT_r = Rot(P, "aTm", [128, 2, 512], BF16, 2)
            sg_r = Rot(P, "sgm", [128, 512], F32, 3)
            yb_r = Rot(P, "ybm", [128, D], BF16, 4)
            def loads3(j):
                Wg = Wg_r.get(); Wu = Wu_r.get(); Wd = Wd_r.get()
                for Wt, src in ((Wg, ewg_l), (Wu, ewu_l), (Wd, ewd_l)):
                    P.dma_fn(lambda e, Wt=Wt, src=src, j=j: e.indirect_dma_start(
                        out=Wt[:], out_offset=None, in_=src[l][:, :],
                        in_offset=bass.IndirectOffsetOnAxis(ap=widx[:, j:j + 1], axis=0),
                        bounds_check=breg(e, NEXP * 128 - 1), oob_is_err=False), "pool", [widx], [Wt])
                xbs = []
                for t in range(4):
                    xb = xb_r.get()
                    r0 = j * BLK + t * 128
                    P.dma(xb[:], Xs_d[r0:r0 + 128, :], reads=["Xs_all"])
                    xbs.append(xb)
                return Wg, Wu, Wd, xbs

            nxt = loads3(0)
            for j in range(NBLK):
                Wg, Wu, Wd, xbs = nxt
                if j + 1 < NBLK:
                    nxt = loads3(j + 1)
                Wg3 = Wg[:].rearrange("p (kc f) -> p kc f", kc=8)
                Wu3 = Wu[:].rearrange("p (kc f) -> p kc f", kc=8)
                Wd3 = Wd[:].rearrange("p (c f) -> p c f", c=2)
                xT = xT_r.get()
                for t in range(4):
                    transpose_to(xbs[t][:], 8, 128, xT[:, :, t * 128:(t + 1) * 128])
                pg = [bank(), bank()]
                pu = [bank(), bank()]
                for fc in range(2):
                    for kc in range(8):
                        P.mm(pg[fc][:, :], Wg3[:, kc, fc * 128:(fc + 1) * 128], xT[:, kc, :], start=(kc == 0), stop=(kc == 7))
                    for kc in range(8):
                        P.mm(pu[fc][:, :], Wu3[:, kc, fc * 128:(fc + 1) * 128], xT[:, kc, :], start=(kc == 0), stop=(kc == 7))
                aT = aT_r.get()
                for fc in range(2):
                    sg = sg_r.get()
                    P.actf(sg[:], pg[fc][:, :], AF.Silu)
                    P.tt(aT[:, fc, :], sg[:], pu[fc][:, :], ALU.mult)
                for t in range(4):
                    yb = yb_r.get()
                    for hf in range(2):
                        pd = bank()
                        for fc in range(2):
                            P.mm(pd[:, :], aT[:, fc, t * 128:(t + 1) * 128], Wd3[:, fc, hf * 512:(hf + 1) * 512],
                                 start=(fc == 0), stop=(fc == 1))
                        P.copy(yb[:, hf * 512:(hf + 1) * 512], pd[:, :], eng=("act" if hf == 0 else "dve"))
                    r0 = j * BLK + t * 128
                    P.dma(Ys_d[r0:r0 + 128, :], yb[:], queue="sp", writes=[("Ys", j, t)])
            P.barrier()
            P.emit()
          if os.environ.get("MK_MOE_STOP") == "3":
              return
          with contextlib.ExitStack() as st:
            P.stack = st
            stage_pools()
            x_r = Rot(P, "xm4", [128, D], F32, 4)
            y_r = Rot(P, "ym4", [128, D], BF16, 8)
            def loads4(i):
                b, tile = tile_of(i)
                x1 = x_r.get()
                P.dma(x1[:], out_d[b, tile * 128:(tile + 1) * 128, :], reads=[okey(b, tile)])
                ys = []
                for k in range(2):
                    y = y_r.get()
                    P.dma_fn(lambda e, i=i, k=k, y=y: e.indirect_dma_start(
                        out=y[:], out_offset=None, in_=Ys_d[:, :],
                        in_offset=bass.IndirectOffsetOnAxis(ap=IDX[:, i, k:k + 1], axis=0),
                        bounds_check=breg(e, NROWS - 1), oob_is_err=False), "pool", ["Ys_all", IDX], [y])
                    ys.append(y)
                return x1, ys

            nxt = loads4(0)
            for i in range(NTI):
                b, tile = tile_of(i)
                x1, ys = nxt
                if i + 1 < NTI:
                    nxt = loads4(i + 1)
                for k in range(2):
                    y = ys[k]
                    P.op("dve", lambda e, i=i, k=k, y=y, x1=x1: e.scalar_tensor_tensor(x1[:], y[:], G01[:, i, k:k + 1], x1[:], ALU.mult, ALU.add),
                         [y, x1, G01], [x1])
                P.dma(out_d[b, tile * 128:(tile + 1) * 128, :], x1[:], queue="sp", writes=[okey(b, tile)])
            if l == 0:
                dbg_dump("moe0")
            stage_end()

        if run("XA0"):
            stage_xa(0)
        SPARSE = bool(int(os.environ.get("MK_SPARSE", "1")))
        if run("MOE0"):
            (stage_moe_sparse if SPARSE else stage_moe)(0)
        if run("POOL"):
            stage_pool()
        if run("XA1"):
            stage_xa(1)
        if run("MOE1"):
            (stage_moe_sparse if SPARSE else stage_moe)(1)
        P.barrier(new_sems=False)
        P.emit()
        return nc, P, None


def host_consts():
    i = np.arange(128)
    c = {}
    c["c_ident"] = np.eye(128, dtype=np.float32)
    c["c_ut"] = (i[:, None] <= i[None, :]).astype(np.float32)
    c["c_ls"] = (i[:, None] > i[None, :]).astype(np.float32)
    c["c_invf"] = (THETA ** (-np.arange(0, ROPE // 2, dtype=np.float32) * 2.0 / ROPE)).astype(np.float32)
    c["c_uts"] = (i[:, None] < i[None, :]).astype(np.float32)
    c["c_thr"] = (np.arange(16) * 512).astype(np.float32)
    c["c_iota"] = np.arange(128, dtype=np.float32).reshape(128, 1)
    t = np.arange(512, dtype=np.float32)
    c["c_icnt"] = np.stack([1.0 / np.minimum(t + 1.0, float(w)) for w in (2, 4, 8, 16)]).astype(np.float32)
    return c


WEIGHT_KEYS = ["ln_mix", "w_in", "q_lat_norm", "w_uq", "kv_lat_norm", "w_ukv", "q_norm", "k_norm",
               "dt_bias", "a_log", "d_skip", "ssd_norm", "w_out", "pool_w", "pool_b", "pool_scale",
               "ln_xq", "ln_mem", "xq_w", "xkv_w", "xq_norm", "xk_norm", "xo_w", "ln_ffn", "rg_w", "rg_b",
               "re_w", "re_b"]


def make_in_maps(inputs, NS, S, n_cores):
    NT = S // 128
    shared = {k: np.ascontiguousarray(np.asarray(inputs[k], dtype=np.float32)) for k in WEIGHT_KEYS}
    cw = np.asarray(inputs["conv_w"], dtype=np.float32)[0]
    shared["conv_wl"] = np.ascontiguousarray(cw.reshape(CK, 8, 128).transpose(2, 1, 0))
    cb = np.asarray(inputs["conv_b"], dtype=np.float32)[0]
    shared["conv_bl"] = np.ascontiguousarray(cb.reshape(8, 128).T)
    shared.update(host_consts())
    nblk = (2 * NS * S) // 512 + NEXP
    shared["c_jidx"] = np.arange(nblk, dtype=np.float32)
    g = np.asarray(inputs["exp_w_gate"], dtype=np.float32); u = np.asarray(inputs["exp_w_up"], dtype=np.float32)
    dn = np.asarray(inputs["exp_w_down"], dtype=np.float32)
    for li in range(2):
        shared["ewg_l%d" % li] = np.ascontiguousarray(g[li].reshape(NEXP, 8, 128, EFF).transpose(0, 2, 1, 3)).reshape(NEXP * 128, 2048)
        shared["ewu_l%d" % li] = np.ascontiguousarray(u[li].reshape(NEXP, 8, 128, EFF).transpose(0, 2, 1, 3)).reshape(NEXP * 128, 2048)
        shared["ewd_l%d" % li] = np.ascontiguousarray(dn[li].reshape(NEXP, 2, 128, D).transpose(0, 2, 1, 3)).reshape(NEXP * 128, 2048)
    x = np.asarray(inputs["x"]); mem = np.asarray(inputs["mem"]); pos = np.asarray(inputs["positions"])
    maps = []
    for c in range(n_cores):
        sl = slice(c * NS, (c + 1) * NS)
        m = dict(shared)
        m["x"] = np.ascontiguousarray(x[sl], dtype=np.float32)
        m["mem"] = np.ascontiguousarray(mem[sl], dtype=np.float32)
        p = pos[sl].astype(np.int32).reshape(NS * NT, 128).T
        m["posT"] = np.ascontiguousarray(p)
        maps.append(m)
    return maps


_CACHE = {}


def kernel(**inputs):
    n_cores = 8
    x = np.asarray(inputs["x"])
    B, S, _ = x.shape
    NS = B // n_cores
    key = (NS, S)
    if key not in _CACHE:
        nc, _, _ = build_program(NS, S, dbg=False)
        _CACHE[key] = nc
    nc = _CACHE[key]
    maps = make_in_maps(inputs, NS, S, n_cores)
    res = run_bass_kernel_spmd(nc, maps, core_ids=list(range(n_cores)))
    out = np.concatenate([np.asarray(r["out"]) for r in res.results], axis=0)
    return out.astype(np.float32, copy=False)
```
